# Optimizing a Trainium2 kernel written in Bass

```python
import math
import jax, jax.numpy as jnp
from jax import lax
import numpy as np

D_MODEL = 1024
BATCH = 16
SEQ = 4096
DEPTH = 4

N_MIXERS = 2
N_HGRN_LAYERS = (DEPTH + 1) // 2
N_ATTN_LAYERS = DEPTH // 2

HGRN_EXPAND = 128
HGRN_HEADS = D_MODEL // HGRN_EXPAND
HGRN_HEAD_K = HGRN_EXPAND
HGRN_HEAD_V = D_MODEL // HGRN_HEADS
HGRN_CHUNK = 32

ATTN_HEAD_DIM = 64
ATTN_HEADS = D_MODEL // (2 * ATTN_HEAD_DIM)
ATTN_BLOCK = 128
ROPE_THETA = 10000.0

MOE_GROUPS = 4
MOE_EXPERTS_PER_GROUP = 8
MOE_EXPERTS = MOE_GROUPS * MOE_EXPERTS_PER_GROUP
MOE_TOP_K = 2
MOE_FF = D_MODEL // 2
MOE_BLOCK = 256

DEEPNORM_ALPHA = (2 * DEPTH) ** 0.25
DEEPNORM_BETA = (8 * DEPTH) ** -0.25
NORM_EPS = 1e-5

kernel_name = "hybrid_hgrn2_diffattn_hmoe_deepnorm"


def layer_norm(x, g, b):
    xf = x.astype(jnp.float32)
    mu = jnp.mean(xf, axis=-1, keepdims=True)
    var = jnp.mean(jnp.square(xf - mu), axis=-1, keepdims=True)
    y = (xf - mu) * lax.rsqrt(var + NORM_EPS) * g.astype(jnp.float32) + b.astype(jnp.float32)
    return y.astype(x.dtype)


def rms_norm(x, w):
    xf = x.astype(jnp.float32)
    return xf * lax.rsqrt(jnp.mean(jnp.square(xf), axis=-1, keepdims=True) + NORM_EPS) * w.astype(jnp.float32)


def rope(t):
    S, d = t.shape[1], t.shape[-1]
    half = d // 2
    inv_freq = ROPE_THETA ** (-jnp.arange(half, dtype=jnp.float32) / half)
    ang = jnp.arange(S, dtype=jnp.float32)[:, None] * inv_freq[None, :]
    cos = jnp.cos(ang)[None, :, None, :]
    sin = jnp.sin(ang)[None, :, None, :]
    tf = t.astype(jnp.float32)
    t1, t2 = tf[..., :half], tf[..., half:]
    return jnp.concatenate([t1 * cos - t2 * sin, t2 * cos + t1 * sin], axis=-1).astype(t.dtype)


def hgrn2_mixer(h, w_in, w_out, lower_bound, norm_w):
    B, S, D = h.shape
    H, K, V, C = HGRN_HEADS, HGRN_HEAD_K, HGRN_HEAD_V, HGRN_CHUNK
    n_chunks = S // C
    q, z, v, gate = jnp.split(h @ w_in, 4, axis=-1)
    z = z.astype(jnp.float32)
    lb = lower_bound.astype(jnp.float32)
    log_f = jnp.logaddexp(jax.nn.log_sigmoid(z), jnp.log(lb) + jax.nn.log_sigmoid(-z))
    k = (1.0 - lb) * jax.nn.sigmoid(-z)

    def to_chunks(t, dh):
        return t.astype(jnp.float32).reshape(B, n_chunks, C, H, dh).transpose(1, 0, 3, 2, 4)

    causal = jnp.tril(jnp.ones((C, C), dtype=bool))[:, :, None]

    def chunk_step(state, inp):
        q_c, k_c, v_c, g_c = inp
        b = jnp.cumsum(g_c, axis=2)
        o_inter = jnp.einsum('bhtk,bhkv->bhtv', q_c * jnp.exp(b), state)
        rel = b[:, :, :, None, :] - b[:, :, None, :, :]
        decay = jnp.where(causal, jnp.exp(jnp.where(causal, rel, 0.0)), 0.0)
        scores = jnp.einsum('bhtk,bhsk,bhtsk->bhts', q_c, k_c, decay)
        o_c = o_inter + jnp.einsum('bhts,bhsv->bhtv', scores, v_c)
        b_last = b[:, :, -1:, :]
        state = (jnp.exp(b_last[:, :, 0, :, None]) * state
                 + jnp.einsum('bhsk,bhsv->bhkv', k_c * jnp.exp(b_last - b), v_c))
        return state, o_c

    state0 = jnp.zeros((B, H, K, V), jnp.float32)
    _, o = lax.scan(chunk_step, state0,
                    (to_chunks(q, K), to_chunks(k, K), to_chunks(v, V), to_chunks(log_f, K)))
    o = o.transpose(1, 0, 3, 2, 4).reshape(B, S, H, V)
    o = rms_norm(o, norm_w) * jax.nn.silu(gate.astype(jnp.float32).reshape(B, S, H, V))
    return o.reshape(B, S, D).astype(h.dtype) @ w_out


def diff_attention(h, w_in, w_out, lam_params, subln_w, lambda_init):
    B, S, D = h.shape
    H, d, Q = ATTN_HEADS, ATTN_HEAD_DIM, ATTN_BLOCK
    q, k, v = jnp.split(h @ w_in, 3, axis=-1)
    q = rope(q.reshape(B, S, 2 * H, d)).reshape(B, S, H, 2, d).transpose(0, 2, 3, 1, 4)
    k = rope(k.reshape(B, S, 2 * H, d)).reshape(B, S, H, 2, d).transpose(0, 2, 3, 1, 4)
    v = v.reshape(B, S, H, 2 * d).transpose(0, 2, 1, 3).astype(jnp.float32)
    lp = lam_params.astype(jnp.float32)
    lam = jnp.exp(jnp.sum(lp[0] * lp[1])) - jnp.exp(jnp.sum(lp[2] * lp[3])) + lambda_init
    scale = d ** -0.5
    outs = []
    for blk in range(S // Q):
        q0, kend = blk * Q, (blk + 1) * Q
        s = jnp.einsum('bhmqd,bhmkd->bhmqk', q[:, :, :, q0:kend], k[:, :, :, :kend]).astype(jnp.float32) * scale
        mask = (q0 + jnp.arange(Q))[:, None] >= jnp.arange(kend)[None, :]
        p = jax.nn.softmax(jnp.where(mask, s, -jnp.inf), axis=-1)
        a = p[:, :, 0] - lam * p[:, :, 1]
        outs.append(jnp.einsum('bhqk,bhkv->bhqv', a, v[:, :, :kend]))
    o = jnp.concatenate(outs, axis=2)
    o = rms_norm(o, subln_w) * (1.0 - lambda_init)
    return o.transpose(0, 2, 1, 3).reshape(B, S, D).astype(h.dtype) @ w_out


def hierarchical_moe(h, wg, bg, we, be, w1, w3, w2):
    B, S, D = h.shape
    N = B * S
    E, G, EPG, T = MOE_EXPERTS, MOE_GROUPS, MOE_EXPERTS_PER_GROUP, MOE_BLOCK
    xf = h.reshape(N, D)
    g_prob = jax.nn.softmax((xf @ wg).astype(jnp.float32) + bg.astype(jnp.float32), axis=-1)
    g_w, g_idx = lax.top_k(g_prob, 1)
    e_logits = (xf @ we).astype(jnp.float32).reshape(N, G, EPG) + be.astype(jnp.float32).reshape(G, EPG)
    e_prob = jax.nn.softmax(jnp.take_along_axis(e_logits, g_idx[:, :, None], axis=1)[:, 0], axis=-1)
    e_w, e_loc = lax.top_k(e_prob, MOE_TOP_K)
    weights = g_w * (e_w / jnp.sum(e_w, axis=-1, keepdims=True))
    expert = g_idx * EPG + e_loc

    A = N * MOE_TOP_K
    eid = expert.reshape(A)
    tok = jnp.repeat(jnp.arange(N, dtype=jnp.int32), MOE_TOP_K)
    wt = weights.reshape(A)
    order = jnp.argsort(eid)
    eid_s, tok_s, wt_s = eid[order], tok[order], wt[order]
    counts = jnp.bincount(eid, length=E)
    start = jnp.cumsum(counts) - counts
    padded = ((counts + T - 1) // T) * T
    pad_end = jnp.cumsum(padded)
    pad_start = pad_end - padded
    dest = pad_start[eid_s] + (jnp.arange(A, dtype=jnp.int32) - start[eid_s])
    L = A + E * T
    NB = L // T
    buf_tok = jnp.full((L,), N, jnp.int32).at[dest].set(tok_s)
    buf_w = jnp.zeros((L,), jnp.float32).at[dest].set(wt_s)
    blk_expert = jnp.minimum(jnp.searchsorted(pad_end, jnp.arange(NB, dtype=jnp.int32) * T, side='right'), E - 1)
    x_pad = jnp.concatenate([xf, jnp.zeros((1, D), xf.dtype)], axis=0)
    x_buf = x_pad[buf_tok].reshape(NB, T, D)

    def expert_block(args):
        xb, e = args
        return (jax.nn.silu(xb @ w1[e]) * (xb @ w3[e])) @ w2[e]

    y_buf = lax.map(expert_block, (x_buf, blk_expert)).reshape(L, D)
    y = jax.ops.segment_sum(y_buf.astype(jnp.float32) * buf_w[:, None], buf_tok, num_segments=N + 1)[:N]
    return y.reshape(B, S, D).astype(h.dtype)


def setup_inputs(seed: int = 0) -> dict:
    key = jax.random.key(seed)
    ks = jax.random.split(key, 24)
    D, F, E = D_MODEL, MOE_FF, MOE_EXPERTS
    nrm = lambda k, shape, s: jax.random.normal(k, shape, jnp.float32) * s
    hgrn_w_in = nrm(ks[6], (N_HGRN_LAYERS, D, 4 * D), D ** -0.5)
    hgrn_w_in = hgrn_w_in.at[:, :, 2 * D:3 * D].multiply(DEEPNORM_BETA)
    attn_w_in = nrm(ks[10], (N_ATTN_LAYERS, D, 3 * D), D ** -0.5)
    attn_w_in = attn_w_in.at[:, :, 2 * D:].multiply(DEEPNORM_BETA)
    return {
        "x": nrm(ks[0], (BATCH, SEQ, D), 1.0),
        "c": nrm(ks[1], (BATCH, D), 1.0),
        "ada_w": nrm(ks[2], (DEPTH, D, 6 * D), 0.5 * D ** -0.5),
        "ada_b": nrm(ks[3], (DEPTH, 6 * D), 0.02),
        "ln_g": 1.0 + nrm(ks[4], (DEPTH, 2, D), 0.02),
        "ln_b": nrm(ks[5], (DEPTH, 2, D), 0.02),
        "hgrn_w_in": hgrn_w_in,
        "hgrn_w_out": nrm(ks[7], (N_HGRN_LAYERS, D, D), DEEPNORM_BETA * D ** -0.5),
        "hgrn_lb": nrm(ks[8], (N_HGRN_LAYERS, HGRN_HEADS * HGRN_HEAD_K), 0.5),
        "hgrn_norm_w": 1.0 + nrm(ks[9], (N_HGRN_LAYERS, HGRN_HEAD_V), 0.02),
        "attn_w_in": attn_w_in,
        "attn_w_out": nrm(ks[11], (N_ATTN_LAYERS, D, D), DEEPNORM_BETA * D ** -0.5),
        "attn_lambda": nrm(ks[12], (N_ATTN_LAYERS, 4, ATTN_HEAD_DIM), 0.1),
        "attn_subln_w": 1.0 + nrm(ks[13], (N_ATTN_LAYERS, 2 * ATTN_HEAD_DIM), 0.02),
        "router_g_w": nrm(ks[14], (DEPTH, D, MOE_GROUPS), D ** -0.5),
        "router_g_b": nrm(ks[15], (DEPTH, MOE_GROUPS), 0.01),
        "router_e_w": nrm(ks[16], (DEPTH, D, E), D ** -0.5),
        "router_e_b": nrm(ks[17], (DEPTH, E), 0.01),
        "moe_w1": nrm(ks[18], (DEPTH, E, D, F), D ** -0.5),
        "moe_w3": nrm(ks[19], (DEPTH, E, D, F), D ** -0.5),
        "moe_w2": nrm(ks[20], (DEPTH, E, F, D), DEEPNORM_BETA * F ** -0.5),
    }


def reference(x, c, ada_w, ada_b, ln_g, ln_b, hgrn_w_in, hgrn_w_out, hgrn_lb, hgrn_norm_w,
              attn_w_in, attn_w_out, attn_lambda, attn_subln_w, router_g_w, router_g_b,
              router_e_w, router_e_b, moe_w1, moe_w3, moe_w2):
    lb_all = jnp.cumsum(jax.nn.softmax(hgrn_lb.astype(jnp.float32), axis=0), axis=0)
    lb_all = lb_all - lb_all[0:1]
    cond = jax.nn.silu(c.astype(jnp.float32))
    for i in range(DEPTH):
        mod = (cond @ ada_w[i].astype(jnp.float32) + ada_b[i].astype(jnp.float32)).astype(x.dtype)
        sh1, sc1, g1, sh2, sc2, g2 = jnp.split(mod[:, None, :], 6, axis=-1)
        h = x * (1 + sc1) + sh1
        j = i // N_MIXERS
        if i % N_MIXERS == 0:
            y = hgrn2_mixer(h, hgrn_w_in[j], hgrn_w_out[j], lb_all[j], hgrn_norm_w[j])
        else:
            lambda_init = 0.8 - 0.6 * math.exp(-0.3 * i)
            y = diff_attention(h, attn_w_in[j], attn_w_out[j], attn_lambda[j], attn_subln_w[j], lambda_init)
        x = layer_norm(DEEPNORM_ALPHA * x + g1 * y, ln_g[i, 0], ln_b[i, 0])
        h = x * (1 + sc2) + sh2
        y = hierarchical_moe(h, router_g_w[i], router_g_b[i], router_e_w[i], router_e_b[i],
                             moe_w1[i], moe_w3[i], moe_w2[i])
        x = layer_norm(DEEPNORM_ALPHA * x + g2 * y, ln_g[i, 1], ln_b[i, 1])
    return x
```

```python
import contextlib
import numpy as np
import concourse.bass as bass
import concourse.mybir as mybir

F32 = mybir.dt.float32
BF16 = mybir.dt.bfloat16
I32 = mybir.dt.int32
U32 = mybir.dt.uint32
AF = mybir.ActivationFunctionType
ALU = mybir.AluOpType
AX = mybir.AxisListType

QUEUES = ("pe", "act", "dve", "pool", "sp")
N_DMA_CH = 12


class Buf:
    __slots__ = ("name", "w", "r", "excl")

    def __init__(self, name="", excl=False):
        self.name = name
        self.excl = excl
        self.w = None
        self.r = []


class Prog:
    def __init__(self, nc, same_engine_sync=True):
        self.nc = nc
        self.stack = contextlib.ExitStack()
        self.ops = {q: [] for q in QUEUES}
        self.cnt = {q: 0 for q in QUEUES}
        self.sems = {}
        self.seen = {q: {} for q in QUEUES}
        self.same_engine_sync = same_engine_sync
        for q in QUEUES:
            self.sems["e_" + q] = self.stack.enter_context(nc.semaphore("e_" + q))
        self.dma_ch = {}
        self.dma_rr = {}
        for q in ("sp", "act", "pool"):
            chs = []
            for i in range(N_DMA_CH):
                key = "d_%s_%d" % (q, i)
                self.sems[key] = self.stack.enter_context(nc.semaphore(key))
                chs.append([key, 0])
            self.dma_ch[q] = chs
            self.dma_rr[q] = 0
        self.n_inst = 0
        self.uid = 0
        self.scopes = [self.stack]

    def sb(self, name, shape, dtype):
        self.uid += 1
        return self.scopes[-1].enter_context(self.nc.sbuf_tensor("sb%d_%s" % (self.uid, name), list(shape), dtype))

    def ps(self, name, shape, dtype=F32):
        self.uid += 1
        return self.scopes[-1].enter_context(self.nc.psum_tensor("ps%d_%s" % (self.uid, name), list(shape), dtype))

    def push(self):
        self.scopes.append(contextlib.ExitStack())

    def pop(self):
        self.barrier()
        self.scopes.pop().close()

    def barrier(self):
        targets = []
        for q in QUEUES:
            if self.cnt[q] > 0:
                targets.append(("e_" + q, self.cnt[q], q))
        for q, chs in self.dma_ch.items():
            for key, val in chs:
                if val > 0:
                    targets.append((key, val, None))
        sems = self.sems
        for q in QUEUES:
            seen = self.seen[q]
            waits = []
            for key, val, wq in targets:
                if wq == q and q != "sp":
                    continue
                if seen.get(key, 0) >= val:
                    continue
                seen[key] = val
                waits.append((key, val))

            def emit(eng, waits=waits):
                for k, v in waits:
                    eng.wait_ge(sems[k], v)

            self.ops[q].append(emit)
            self.n_inst += len(waits)

    def _collect(self, q, reads, writes, is_dma):
        waits = {}

        def need(tok):
            key, val, wq, wdma = tok
            if (not wdma) and (not is_dma) and wq == q:
                if q == "pe" or not self.same_engine_sync:
                    return
            if waits.get(key, 0) < val:
                waits[key] = val

        for b in reads:
            if b.w is not None:
                need(b.w)
        for b in writes:
            if b.w is not None:
                need(b.w)
            for t in b.r:
                need(t)
        out = []
        seen = self.seen[q]
        for key, val in waits.items():
            if seen.get(key, 0) >= val:
                continue
            seen[key] = val
            out.append((key, val))
        return out

    def _commit(self, tok, reads, writes):
        for b in reads:
            b.r.append(tok)
        for b in writes:
            b.w = tok
            b.r = []

    def op(self, q, fn, reads=(), writes=()):
        ex = [b for b in reads if b.excl]
        if ex:
            writes = list(writes) + [b for b in ex if b not in writes]
        waits = self._collect(q, reads, writes, False)
        self.cnt[q] += 1
        key = "e_" + q
        val = self.cnt[q]
        tok = (key, val, q, False)
        sems = self.sems

        def emit(eng, waits=waits, fn=fn, key=key):
            for k, v in waits:
                eng.wait_ge(sems[k], v)
            fn(eng).then_inc(sems[key], 1)

        self.ops[q].append(emit)
        self.n_inst += 1 + len(waits)
        self._commit(tok, reads, writes)
        return tok

    def dma(self, q, fn, reads=(), writes=()):
        waits = self._collect(q, reads, writes, True)
        chs = self.dma_ch[q]
        i = self.dma_rr[q]
        self.dma_rr[q] = (i + 1) % len(chs)
        ch = chs[i]
        key = ch[0]
        prev = ch[1]
        ch[1] += 16
        val = ch[1]
        seen = self.seen[q]
        if prev > 0 and seen.get(key, 0) < prev:
            seen[key] = prev
            waits = waits + [(key, prev)]
        tok = (key, val, q, True)
        sems = self.sems

        def emit(eng, waits=waits, fn=fn, key=key):
            for k, v in waits:
                eng.wait_ge(sems[k], v)
            fn(eng).then_inc(sems[key], 16)

        self.ops[q].append(emit)
        self.n_inst += 1 + len(waits)
        self._commit(tok, reads, writes)
        return tok

    def finish(self):
        nc = self.nc
        final = []
        for q, chs in self.dma_ch.items():
            for key, val in chs:
                if val > 0:
                    final.append((key, val))
        for q in QUEUES:
            if q != "sp" and self.cnt[q] > 0:
                final.append(("e_" + q, self.cnt[q]))
        sems = self.sems
        ops = self.ops
        with nc.Block() as block:
            @block.tensor
            def _(eng):
                for f in ops["pe"]:
                    f(eng)

            @block.scalar
            def _(eng):
                for f in ops["act"]:
                    f(eng)

            @block.vector
            def _(eng):
                for f in ops["dve"]:
                    f(eng)

            @block.gpsimd
            def _(eng):
                for f in ops["pool"]:
                    f(eng)

            @block.sync
            def _(eng):
                for f in ops["sp"]:
                    f(eng)
                for k, v in final:
                    eng.wait_ge(sems[k], v)
        self.stack.close()

DEBUG = False
MOE_STAGE = 2
MOE_CUT = 2
D = 1024
DEPTH = 4
ALPHA = (2 * DEPTH) ** 0.25
EPS = 1e-5
WDEPTH = 4


def wnames():
    L = WDEPTH
    return [("ada_w", [L, 1024, 6144]), ("ada_b", [L, 6144]), ("ln_g", [L, 2, 1024]), ("ln_b", [L, 2, 1024]),
            ("hgrn_w_in", [2, 1024, 4096]), ("hgrn_w_out", [2, 1024, 1024]), ("hgrn_lb", [2, 1024]),
            ("hgrn_norm_w", [2, 128]), ("attn_w_in", [2, 1024, 3072]), ("attn_w_out", [2, 1024, 1024]),
            ("attn_lambda", [2, 4, 64]), ("attn_subln_w", [2, 128]), ("router_w", [L, 128, 8, 36]), ("router_b", [L, 36]),
            ("moe_w1", [L, 32, 1024, 512]), ("moe_w3", [L, 32, 1024, 512]), ("moe_w2", [L, 32, 512, 1024])]


def make_consts(S):
    c = {}
    c["identf"] = np.eye(128, dtype=np.float32)
    s = np.arange(128)[:, None]
    t = np.arange(128)[None, :]
    c["blockmask"] = ((s // 32 == t // 32) & (s <= t)).astype(np.float32)
    c["trimask"] = (s <= t).astype(np.float32)
    sm = np.ones((128, 128), np.float32)
    sm[:, ::32] = 0.0
    c["scanmask"] = sm
    ci = np.zeros((128, 4), np.float32)
    for k in range(4):
        ci[k * 32:(k + 1) * 32, k] = 1.0
    c["chunkind"] = ci
    half = 32
    inv_freq = (np.float32(10000.0) ** (-np.arange(half, dtype=np.float32) / np.float32(half))).astype(np.float32)
    ang = (np.arange(S, dtype=np.float32)[:, None] * inv_freq[None, :]).astype(np.float32)
    cos = np.cos(ang).astype(np.float32).T
    sin = np.sin(ang).astype(np.float32).T
    c["ropecos"] = np.ascontiguousarray(np.concatenate([cos, cos, cos, cos], 0))
    c["ropesin"] = np.ascontiguousarray(np.concatenate([-sin, sin, -sin, sin], 0))
    return c


def build(NSEQ, S, layers, sub=("mix", "moe"), NE=32):
    NT = S // 128
    NTOK = NSEQ * S
    nc = bass.Bass("TRN2", target_bir_lowering=False)
    dtn = nc.dram_tensor
    x_d = dtn("x", [NTOK, D], F32, kind="ExternalInput").ap()
    c_d = dtn("cT", [128, 8, NSEQ], F32, kind="ExternalInput").ap()
    W = {}
    for nm, shp in wnames():
        W[nm] = dtn(nm, shp, F32, kind="ExternalInput").ap()
    consts = make_consts(S)
    CD = {}
    for nm, arr in consts.items():
        CD[nm] = dtn(nm, list(arr.shape), F32, kind="ExternalInput").ap()
    out_d = dtn("out", [NTOK, D], F32, kind="ExternalOutput").ap()
    xs = [dtn("xs0", [NTOK, D], F32, kind="Internal").ap(), dtn("xs1", [NTOK, D], F32, kind="Internal").ap()]
    mod_d = dtn("mod_d", [WDEPTH, NSEQ, 6144], F32, kind="Internal").ap()
    on_d = dtn("on_d", [NTOK, D], BF16, kind="Internal").ap()

    P = Prog(nc)
    DBG = {}

    def dbg(name, ap, bufs, psum=False):
        if not DEBUG or name in DBG:
            return
        shp = list(ap.shape)
        d = dtn("dbg_" + name, shp, F32, kind="ExternalOutput").ap()
        DBG[name] = d
        if psum:
            tmp = P.sb("dbgtmp_" + name, shp, F32)
            tb = Buf()
            P.op("dve", lambda e: e.tensor_copy(out=tmp[:], in_=ap), nrm(bufs), [tb])
            P.dma("pool", lambda e: e.dma_start(out=d, in_=tmp[:]), [tb], [])
        else:
            P.dma("pool", lambda e: e.dma_start(out=d, in_=ap), nrm(bufs), [])

    def nrm(l):
        return [b.b if hasattr(b, "b") else b for b in l]

    def MM(out, lhsT, rhs, start, stop, r, w):
        P.op("pe", lambda e: e.matmul(out, lhsT=lhsT, rhs=rhs, start=start, stop=stop), nrm(r), nrm(w))

    def TR(out, in_, ident, r, w):
        P.op("pe", lambda e: e.transpose(out=out, in_=in_, identity=ident), nrm(r), nrm(w))

    def ACT(out, in_, func, r, w, **kw):
        P.op("act", lambda e: e.activation(out=out, in_=in_, func=func, **kw), nrm(r), nrm(w))

    def TT(q, out, in0, in1, op, r, w):
        P.op(q, lambda e: e.tensor_tensor(out=out, in0=in0, in1=in1, op=op), nrm(r), nrm(w))

    def TS(q, out, in0, s1, s2, op0, op1, r, w):
        if op1 is None:
            P.op(q, lambda e: e.tensor_scalar(out=out, in0=in0, scalar1=s1, scalar2=None, op0=op0), nrm(r), nrm(w))
        else:
            P.op(q, lambda e: e.tensor_scalar(out=out, in0=in0, scalar1=s1, scalar2=s2, op0=op0, op1=op1), nrm(r), nrm(w))

    def STT(out, in0, scalar, in1, op0, op1, r, w):
        P.op("dve", lambda e: e.scalar_tensor_tensor(out=out, in0=in0, scalar=scalar, in1=in1, op0=op0, op1=op1), nrm(r), nrm(w))

    def CP(q, out, in_, r, w):
        P.op(q, lambda e: e.tensor_copy(out=out, in_=in_), nrm(r), nrm(w))

    def MS(q, ap, val, w):
        P.op(q, lambda e: e.memset(ap, val), [], nrm(w))

    def DMA(q, out, in_, r, w, **kw):
        P.dma(q, lambda e: e.dma_start(out=out, in_=in_, **kw), nrm(r), nrm(w))

    class Tl:
        def __init__(self, name, shape, dtype, psum=False):
            self.t = P.ps(name, shape, dtype) if psum else P.sb(name, shape, dtype)
            self.b = Buf(name, excl=psum)

        def __getitem__(self, k):
            return self.t[k]

    xb = {"x": [Buf() for _ in range(NSEQ * NT)], 0: [Buf() for _ in range(NSEQ * NT)],
          1: [Buf() for _ in range(NSEQ * NT)], "out": [Buf() for _ in range(NSEQ * NT)]}
    modb = Buf("mod_d")
    onb = [Buf() for _ in range(NSEQ * NT)]

    identf = Tl("identf", [128, 128], F32)
    identb = Tl("identb", [128, 128], BF16)
    blockmask = Tl("blockmask", [128, 128], F32)
    trimask = Tl("trimask", [128, 128], BF16)
    scanmask = Tl("scanmask", [128, 128], F32)
    chunkind = Tl("chunkind", [128, 4], F32)
    DMA("sp", identf[:], CD["identf"], [], [identf])
    DMA("pool", identb[:], CD["identf"], [], [identb])
    DMA("sp", blockmask[:], CD["blockmask"], [], [blockmask])
    DMA("pool", trimask[:], CD["trimask"], [], [trimask])
    DMA("sp", scanmask[:], CD["scanmask"], [], [scanmask])
    DMA("sp", chunkind[:], CD["chunkind"], [], [chunkind])

    def phase_mod():
        P.push()
        condT = Tl("condT", [128, 8, NSEQ], F32)
        DMA("sp", condT[:], c_d, [], [condT])
        ACT(condT[:], condT[:], AF.Silu, [condT], [condT])
        stg = [Tl("adastg%d" % i, [128, 8, 512], F32) for i in range(2)]
        adab = Tl("adab", [NSEQ, 6144], F32)
        modrow = Tl("modrow", [NSEQ, 6144], F32)
        mps = [Tl("modps%d" % i, [128, 512], F32, psum=True) for i in range(2)]
        n = 0
        for l in layers:
            DMA("sp", adab[:], W["ada_b"][l:l + 1, :].partition_broadcast(NSEQ) if NSEQ > 1 else W["ada_b"][l:l + 1, :], [], [adab])
            for j in range(12):
                st = stg[n % 2]
                pp = mps[n % 2]
                n += 1
                DMA("sp", st[:], W["ada_w"][l, :, j * 512:(j + 1) * 512].rearrange("(kc p) n -> p kc n", p=128), [], [st])
                for kc in range(8):
                    MM(pp[0:NSEQ, :], condT[:, kc, :], st[:, kc, :], kc == 0, kc == 7, [condT, st], [pp])
                TT("dve", modrow[:, j * 512:(j + 1) * 512], pp[0:NSEQ, :], adab[:, j * 512:(j + 1) * 512], ALU.add, [pp, adab], [modrow])
            DMA("sp", mod_d[l], modrow[:], [modrow], [modb])
        P.pop()

    def load_mod_bc(tl, l, s, j, plus1=False):
        DMA("sp", tl[:], mod_d[l, s:s + 1, j * 1024:(j + 1) * 1024].partition_broadcast(128), [modb], [tl])
        if plus1:
            TS("pool", tl[:], tl[:], 1.0, None, ALU.add, None, [tl], [tl])

    def make_hT(xt, scp, sh, hT_out_ap, hTbuf, trp, work, hTf_ap=None, hTfbuf=None):
        TT("dve", work[:], xt[:], scp[:], ALU.mult, [xt, scp], [work])
        TT("pool", work[:], work[:], sh[:], ALU.add, [work, sh], [work])
        for half in range(2):
            for i in range(4):
                kc = half * 4 + i
                TR(trp[:, i, :], work[:, kc * 128:(kc + 1) * 128], identf[:], [work, identf], [trp])
            if half == 0:
                ACT(hT_out_ap[:, 0:4, :], trp[:], AF.Copy, [trp], [hTbuf])
            else:
                CP("dve", hT_out_ap[:, 4:8, :], trp[:], [trp], [hTbuf])
            if hTf_ap is not None:
                if half == 0:
                    CP("dve", hTf_ap[:, 0:4, :], trp[:], [trp], [hTfbuf])
                else:
                    ACT(hTf_ap[:, 4:8, :], trp[:], AF.Copy, [trp], [hTfbuf])

    def resid_ln(xt, yps_list, gbc, lng, lnb, r, stat, dst_ap, dst_buf):
        for half in range(2):
            yap, ybuf = yps_list[half]
            sl = slice(half * 512, (half + 1) * 512)
            TT("dve", r[:, sl], yap, gbc[:, sl], ALU.mult, [ybuf, gbc], [r])
        STT(r[:], xt[:], ALPHA, r[:], ALU.mult, ALU.add, [xt, r], [r])
        for c4 in range(2):
            P.op("dve", lambda e, c4=c4: e.bn_stats(out=stat[:, c4 * 6:(c4 + 1) * 6], in_=r[:, c4 * 512:(c4 + 1) * 512]), nrm([r]), nrm([stat]))
        P.op("dve", lambda e: e.bn_aggr(out=stat[:, 12:14], in_=stat[:, 0:12]), nrm([stat]), nrm([stat]))
        TS("dve", stat[:, 14:15], stat[:, 13:14], EPS, None, ALU.add, None, [stat], [stat])
        ACT(stat[:, 14:15], stat[:, 14:15], AF.Sqrt, [stat], [stat])
        P.op("dve", lambda e: e.reciprocal(out=stat[:, 15:16], in_=stat[:, 14:15]), nrm([stat]), nrm([stat]))
        STT(stat[:, 16:17], stat[:, 12:13], -1.0, stat[:, 15:16], ALU.mult, ALU.mult, [stat], [stat])
        ACT(r[:], r[:], AF.Identity, [r, stat], [r], scale=stat[:, 15:16], bias=stat[:, 16:17])
        TT("dve", r[:], r[:], lng[:], ALU.mult, [r, lng], [r])
        TT("pool", r[:], r[:], lnb[:], ALU.add, [r, lnb], [r])
        DMA("sp", dst_ap, r[:], [r], [dst_buf])

    def phase_hgrn(l, src, srcb, dst, dstb):
        j = l // 2
        P.push()
        w_in = Tl("hw_in", [128, 8, 4096], BF16)
        w_out = Tl("hw_out", [128, 8, 1024], BF16)
        for kc in range(8):
            DMA("pool", w_in[:, kc, :], W["hgrn_w_in"][j, kc * 128:(kc + 1) * 128, :], [], [w_in], max_dma_last_dim=4096)
        DMA("pool", w_out[:], W["hgrn_w_out"][j].rearrange("(kc p) n -> p kc n", p=128), [], [w_out], max_dma_last_dim=4096)
        lbraw = Tl("lbraw", [128, 2, 8], F32)
        lbc = Tl("lbc", [128, 8], F32)
        oml = Tl("oml", [128, 8], F32)
        DMA("sp", lbraw[:], W["hgrn_lb"].rearrange("j (h p) -> p j h", p=128), [], [lbraw], allow_slow_non_contiguous=True)
        if j == 0:
            TT("dve", lbc[:], lbraw[:, 0, :], lbraw[:, 0, :], ALU.subtract, [lbraw], [lbc])
        else:
            TT("dve", lbc[:], lbraw[:, 1, :], lbraw[:, 0, :], ALU.subtract, [lbraw], [lbc])
            ACT(lbc[:], lbc[:], AF.Sigmoid, [lbc], [lbc])
        TS("dve", oml[:], lbc[:], -1.0, 1.0, ALU.mult, ALU.add, [lbc], [oml])
        normw = Tl("normw", [128, 1024], F32)
        for h in range(8):
            DMA("sp", normw[:, h * 128:(h + 1) * 128], W["hgrn_norm_w"][j:j + 1, :].partition_broadcast(128), [], [normw])
        lng = Tl("lng", [128, 1024], F32)
        lnb = Tl("lnb", [128, 1024], F32)
        DMA("sp", lng[:], W["ln_g"][l, 0:1, :].partition_broadcast(128), [], [lng])
        DMA("sp", lnb[:], W["ln_b"][l, 0:1, :].partition_broadcast(128), [], [lnb])
        scp = Tl("scp", [128, 1024], F32)
        shb = Tl("shb", [128, 1024], F32)
        gbc = Tl("gbc", [128, 1024], F32)
        xts = [Tl("xt%d" % i, [128, 1024], F32) for i in range(2)]
        work = Tl("work", [128, 1024], F32)
        rr = Tl("rr", [128, 1024], F32)
        stat = Tl("stat", [128, 32], F32)
        hT = Tl("hT", [128, 8, 128], BF16)
        trp = Tl("trp", [128, 4, 128], F32, psum=True)
        qz = Tl("qzps", [128, 4, 128], F32, psum=True)
        qzb = [[Buf(), Buf()], [Buf(), Buf()]]
        vg = [Tl("vgps%d" % i, [128, 512], F32, psum=True) for i in range(2)]
        hd = [Tl("hdps%d" % i, [128, 4, 128], F32, psum=True) for i in range(2)]
        hdb = [[Buf() for _ in range(4)] for _ in range(2)]
        ktp = Tl("ktps", [128, 8, 128], BF16, psum=True)
        ktb = [Buf(), Buf()]
        ontp = Tl("ontps", [128, 8, 128], BF16, psum=True)
        vsb = Tl("vsb", [128, 1024], BF16)
        gw = Tl("gw", [128, 1024], F32)
        sig = [Tl("sig%d" % i, [128, 128], F32) for i in range(2)]
        lf = [Tl("lf%d" % i, [128, 128], F32) for i in range(2)]
        kk = [Tl("kk%d" % i, [128, 128], F32) for i in range(2)]
        bb = [Tl("bb%d" % i, [128, 128], F32) for i in range(2)]
        Ep = [Tl("Ep%d" % i, [128, 128], F32) for i in range(2)]
        Em = [Tl("Em%d" % i, [128, 128], F32) for i in range(2)]
        qT = [Tl("qT%d" % i, [128, 128], BF16) for i in range(2)]
        kT = [Tl("kT%d" % i, [128, 128], BF16) for i in range(2)]
        AT = [Tl("AT%d" % i, [128, 128], BF16) for i in range(2)]
        qpad = [Tl("qpad%d" % i, [128, 640], BF16) for i in range(2)]
        kmask = [Tl("kmask%d" % i, [128, 4, 128], BF16) for i in range(2)]
        kmb = [[Buf() for _ in range(4)] for _ in range(2)]
        s1 = [Tl("s1_%d" % i, [128, 128], F32) for i in range(2)]
        ss = Tl("ssq", [128, 16], F32)
        junk = Tl("junk", [128, 128], F32)
        on_all = Tl("on_all", [128, 8, 128], BF16)
        onT = Tl("onT", [128, 8, 128], BF16)
        state = [[Tl("st_%d_%d" % (s, h), [128, 128], F32) for h in range(8)] for s in range(NSEQ)]
        stbf = [[Tl("stb_%d_%d" % (s, h), [128, 128], BF16) for h in range(8)] for s in range(NSEQ)]
        for i in range(2):
            MS("pool", qpad[i][:], 0.0, [qpad[i]])
        for s in range(NSEQ):
            for h in range(8):
                MS("pool", state[s][h][:], 0.0, [state[s][h]])
                MS("pool", stbf[s][h][:], 0.0, [stbf[s][h]])
        it = 0
        for s in range(NSEQ):
            load_mod_bc(scp, l, s, 1, plus1=True)
            load_mod_bc(shb, l, s, 0)
            load_mod_bc(gbc, l, s, 2)
            for t in range(NT):
                g = s * NT + t
                xt = xts[g % 2]
                DMA("sp", xt[:], src[g * 128:(g + 1) * 128, :], [srcb[g]], [xt])
                make_hT(xt, scp, shb, hT, hT, trp, work)
                for cch in range(4):
                    pp = vg[cch % 2]
                    for kc in range(8):
                        MM(pp[:], hT[:, kc, :], w_in[:, kc, 2048 + cch * 512:2048 + (cch + 1) * 512], kc == 0, kc == 7, [hT, w_in], [pp])
                    if cch < 2:
                        CP("dve", vsb[:, cch * 512:(cch + 1) * 512], pp[:], [pp], [vsb])
                    else:
                        ACT(gw[:, (cch - 2) * 512:(cch - 1) * 512], pp[:], AF.Silu, [pp], [gw])
                TT("pool", gw[:], gw[:], normw[:], ALU.mult, [gw, normw], [gw])
                for h in range(8):
                    p2 = it % 2
                    it += 1
                    qps, zps = qz[:, 2 * p2, :], qz[:, 2 * p2 + 1, :]
                    qb_, zb_ = qzb[p2]
                    for kc in range(8):
                        MM(qps, w_in[:, kc, h * 128:(h + 1) * 128], hT[:, kc, :], kc == 0, kc == 7, [hT, w_in], [qb_])
                    for kc in range(8):
                        MM(zps, w_in[:, kc, 1024 + h * 128:1024 + (h + 1) * 128], hT[:, kc, :], kc == 0, kc == 7, [hT, w_in], [zb_])
                    ACT(sig[p2][:], zps, AF.Sigmoid, [zb_], [sig[p2]])
                    TS("dve", sig[p2][:], sig[p2][:], oml[:, h:h + 1], lbc[:, h:h + 1], ALU.mult, ALU.add, [sig[p2], oml, lbc], [sig[p2]])
                    ACT(lf[p2][:], sig[p2][:], AF.Ln, [sig[p2]], [lf[p2]])
                    dbg('f', sig[p2][:], [sig[p2]]); dbg('lf', lf[p2][:], [lf[p2]])
                    TS("pool", kk[p2][:], sig[p2][:], -1.0, 1.0, ALU.mult, ALU.add, [sig[p2]], [kk[p2]])
                    P.op("dve", lambda e, p2=p2: e.tensor_tensor_scan(out=bb[p2][:], data0=scanmask[:], data1=lf[p2][:], initial=0.0,
                                                                     op0=ALU.mult, op1=ALU.add), nrm([scanmask, lf[p2]]), nrm([bb[p2]]))
                    ACT(Ep[p2][:], bb[p2][:], AF.Exp, [bb[p2]], [Ep[p2]])
                    ACT(Em[p2][:], bb[p2][:], AF.Exp, [bb[p2]], [Em[p2]], scale=-1.0)
                    TT("dve", qT[p2][:], qps, Ep[p2][:], ALU.mult, [qb_, Ep[p2]], [qT[p2]])
                    CP("pool", qpad[p2][:].rearrange("p (c x) -> p c x", x=160)[:, :, 0:32],
                       qT[p2][:].rearrange("p (c j) -> p c j", j=32), [qT[p2]], [qpad[p2]])
                    TT("pool", kT[p2][:], kk[p2][:], Em[p2][:], ALU.mult, [kk[p2], Em[p2]], [kT[p2]])
                    dbg('bb', bb[p2][:], [bb[p2]]); dbg('qT', qT[p2][:], [qT[p2]]); dbg('kT', kT[p2][:], [kT[p2]]); dbg('qpad', qpad[p2][:], [qpad[p2]])
                    stp, ops_, up = hd[p2][:, 0, :], vg[p2][:, 0:128], [hd[p2][:, 2, :], hd[p2][:, 3, :]]
                    stb_, ob_, ub_ = hdb[p2][0], vg[p2], [hdb[p2][2], hdb[p2][3]]
                    MM(stp, kT[p2][:], qT[p2][:], True, True, [kT[p2], qT[p2]], [stb_])
                    TR(ktp[:, p2, :], kT[p2][:], identb[:], [kT[p2], identb], [ktb[p2]])
                    TT("dve", AT[p2][:], stp, blockmask[:], ALU.mult, [stb_, blockmask], [AT[p2]])
                    for c in range(4):
                        ACT(kmask[p2][:, c, :], ktp[:, p2, :], AF.Copy, [ktb[p2], chunkind], [kmb[p2][c]], scale=chunkind[:, c:c + 1])
                    vh = vsb[:, h * 128:(h + 1) * 128]
                    MM(ops_, AT[p2][:], vh, True, False, [AT[p2], vsb], [ob_])
                    stt, stb16 = state[s][h], stbf[s][h]
                    for c in range(4):
                        MM(ops_, qpad[p2][:, c * 128:(c + 1) * 128], stb16[:], False, c == 3, [qpad[p2], stb16], [ob_])
                        MM(up[c % 2], kmask[p2][:, c, :], vh, True, True, [kmb[p2][c], vsb], [ub_[c % 2]])
                        ebl = Ep[p2][:, 32 * c + 31:32 * c + 32]
                        TS("pool", s1[p2][:], stt[:], ebl, None, ALU.mult, None, [stt, Ep[p2]], [s1[p2]])
                        STT(stt[:], up[c % 2], ebl, s1[p2][:], ALU.mult, ALU.add, [ub_[c % 2], Ep[p2], s1[p2]], [stt])
                        ACT(stb16[:], stt[:], AF.Copy, [stt], [stb16])
                    dbg('AT', AT[p2][:], [AT[p2]]); dbg('ops', ops_, [ob_], psum=True); dbg('kmask', kmask[p2][:], kmb[p2]); dbg('state', stt[:], [stt])
                    ACT(junk[:], ops_, AF.Square, [ob_], [junk, ss], accum_out=ss[:, h:h + 1])
                    ACT(ss[:, 8 + h:9 + h], ss[:, h:h + 1], AF.Sqrt, [ss], [ss], scale=1.0 / 128.0, bias=EPS)
                    P.op("dve", lambda e, h=h: e.reciprocal(out=ss[:, 8 + h:9 + h], in_=ss[:, 8 + h:9 + h]), nrm([ss]), nrm([ss]))
                    STT(on_all[:, h, :], ops_, ss[:, 8 + h:9 + h], gw[:, h * 128:(h + 1) * 128], ALU.mult, ALU.mult, [ob_, ss, gw], [on_all])
                    TR(ontp[:, h, :], on_all[:, h, :], identb[:], [on_all, identb], [ontp])
                dbg('on_all', on_all[:], [on_all]); dbg('gw', gw[:], [gw]); dbg('vsb', vsb[:], [vsb]); dbg('hT', hT[:], [hT]); dbg('ss', ss[:], [ss])
                CP("dve", onT[:, 0:4, :], ontp[:, 0:4, :], [ontp], [onT])
                ACT(onT[:, 4:8, :], ontp[:, 4:8, :], AF.Copy, [ontp], [onT])
                for half in range(2):
                    for h in range(8):
                        MM(vg[half][:], onT[:, h, :], w_out[:, h, half * 512:(half + 1) * 512], h == 0, h == 7, [onT, w_out], [vg[half]])
                dbg('y0', vg[0][:], [vg[0]], psum=True)
                resid_ln(xt, [(vg[0][:], vg[0]), (vg[1][:], vg[1])], gbc, lng, lnb, rr, stat, dst[g * 128:(g + 1) * 128, :], dstb[g])
        P.pop()

    def phase_moe(l, src, srcb, dst, dstb):
        P.push()
        GT = min(16, NT)
        NG = (NSEQ * NT) // GT
        SGT = min(4, GT)
        wr = Tl("wr", [128, 8, 36], F32)
        DMA("sp", wr[:], W["router_w"][l], [], [wr])
        rbias = Tl("rbias", [128, 36], F32)
        DMA("sp", rbias[:], W["router_b"][l:l + 1, :].partition_broadcast(128), [], [rbias])
        lng = Tl("lng", [128, 1024], F32)
        lnb = Tl("lnb", [128, 1024], F32)
        DMA("sp", lng[:], W["ln_g"][l, 1:2, :].partition_broadcast(128), [], [lng])
        DMA("sp", lnb[:], W["ln_b"][l, 1:2, :].partition_broadcast(128), [], [lnb])
        scp = Tl("scp", [128, 1024], F32)
        shb = Tl("shb", [128, 1024], F32)
        gbc = Tl("gbc", [128, 1024], F32)
        xts = [Tl("xt%d" % i, [128, 1024], F32) for i in range(2)]
        work = Tl("work", [128, 1024], F32)
        rr = Tl("rr", [128, 1024], F32)
        stat = Tl("stat", [128, 32], F32)
        h2T = Tl("h2T", [128, 8, GT * 128], BF16)
        h2Tf = Tl("h2Tf", [128, 8, 128], F32)
        yacc = Tl("yacc", [128, GT, 1024], F32)
        yaccb = [Buf() for _ in range(GT)]
        gates = Tl("gates", [128, GT, 32], F32)
        gT = [Tl("gT%d" % i, [128, 4, 512], BF16) for i in range(2)]
        slt = [Tl("slt%d" % i, [128, 512], F32) for i in range(2)]
        wb = [dict(w1=Tl("w1_%d" % i, [128, 8, 512], BF16), w3=Tl("w3_%d" % i, [128, 8, 512], BF16),
                   w2=Tl("w2_%d" % i, [128, 4, 1024], BF16)) for i in range(2)]
        trp = Tl("trp", [128, 4, 128], F32, psum=True)
        lgp = Tl("lgp", [128, 512], F32, psum=True)
        hp1 = [Tl("hp1_%d" % i, [128, 512], F32, psum=True) for i in range(2)]
        hp3 = [Tl("hp3_%d" % i, [128, 512], F32, psum=True) for i in range(2)]
        yp = [Tl("yp%d" % i, [128, 512], F32, psum=True) for i in range(2)]
        lg = Tl("lg", [128, 36], F32)
        sm = Tl("rsm", [128, 16], F32)
        oh = Tl("oh", [128, 4], F32)
        ejunk = Tl("ejunk", [128, 4], F32)
        m1 = Tl("m1", [128, 4], F32)
        m2 = Tl("m2", [128, 4], F32)
        dd = Tl("dd", [128, 4], F32)
        c1 = Tl("c1", [128, 4], F32)
        c2 = Tl("c2", [128, 4], F32)
        mk1 = Tl("mk1", [128, 4, 8], F32)
        mk2 = Tl("mk2", [128, 4, 8], F32)
        el2 = Tl("el2", [128, 4, 8], F32)

        def bc48(ap):
            return ap.unsqueeze(2).to_broadcast([128, 4, 8])

        for gidx in range(NG):
            g0 = gidx * GT
            s = g0 // NT
            load_mod_bc(scp, l, s, 4, plus1=True)
            load_mod_bc(shb, l, s, 3)
            load_mod_bc(gbc, l, s, 5)
            if MOE_CUT != -3:
                MS("pool", yacc[:], 0.0, yaccb)
            for i in range(GT):
                g = g0 + i
                xt = xts[g % 2]
                DMA("sp", xt[:], src[g * 128:(g + 1) * 128, :], [srcb[g]], [xt])
                if MOE_CUT >= -1:
                    make_hT(xt, scp, shb, h2T[:, :, i * 128:(i + 1) * 128], h2T, trp, work, h2Tf if MOE_CUT >= 0 else None, h2Tf)
                if MOE_CUT <= 0:
                    continue
                for kc in range(8):
                    MM(lgp[:, 0:36], h2Tf[:, kc, :], wr[:, kc, :], kc == 0, kc == 7, [h2Tf, wr], [lgp])
                TT("dve", lg[:], lgp[:, 0:36], rbias[:], ALU.add, [lgp, rbias], [lg])
                if MOE_CUT == 1:
                    continue
                gl = lg[:, 0:4]
                el = lg[:, 4:36].rearrange("p (g j) -> p g j", j=8)
                P.op("dve", lambda e, gl=gl: e.tensor_reduce(out=sm[:, 0:1], in_=gl, axis=AX.X, op=ALU.max), nrm([lg]), nrm([sm]))
                TS("dve", oh[:], gl, sm[:, 0:1], None, ALU.is_equal, None, [lg, sm], [oh])
                TS("dve", sm[:, 1:2], sm[:, 0:1], -1.0, None, ALU.mult, None, [sm], [sm])
                ACT(ejunk[:], gl, AF.Exp, [lg, sm], [ejunk, sm], bias=sm[:, 1:2], accum_out=sm[:, 2:3])
                P.op("dve", lambda e: e.reciprocal(out=sm[:, 3:4], in_=sm[:, 2:3]), nrm([sm]), nrm([sm]))
                P.op("dve", lambda e, el=el: e.tensor_reduce(out=m1[:], in_=el, axis=AX.X, op=ALU.max), nrm([lg]), nrm([m1]))
                TT("dve", mk1[:], el, bc48(m1[:]), ALU.is_equal, [lg, m1], [mk1])
                STT(el2[:], mk1[:], -1.0e30, el, ALU.mult, ALU.add, [mk1, lg], [el2])
                P.op("dve", lambda e: e.tensor_reduce(out=m2[:], in_=el2[:], axis=AX.X, op=ALU.max), nrm([el2]), nrm([m2]))
                TT("dve", mk2[:], el2[:], bc48(m2[:]), ALU.is_equal, [el2, m2], [mk2])
                TT("dve", dd[:], m2[:], m1[:], ALU.subtract, [m1, m2], [dd])
                ACT(dd[:], dd[:], AF.Exp, [dd], [dd])
                TS("dve", c1[:], dd[:], 1.0, None, ALU.add, None, [dd], [c1])
                P.op("dve", lambda e: e.reciprocal(out=c1[:], in_=c1[:]), nrm([c1]), nrm([c1]))
                TT("dve", c2[:], dd[:], c1[:], ALU.mult, [dd, c1], [c2])
                TS("dve", oh[:], oh[:], sm[:, 3:4], None, ALU.mult, None, [oh, sm], [oh])
                TT("dve", c1[:], c1[:], oh[:], ALU.mult, [c1, oh], [c1])
                TT("dve", c2[:], c2[:], oh[:], ALU.mult, [c2, oh], [c2])
                TT("dve", mk1[:], mk1[:], bc48(c1[:]), ALU.mult, [mk1, c1], [mk1])
                TT("dve", mk2[:], mk2[:], bc48(c2[:]), ALU.mult, [mk2, c2], [mk2])
                TT("dve", gates[:, i, :].rearrange("p (g j) -> p g j", j=8), mk1[:], mk2[:], ALU.add, [mk1, mk2], [gates])
            if DEBUG:
                dbg("gates", gates[:], [gates])
            nsub = 0
            for e in range(NE if MOE_STAGE >= 2 else 0):
                wbe = wb[e % 2]
                DMA("pool", wbe["w1"][:], W["moe_w1"][l, e].rearrange("(kc p) n -> p kc n", p=128), [], [wbe["w1"]])
                DMA("pool", wbe["w3"][:], W["moe_w3"][l, e].rearrange("(kc p) n -> p kc n", p=128), [], [wbe["w3"]])
                DMA("pool", wbe["w2"][:], W["moe_w2"][l, e].rearrange("(kc p) n -> p kc n", p=128), [], [wbe["w2"]], max_dma_last_dim=4096)
                for sg in range(GT // SGT):
                    ncol = SGT * 128
                    c0 = sg * ncol
                    gt_ = gT[nsub % 2]
                    nsub += 1
                    for fc in range(4):
                        a1, a3 = hp1[fc % 2], hp3[fc % 2]
                        for kc in range(8):
                            MM(a1[:, 0:ncol], wbe["w1"][:, kc, fc * 128:(fc + 1) * 128], h2T[:, kc, c0:c0 + ncol], kc == 0, kc == 7, [wbe["w1"], h2T], [a1])
                        for kc in range(8):
                            MM(a3[:, 0:ncol], wbe["w3"][:, kc, fc * 128:(fc + 1) * 128], h2T[:, kc, c0:c0 + ncol], kc == 0, kc == 7, [wbe["w3"], h2T], [a3])
                        sl = slt[fc % 2]
                        ACT(sl[:, 0:ncol], a1[:, 0:ncol], AF.Silu, [a1], [sl])
                        TT("dve", gt_[:, fc, 0:ncol], sl[:, 0:ncol], a3[:, 0:ncol], ALU.mult, [sl, a3], [gt_])
                    for ti in range(SGT):
                        i = sg * SGT + ti
                        for half in range(2):
                            ypp = yp[half]
                            for fc in range(4):
                                MM(ypp[:], gt_[:, fc, ti * 128:(ti + 1) * 128], wbe["w2"][:, fc, half * 512:(half + 1) * 512], fc == 0, fc == 3, [gt_, wbe["w2"]], [ypp])
                            ya = yacc[:, i, half * 512:(half + 1) * 512]
                            STT(ya, ypp[:], gates[:, i, e:e + 1], ya, ALU.mult, ALU.add, [ypp, gates, yaccb[i]], [yaccb[i]])
            for i in range(GT):
                g = g0 + i
                xt = xts[g % 2]
                DMA("sp", xt[:], src[g * 128:(g + 1) * 128, :], [srcb[g]], [xt])
                resid_ln(xt, [(yacc[:, i, 0:512], yaccb[i]), (yacc[:, i, 512:1024], yaccb[i])], gbc, lng, lnb, rr, stat,
                         dst[g * 128:(g + 1) * 128, :], dstb[g])
        P.pop()

    def phase_attn(l, src, srcb, dst, dstb):
        import math
        j = l // 2
        lam_init = 0.8 - 0.6 * math.exp(-0.3 * l)
        QB = min(512, S)
        NQB = QB // 128
        NSB = S // QB
        P.push()
        banks = [Tl("bk%d" % i, [128, 512], F32, psum=True) for i in range(8)]

        class TrV:
            def __init__(self, bank):
                self.b = bank.b
                self.v = bank[:].rearrange("p (i t) -> p i t", t=128)

            def __getitem__(self, k):
                return self.v[k]
        trp_t = banks[0]
        w_out = Tl("aw_out", [128, 8, 1024], BF16)
        DMA("pool", w_out[:], W["attn_w_out"][j].rearrange("(kc p) n -> p kc n", p=128), [], [w_out], max_dma_last_dim=4096)
        lng = Tl("lng", [128, 1024], F32)
        lnb = Tl("lnb", [128, 1024], F32)
        DMA("sp", lng[:], W["ln_g"][l, 0:1, :].partition_broadcast(128), [], [lng])
        DMA("sp", lnb[:], W["ln_b"][l, 0:1, :].partition_broadcast(128), [], [lnb])
        subw = Tl("subw", [128, 128], F32)
        DMA("sp", subw[:], W["attn_subln_w"][j:j + 1, :].partition_broadcast(128), [], [subw])
        TS("dve", subw[:], subw[:], 1.0 - lam_init, None, ALU.mult, None, [subw], [subw])
        lamt = Tl("lamt", [128, 256], F32)
        lams = Tl("lams", [128, 8], F32)
        DMA("sp", lamt[:], W["attn_lambda"][j:j + 1].rearrange("o a d -> o (a d)").partition_broadcast(128), [], [lamt])
        TT("dve", lamt[:, 0:64], lamt[:, 0:64], lamt[:, 64:128], ALU.mult, [lamt], [lamt])
        TT("dve", lamt[:, 128:192], lamt[:, 128:192], lamt[:, 192:256], ALU.mult, [lamt], [lamt])
        P.op("dve", lambda e: e.tensor_reduce(out=lams[:, 0:1], in_=lamt[:, 0:64], axis=AX.X, op=ALU.add), nrm([lamt]), nrm([lams]))
        P.op("dve", lambda e: e.tensor_reduce(out=lams[:, 1:2], in_=lamt[:, 128:192], axis=AX.X, op=ALU.add), nrm([lamt]), nrm([lams]))
        ACT(lams[:, 0:2], lams[:, 0:2], AF.Exp, [lams], [lams])
        TT("dve", lams[:, 2:3], lams[:, 0:1], lams[:, 1:2], ALU.subtract, [lams], [lams])
        TS("dve", lams[:, 2:3], lams[:, 2:3], lam_init, None, ALU.add, None, [lams], [lams])
        lam = lams[:, 2:3]
        cosT = Tl("cosT", [128, S], F32)
        sinT = Tl("sinT", [128, S], F32)
        DMA("sp", cosT[:], CD["ropecos"], [], [cosT])
        DMA("sp", sinT[:], CD["ropesin"], [], [sinT])
        scp = Tl("scp", [128, 1024], F32)
        shb = Tl("shb", [128, 1024], F32)
        gbc = Tl("gbc", [128, 1024], F32)
        xts = [Tl("xt%d" % i, [128, 1024], F32) for i in range(2)]
        work = Tl("work", [128, 1024], F32)
        rr = Tl("rr", [128, 1024], F32)
        stat = Tl("stat", [128, 32], F32)
        hTall = Tl("hTall", [128, 8, S], BF16)
        qT = Tl("aqT", [128, S], BF16)
        kT = Tl("akT", [128, S], BF16)
        vext = Tl("vext", [128, NT, 129], BF16)
        MS("pool", vext[:, :, 128:129], 1.0, [vext])
        wsl = {nm: Tl("aw_" + nm, [128, 8, 128], BF16) for nm in ("q", "qs", "k", "ks", "v")}
        t1 = Tl("rt1", [128, 512], F32)
        t2 = Tl("rt2", [128, 512], F32)
        pT = [[Tl("pT%d_%d" % (i, m), [128, 512], BF16) for m in range(2)] for i in range(2)]
        rs = Tl("ars", [128, 8], F32)
        ot = Tl("aot", [128, 128], F32)
        ot2 = Tl("aot2", [128, 128], F32)
        o1s = Tl("ao1s", [128, 4, 128], F32)
        junk = Tl("ajunk", [128, 128], F32)
        onb16 = [Tl("aon%d" % i, [128, 128], BF16) for i in range(2)]
        ont = Tl("aont", [128, 1024], BF16)
        onT = Tl("aonT", [128, 8, 128], BF16)
        win = W["attn_w_in"][j]
        nst = 0
        for s in range(NSEQ):
            load_mod_bc(scp, l, s, 1, plus1=True)
            load_mod_bc(shb, l, s, 0)
            load_mod_bc(gbc, l, s, 2)
            for t in range(NT):
                g = s * NT + t
                xt = xts[g % 2]
                DMA("sp", xt[:], src[g * 128:(g + 1) * 128, :], [srcb[g]], [xt])
                make_hT(xt, scp, shb, hTall[:, :, t * 128:(t + 1) * 128], hTall, TrV(banks[0]), work)
            for h in range(8):
                def wv3(c0, n):
                    return win[:, c0:c0 + n].rearrange("(kc p) n -> p kc n", p=128)
                DMA("pool", wsl["q"][:], wv3(h * 128, 128), [], [wsl["q"]])
                DMA("pool", wsl["k"][:], wv3(1024 + h * 128, 128), [], [wsl["k"]])
                DMA("pool", wsl["v"][:], wv3(2048 + h * 128, 128), [], [wsl["v"]])
                for nm, base in (("qs", 0), ("ks", 1024)):
                    for m in range(2):
                        b0 = base + h * 128 + m * 64
                        DMA("pool", wsl[nm][:, :, m * 64:m * 64 + 32], wv3(b0 + 32, 32), [], [wsl[nm]])
                        DMA("pool", wsl[nm][:, :, m * 64 + 32:m * 64 + 64], wv3(b0, 32), [], [wsl[nm]])
                for nb in range(NSB):
                    cs = slice(nb * QB, (nb + 1) * QB)
                    for (wn, wsn, dstT, bi) in (("q", "qs", qT, 2), ("k", "ks", kT, 2)):
                        pa, pb = banks[bi], banks[bi + 1]
                        for kc in range(8):
                            MM(pa[:, 0:QB], wsl[wn][:, kc, :], hTall[:, kc, cs], kc == 0, kc == 7, [wsl[wn], hTall], [pa])
                        for kc in range(8):
                            MM(pb[:, 0:QB], wsl[wsn][:, kc, :], hTall[:, kc, cs], kc == 0, kc == 7, [wsl[wsn], hTall], [pb])
                        TT("dve", t1[:, 0:QB], pa[:, 0:QB], cosT[:, cs], ALU.mult, [pa, cosT], [t1])
                        TT("dve", t2[:, 0:QB], pb[:, 0:QB], sinT[:, cs], ALU.mult, [pb, sinT], [t2])
                        TT("pool", dstT[:, cs], t1[:, 0:QB], t2[:, 0:QB], ALU.add, [t1, t2], [dstT])
                for t in range(NT):
                    pv = banks[4 + (t % 2)]
                    for kc in range(8):
                        MM(pv[:, 0:128], hTall[:, kc, t * 128:(t + 1) * 128], wsl["v"][:, kc, :], kc == 0, kc == 7, [hTall, wsl["v"]], [pv])
                    CP("dve", vext[:, t, 0:128], pv[:, 0:128], [pv], [vext])
                for Q in range(NSB):
                    nkb = Q * NQB + NQB
                    for m in range(2):
                        for kb in range(nkb):
                            j0 = max(0, kb - Q * NQB)
                            csl = slice(j0 * 128, QB)
                            stb = banks[nst % 2]
                            pt = pT[nst % 2][0]
                            nst += 1
                            MM(stb[:, csl], kT[m * 64:(m + 1) * 64, kb * 128:(kb + 1) * 128], qT[m * 64:(m + 1) * 64, Q * QB + j0 * 128:(Q + 1) * QB],
                               True, True, [kT, qT], [stb])
                            ACT(pt[:, csl], stb[:, csl], AF.Exp, [stb], [pt], scale=0.125)
                            if kb >= Q * NQB:
                                dsl = slice(j0 * 128, (j0 + 1) * 128)
                                TT("pool", pt[:, dsl], pt[:, dsl], trimask[:], ALU.mult, [pt, trimask], [pt])
                            for jq in range(j0, NQB):
                                ab = banks[4 + jq]
                                MM(ab[:, 0:129], pt[:, jq * 128:(jq + 1) * 128], vext[:, kb, :], kb == 0, kb == Q * NQB + jq, [pt, vext], [ab])
                        for jq in range(NQB):
                            ab = banks[4 + jq]
                            if m == 0:
                                P.op("dve", lambda e, ab=ab: e.reciprocal(out=rs[:, 0:1], in_=ab[:, 128:129]), nrm([ab]), nrm([rs]))
                                TS("dve", o1s[:, jq, :], ab[:, 0:128], rs[:, 0:1], None, ALU.mult, None, [ab, rs], [o1s])
                            else:
                                t = Q * NQB + jq
                                g = s * NT + t
                                P.op("dve", lambda e, ab=ab: e.reciprocal(out=rs[:, 1:2], in_=ab[:, 128:129]), nrm([ab]), nrm([rs]))
                                TS("dve", rs[:, 1:2], rs[:, 1:2], lam, None, ALU.mult, None, [rs, lams], [rs])
                                TS("dve", ot2[:], ab[:, 0:128], rs[:, 1:2], None, ALU.mult, None, [ab, rs], [ot2])
                                TT("dve", ot[:], o1s[:, jq, :], ot2[:], ALU.subtract, [o1s, ot2], [ot])
                                ACT(junk[:], ot[:], AF.Square, [ot], [junk, rs], accum_out=rs[:, 2:3])
                                ACT(rs[:, 3:4], rs[:, 2:3], AF.Sqrt, [rs], [rs], scale=1.0 / 128.0, bias=EPS)
                                P.op("dve", lambda e: e.reciprocal(out=rs[:, 3:4], in_=rs[:, 3:4]), nrm([rs]), nrm([rs]))
                                ob = onb16[jq % 2]
                                STT(ob[:], ot[:], rs[:, 3:4], subw[:], ALU.mult, ALU.mult, [ot, rs, subw], [ob])
                                DMA("sp", on_d[g * 128:(g + 1) * 128, h * 128:(h + 1) * 128], ob[:], [ob], [onb[g]])
            for t in range(NT):
                g = s * NT + t
                xt = xts[g % 2]
                DMA("sp", xt[:], src[g * 128:(g + 1) * 128, :], [srcb[g]], [xt])
                DMA("sp", ont[:], on_d[g * 128:(g + 1) * 128, :], [onb[g]], [ont])
                tb = banks[1]
                tbv = tb[:].bitcast(BF16).rearrange("p (h t) -> p h t", t=128)
                for h in range(8):
                    TR(tbv[:, h, :], ont[:, h * 128:(h + 1) * 128], identb[:], [ont, identb], [tb])
                CP("dve", onT[:, 0:4, :], tbv[:, 0:4, :], [tb], [onT])
                ACT(onT[:, 4:8, :], tbv[:, 4:8, :], AF.Copy, [tb], [onT])
                for half in range(2):
                    yb = banks[2 + half]
                    for h in range(8):
                        MM(yb[:], onT[:, h, :], w_out[:, h, half * 512:(half + 1) * 512], h == 0, h == 7, [onT, w_out], [yb])
                resid_ln(xt, [(banks[2][:], banks[2]), (banks[3][:], banks[3])], gbc, lng, lnb, rr, stat, dst[g * 128:(g + 1) * 128, :], dstb[g])
        P.pop()

    phase_mod()
    P.barrier()
    cur, curb = x_d, xb["x"]
    for li, l in enumerate(layers):
        last = (li == len(layers) - 1)
        if "mix" in sub:
            dst, dstb = (out_d, xb["out"]) if (last and "moe" not in sub) else (xs[0], xb[0])
            if l % 2 == 0:
                phase_hgrn(l, cur, curb, dst, dstb)
            else:
                phase_attn(l, cur, curb, dst, dstb)
            cur, curb = dst, dstb
        if "moe" in sub:
            dst, dstb = (out_d, xb["out"]) if last else (xs[1], xb[1])
            phase_moe(l, cur, curb, dst, dstb)
            cur, curb = dst, dstb
    P.finish()
    P.DBG = DBG
    return nc, consts, P


_CACHE = {}


def run(inputs, NSEQ, S, layers, sub=("mix", "moe"), n_cores=8, NE=32):
    from concourse.bass_utils import run_bass_kernel_spmd
    nc, consts, P = build(NSEQ, S, layers, sub, NE)
    x = np.ascontiguousarray(inputs["x"], dtype=np.float32).reshape(n_cores, NSEQ * S, D)
    c = np.ascontiguousarray(inputs["c"], dtype=np.float32).reshape(n_cores, NSEQ, D)
    inputs = dict(inputs)
    rw = np.concatenate([np.asarray(inputs["router_g_w"], np.float32), np.asarray(inputs["router_e_w"], np.float32)], axis=2)
    inputs["router_w"] = np.ascontiguousarray(rw.reshape(rw.shape[0], 8, 128, 36).transpose(0, 2, 1, 3))
    inputs["router_b"] = np.concatenate([np.asarray(inputs["router_g_b"], np.float32), np.asarray(inputs["router_e_b"], np.float32)], axis=1)
    in_maps = []
    for i in range(n_cores):
        m = {"x": x[i], "cT": np.ascontiguousarray(c[i].reshape(NSEQ, 8, 128).transpose(2, 1, 0))}
        for nm, shp in wnames():
            m[nm] = np.ascontiguousarray(np.asarray(inputs[nm])[:shp[0]], dtype=np.float32)
        for nm, arr in consts.items():
            m[nm] = arr
        in_maps.append(m)
    res = run_bass_kernel_spmd(nc, in_maps, core_ids=list(range(n_cores)))
    out = np.stack([np.asarray(r["out"]) for r in res.results], 0)
    global LAST_DBG
    LAST_DBG = {k: np.asarray(res.results[0]["dbg_" + k]) for k in P.DBG}
    return out.reshape(n_cores * NSEQ, S, D)


def kernel(**inputs):
    out = run(inputs, 2, 4096, [0, 1, 2, 3])
    return out.astype(np.float32)
```

```python
import contextlib
import numpy as np
import concourse.bass as bass
import concourse.mybir as mybir

F32 = mybir.dt.float32
BF16 = mybir.dt.bfloat16
I32 = mybir.dt.int32
U32 = mybir.dt.uint32
AF = mybir.ActivationFunctionType
ALU = mybir.AluOpType
AX = mybir.AxisListType

QUEUES = ("pe", "act", "dve", "pool", "sp")
N_DMA_CH = 12


class Buf:
    __slots__ = ("name", "w", "r", "excl")

    def __init__(self, name="", excl=False):
        self.name = name
        self.excl = excl
        self.w = None
        self.r = []


class Prog:
    def __init__(self, nc, same_engine_sync=True):
        self.nc = nc
        self.stack = contextlib.ExitStack()
        self.ops = {q: [] for q in QUEUES}
        self.cnt = {q: 0 for q in QUEUES}
        self.sems = {}
        self.seen = {q: {} for q in QUEUES}
        self.same_engine_sync = same_engine_sync
        for q in QUEUES:
            self.sems["e_" + q] = self.stack.enter_context(nc.semaphore("e_" + q))
        self.dma_ch = {}
        self.dma_rr = {}
        for q in ("sp", "act", "pool"):
            chs = []
            for i in range(N_DMA_CH):
                key = "d_%s_%d" % (q, i)
                self.sems[key] = self.stack.enter_context(nc.semaphore(key))
                chs.append([key, 0])
            self.dma_ch[q] = chs
            self.dma_rr[q] = 0
        self.n_inst = 0
        self.uid = 0
        self.scopes = [self.stack]

    def sb(self, name, shape, dtype):
        self.uid += 1
        return self.scopes[-1].enter_context(self.nc.sbuf_tensor("sb%d_%s" % (self.uid, name), list(shape), dtype))

    def ps(self, name, shape, dtype=F32):
        self.uid += 1
        return self.scopes[-1].enter_context(self.nc.psum_tensor("ps%d_%s" % (self.uid, name), list(shape), dtype))

    def push(self):
        self.scopes.append(contextlib.ExitStack())

    def pop(self):
        self.barrier()
        self.scopes.pop().close()

    def barrier(self):
        targets = []
        for q in QUEUES:
            if self.cnt[q] > 0:
                targets.append(("e_" + q, self.cnt[q], q))
        for q, chs in self.dma_ch.items():
            for key, val in chs:
                if val > 0:
                    targets.append((key, val, None))
        sems = self.sems
        for q in QUEUES:
            seen = self.seen[q]
            waits = []
            for key, val, wq in targets:
                if wq == q and q != "sp":
                    continue
                if seen.get(key, 0) >= val:
                    continue
                seen[key] = val
                waits.append((key, val))

            def emit(eng, waits=waits):
                for k, v in waits:
                    eng.wait_ge(sems[k], v)

            self.ops[q].append(emit)
            self.n_inst += len(waits)

    def _collect(self, q, reads, writes, is_dma):
        waits = {}

        def need(tok):
            key, val, wq, wdma = tok
            if (not wdma) and (not is_dma) and wq == q:
                if q == "pe" or not self.same_engine_sync:
                    return
            if waits.get(key, 0) < val:
                waits[key] = val

        for b in reads:
            if b.w is not None:
                need(b.w)
        for b in writes:
            if b.w is not None:
                need(b.w)
            for t in b.r:
                need(t)
        out = []
        seen = self.seen[q]
        for key, val in waits.items():
            if seen.get(key, 0) >= val:
                continue
            seen[key] = val
            out.append((key, val))
        return out

    def _commit(self, tok, reads, writes):
        for b in reads:
            b.r.append(tok)
        for b in writes:
            b.w = tok
            b.r = []

    def op(self, q, fn, reads=(), writes=()):
        ex = [b for b in reads if b.excl]
        if ex:
            writes = list(writes) + [b for b in ex if b not in writes]
        waits = self._collect(q, reads, writes, False)
        self.cnt[q] += 1
        key = "e_" + q
        val = self.cnt[q]
        tok = (key, val, q, False)
        sems = self.sems

        def emit(eng, waits=waits, fn=fn, key=key):
            for k, v in waits:
                eng.wait_ge(sems[k], v)
            fn(eng).then_inc(sems[key], 1)

        self.ops[q].append(emit)
        self.n_inst += 1 + len(waits)
        self._commit(tok, reads, writes)
        return tok

    def dma(self, q, fn, reads=(), writes=()):
        waits = self._collect(q, reads, writes, True)
        chs = self.dma_ch[q]
        i = self.dma_rr[q]
        self.dma_rr[q] = (i + 1) % len(chs)
        ch = chs[i]
        key = ch[0]
        prev = ch[1]
        ch[1] += 16
        val = ch[1]
        seen = self.seen[q]
        if prev > 0 and seen.get(key, 0) < prev:
            seen[key] = prev
            waits = waits + [(key, prev)]
        tok = (key, val, q, True)
        sems = self.sems

        def emit(eng, waits=waits, fn=fn, key=key):
            for k, v in waits:
                eng.wait_ge(sems[k], v)
            fn(eng).then_inc(sems[key], 16)

        self.ops[q].append(emit)
        self.n_inst += 1 + len(waits)
        self._commit(tok, reads, writes)
        return tok

    def finish(self):
        nc = self.nc
        final = []
        for q, chs in self.dma_ch.items():
            for key, val in chs:
                if val > 0:
                    final.append((key, val))
        for q in QUEUES:
            if q != "sp" and self.cnt[q] > 0:
                final.append(("e_" + q, self.cnt[q]))
        sems = self.sems
        ops = self.ops
        with nc.Block() as block:
            @block.tensor
            def _(eng):
                for f in ops["pe"]:
                    f(eng)

            @block.scalar
            def _(eng):
                for f in ops["act"]:
                    f(eng)

            @block.vector
            def _(eng):
                for f in ops["dve"]:
                    f(eng)

            @block.gpsimd
            def _(eng):
                for f in ops["pool"]:
                    f(eng)

            @block.sync
            def _(eng):
                for f in ops["sp"]:
                    f(eng)
                for k, v in final:
                    eng.wait_ge(sems[k], v)
        self.stack.close()

DEBUG = False
MOE_STAGE = 2
MOE_CUT = 5
D = 1024
DEPTH = 4
ALPHA = (2 * DEPTH) ** 0.25
EPS = 1e-5
WDEPTH = 4


def wnames():
    L = WDEPTH
    return [("ada_w", [L, 1024, 6144]), ("ada_b", [L, 6144]), ("ln_g", [L, 2, 1024]), ("ln_b", [L, 2, 1024]),
            ("hgrn_w_in", [2, 1024, 4096]), ("hgrn_w_out", [2, 1024, 1024]), ("hgrn_lb", [2, 1024]),
            ("hgrn_norm_w", [2, 128]), ("attn_w_in", [2, 1024, 3072]), ("attn_w_out", [2, 1024, 1024]),
            ("attn_lambda", [2, 4, 64]), ("attn_subln_w", [2, 128]), ("router_w", [L, 128, 8, 36]), ("router_b", [L, 36]),
            ("moe_w1", [L, 32, 1024, 512]), ("moe_w3", [L, 32, 1024, 512]), ("moe_w2", [L, 32, 512, 1024])]


def make_consts(S):
    c = {}
    c["identf"] = np.eye(128, dtype=np.float32)
    s = np.arange(128)[:, None]
    t = np.arange(128)[None, :]
    c["blockmask"] = ((s // 32 == t // 32) & (s <= t)).astype(np.float32)
    c["trimask"] = (s <= t).astype(np.float32)
    sm = np.ones((128, 128), np.float32)
    sm[:, ::32] = 0.0
    c["scanmask"] = sm
    ci = np.zeros((128, 4), np.float32)
    for k in range(4):
        ci[k * 32:(k + 1) * 32, k] = 1.0
    c["chunkind"] = ci
    half = 32
    inv_freq = (np.float32(10000.0) ** (-np.arange(half, dtype=np.float32) / np.float32(half))).astype(np.float32)
    ang = (np.arange(S, dtype=np.float32)[:, None] * inv_freq[None, :]).astype(np.float32)
    cos = np.cos(ang).astype(np.float32).T
    sin = np.sin(ang).astype(np.float32).T
    c["utri"] = (s < t).astype(np.float32)
    wi = np.zeros((128, 12), np.float32)
    for kc in range(8):
        wi[:, kc] = kc * 128 + np.arange(128)
    for fc in range(4):
        wi[:, 8 + fc] = fc * 128 + np.arange(128)
    c["widx"] = wi
    c["bstart"] = (np.arange(128, dtype=np.float32) * 512.0)[None, :]
    c["ropecos"] = np.ascontiguousarray(np.concatenate([cos, cos, cos, cos], 0))
    c["ropesin"] = np.ascontiguousarray(np.concatenate([-sin, sin, -sin, sin], 0))
    return c


def build(NSEQ, S, layers, sub=("mix", "moe"), NE=32):
    NT = S // 128
    NTOK = NSEQ * S
    nc = bass.Bass("TRN2", target_bir_lowering=False)
    dtn = nc.dram_tensor
    x_d = dtn("x", [NTOK, D], F32, kind="ExternalInput").ap()
    c_d = dtn("cT", [128, 8, NSEQ], F32, kind="ExternalInput").ap()
    W = {}
    for nm, shp in wnames():
        W[nm] = dtn(nm, shp, F32, kind="ExternalInput").ap()
    consts = make_consts(S)
    CD = {}
    for nm, arr in consts.items():
        CD[nm] = dtn(nm, list(arr.shape), F32, kind="ExternalInput").ap()
    out_d = dtn("out", [NTOK, D], F32, kind="ExternalOutput").ap()
    xs = [dtn("xs0", [NTOK, D], F32, kind="Internal").ap(), dtn("xs1", [NTOK, D], F32, kind="Internal").ap()]
    mod_d = dtn("mod_d", [WDEPTH, NSEQ, 6144], F32, kind="Internal").ap()
    on_d = dtn("on_d", [NTOK, D], BF16, kind="Internal").ap()
    MOE_NBLK = (2 * NTOK) // 512 + 32
    moe_h2_d = dtn("moe_h2", [NTOK, D], BF16, kind="Internal").ap()
    moe_xbuf_d = dtn("moe_xbuf", [MOE_NBLK * 512, D], BF16, kind="Internal").ap()
    moe_ybuf_d = dtn("moe_ybuf", [MOE_NBLK * 512, D], BF16, kind="Internal").ap()

    P = Prog(nc)
    DBG = {}

    def dbg(name, ap, bufs, psum=False):
        if not DEBUG or name in DBG:
            return
        shp = list(ap.shape)
        d = dtn("dbg_" + name, shp, F32, kind="ExternalOutput").ap()
        DBG[name] = d
        if psum:
            tmp = P.sb("dbgtmp_" + name, shp, F32)
            tb = Buf()
            P.op("dve", lambda e: e.tensor_copy(out=tmp[:], in_=ap), nrm(bufs), [tb])
            P.dma("pool", lambda e: e.dma_start(out=d, in_=tmp[:]), [tb], [])
        else:
            P.dma("pool", lambda e: e.dma_start(out=d, in_=ap), nrm(bufs), [])

    def nrm(l):
        return [b.b if hasattr(b, "b") else b for b in l]

    def MM(out, lhsT, rhs, start, stop, r, w):
        P.op("pe", lambda e: e.matmul(out, lhsT=lhsT, rhs=rhs, start=start, stop=stop), nrm(r), nrm(w))

    def TR(out, in_, ident, r, w):
        P.op("pe", lambda e: e.transpose(out=out, in_=in_, identity=ident), nrm(r), nrm(w))

    def ACT(out, in_, func, r, w, **kw):
        P.op("act", lambda e: e.activation(out=out, in_=in_, func=func, **kw), nrm(r), nrm(w))

    def TT(q, out, in0, in1, op, r, w):
        P.op(q, lambda e: e.tensor_tensor(out=out, in0=in0, in1=in1, op=op), nrm(r), nrm(w))

    def TS(q, out, in0, s1, s2, op0, op1, r, w):
        if op1 is None:
            P.op(q, lambda e: e.tensor_scalar(out=out, in0=in0, scalar1=s1, scalar2=None, op0=op0), nrm(r), nrm(w))
        else:
            P.op(q, lambda e: e.tensor_scalar(out=out, in0=in0, scalar1=s1, scalar2=s2, op0=op0, op1=op1), nrm(r), nrm(w))

    def STT(out, in0, scalar, in1, op0, op1, r, w):
        P.op("dve", lambda e: e.scalar_tensor_tensor(out=out, in0=in0, scalar=scalar, in1=in1, op0=op0, op1=op1), nrm(r), nrm(w))

    def CP(q, out, in_, r, w):
        P.op(q, lambda e: e.tensor_copy(out=out, in_=in_), nrm(r), nrm(w))

    def MS(q, ap, val, w):
        P.op(q, lambda e: e.memset(ap, val), [], nrm(w))

    def DMA(q, out, in_, r, w, **kw):
        P.dma(q, lambda e: e.dma_start(out=out, in_=in_, **kw), nrm(r), nrm(w))

    class Tl:
        def __init__(self, name, shape, dtype, psum=False):
            self.t = P.ps(name, shape, dtype) if psum else P.sb(name, shape, dtype)
            self.b = Buf(name, excl=psum)

        def __getitem__(self, k):
            return self.t[k]

    xb = {"x": [Buf() for _ in range(NSEQ * NT)], 0: [Buf() for _ in range(NSEQ * NT)],
          1: [Buf() for _ in range(NSEQ * NT)], "out": [Buf() for _ in range(NSEQ * NT)]}
    modb = Buf("mod_d")
    onb = [Buf() for _ in range(NSEQ * NT)]

    identf = Tl("identf", [128, 128], F32)
    identb = Tl("identb", [128, 128], BF16)
    blockmask = Tl("blockmask", [128, 128], F32)
    trimask = Tl("trimask", [128, 128], BF16)
    scanmask = Tl("scanmask", [128, 128], F32)
    chunkind = Tl("chunkind", [128, 4], F32)
    DMA("sp", identf[:], CD["identf"], [], [identf])
    DMA("pool", identb[:], CD["identf"], [], [identb])
    DMA("sp", blockmask[:], CD["blockmask"], [], [blockmask])
    DMA("pool", trimask[:], CD["trimask"], [], [trimask])
    DMA("sp", scanmask[:], CD["scanmask"], [], [scanmask])
    DMA("sp", chunkind[:], CD["chunkind"], [], [chunkind])

    def phase_mod():
        P.push()
        condT = Tl("condT", [128, 8, NSEQ], F32)
        DMA("sp", condT[:], c_d, [], [condT])
        ACT(condT[:], condT[:], AF.Silu, [condT], [condT])
        stg = [Tl("adastg%d" % i, [128, 8, 512], F32) for i in range(2)]
        adab = Tl("adab", [NSEQ, 6144], F32)
        modrow = Tl("modrow", [NSEQ, 6144], F32)
        mps = [Tl("modps%d" % i, [128, 512], F32, psum=True) for i in range(2)]
        n = 0
        for l in layers:
            DMA("sp", adab[:], W["ada_b"][l:l + 1, :].partition_broadcast(NSEQ) if NSEQ > 1 else W["ada_b"][l:l + 1, :], [], [adab])
            for j in range(12):
                st = stg[n % 2]
                pp = mps[n % 2]
                n += 1
                DMA("sp", st[:], W["ada_w"][l, :, j * 512:(j + 1) * 512].rearrange("(kc p) n -> p kc n", p=128), [], [st])
                for kc in range(8):
                    MM(pp[0:NSEQ, :], condT[:, kc, :], st[:, kc, :], kc == 0, kc == 7, [condT, st], [pp])
                TT("dve", modrow[:, j * 512:(j + 1) * 512], pp[0:NSEQ, :], adab[:, j * 512:(j + 1) * 512], ALU.add, [pp, adab], [modrow])
            DMA("sp", mod_d[l], modrow[:], [modrow], [modb])
        P.pop()

    def load_mod_bc(tl, l, s, j, plus1=False):
        DMA("sp", tl[:], mod_d[l, s:s + 1, j * 1024:(j + 1) * 1024].partition_broadcast(128), [modb], [tl])
        if plus1:
            TS("pool", tl[:], tl[:], 1.0, None, ALU.add, None, [tl], [tl])

    def make_hT(xt, scp, sh, hT_out_ap, hTbuf, trp, work, hTf_ap=None, hTfbuf=None):
        TT("dve", work[:], xt[:], scp[:], ALU.mult, [xt, scp], [work])
        TT("pool", work[:], work[:], sh[:], ALU.add, [work, sh], [work])
        for half in range(2):
            for i in range(4):
                kc = half * 4 + i
                TR(trp[:, i, :], work[:, kc * 128:(kc + 1) * 128], identf[:], [work, identf], [trp])
            if half == 0:
                ACT(hT_out_ap[:, 0:4, :], trp[:], AF.Copy, [trp], [hTbuf])
            else:
                CP("dve", hT_out_ap[:, 4:8, :], trp[:], [trp], [hTbuf])
            if hTf_ap is not None:
                if half == 0:
                    CP("dve", hTf_ap[:, 0:4, :], trp[:], [trp], [hTfbuf])
                else:
                    ACT(hTf_ap[:, 4:8, :], trp[:], AF.Copy, [trp], [hTfbuf])

    def resid_ln(xt, yps_list, gbc, lng, lnb, r, stat, dst_ap, dst_buf):
        for half in range(2):
            yap, ybuf = yps_list[half]
            sl = slice(half * 512, (half + 1) * 512)
            TT("dve", r[:, sl], yap, gbc[:, sl], ALU.mult, [ybuf, gbc], [r])
        STT(r[:], xt[:], ALPHA, r[:], ALU.mult, ALU.add, [xt, r], [r])
        for c4 in range(2):
            P.op("dve", lambda e, c4=c4: e.bn_stats(out=stat[:, c4 * 6:(c4 + 1) * 6], in_=r[:, c4 * 512:(c4 + 1) * 512]), nrm([r]), nrm([stat]))
        P.op("dve", lambda e: e.bn_aggr(out=stat[:, 12:14], in_=stat[:, 0:12]), nrm([stat]), nrm([stat]))
        TS("dve", stat[:, 14:15], stat[:, 13:14], EPS, None, ALU.add, None, [stat], [stat])
        ACT(stat[:, 14:15], stat[:, 14:15], AF.Sqrt, [stat], [stat])
        P.op("dve", lambda e: e.reciprocal(out=stat[:, 15:16], in_=stat[:, 14:15]), nrm([stat]), nrm([stat]))
        STT(stat[:, 16:17], stat[:, 12:13], -1.0, stat[:, 15:16], ALU.mult, ALU.mult, [stat], [stat])
        ACT(r[:], r[:], AF.Identity, [r, stat], [r], scale=stat[:, 15:16], bias=stat[:, 16:17])
        TT("dve", r[:], r[:], lng[:], ALU.mult, [r, lng], [r])
        TT("pool", r[:], r[:], lnb[:], ALU.add, [r, lnb], [r])
        DMA("sp", dst_ap, r[:], [r], [dst_buf])

    def phase_hgrn(l, src, srcb, dst, dstb):
        j = l // 2
        P.push()
        w_in = Tl("hw_in", [128, 8, 4096], BF16)
        w_out = Tl("hw_out", [128, 8, 1024], BF16)
        for kc in range(8):
            DMA("pool", w_in[:, kc, :], W["hgrn_w_in"][j, kc * 128:(kc + 1) * 128, :], [], [w_in], max_dma_last_dim=4096)
        DMA("pool", w_out[:], W["hgrn_w_out"][j].rearrange("(kc p) n -> p kc n", p=128), [], [w_out], max_dma_last_dim=4096)
        lbraw = Tl("lbraw", [128, 2, 8], F32)
        lbc = Tl("lbc", [128, 8], F32)
        oml = Tl("oml", [128, 8], F32)
        DMA("sp", lbraw[:], W["hgrn_lb"].rearrange("j (h p) -> p j h", p=128), [], [lbraw], allow_slow_non_contiguous=True)
        if j == 0:
            TT("dve", lbc[:], lbraw[:, 0, :], lbraw[:, 0, :], ALU.subtract, [lbraw], [lbc])
        else:
            TT("dve", lbc[:], lbraw[:, 1, :], lbraw[:, 0, :], ALU.subtract, [lbraw], [lbc])
            ACT(lbc[:], lbc[:], AF.Sigmoid, [lbc], [lbc])
        TS("dve", oml[:], lbc[:], -1.0, 1.0, ALU.mult, ALU.add, [lbc], [oml])
        normw = Tl("normw", [128, 1024], F32)
        for h in range(8):
            DMA("sp", normw[:, h * 128:(h + 1) * 128], W["hgrn_norm_w"][j:j + 1, :].partition_broadcast(128), [], [normw])
        lng = Tl("lng", [128, 1024], F32)
        lnb = Tl("lnb", [128, 1024], F32)
        DMA("sp", lng[:], W["ln_g"][l, 0:1, :].partition_broadcast(128), [], [lng])
        DMA("sp", lnb[:], W["ln_b"][l, 0:1, :].partition_broadcast(128), [], [lnb])
        scp = Tl("scp", [128, 1024], F32)
        shb = Tl("shb", [128, 1024], F32)
        gbc = Tl("gbc", [128, 1024], F32)
        xts = [Tl("xt%d" % i, [128, 1024], F32) for i in range(2)]
        work = Tl("work", [128, 1024], F32)
        rr = Tl("rr", [128, 1024], F32)
        stat = Tl("stat", [128, 32], F32)
        hT = Tl("hT", [128, 8, 128], BF16)
        trp = Tl("trp", [128, 4, 128], F32, psum=True)
        qz = Tl("qzps", [128, 4, 128], F32, psum=True)
        qzb = [[Buf(), Buf()], [Buf(), Buf()]]
        vg = [Tl("vgps%d" % i, [128, 512], F32, psum=True) for i in range(2)]
        hd = [Tl("hdps%d" % i, [128, 4, 128], F32, psum=True) for i in range(2)]
        hdb = [[Buf() for _ in range(4)] for _ in range(2)]
        ktp = Tl("ktps", [128, 8, 128], BF16, psum=True)
        ktb = [Buf(), Buf()]
        ontp = Tl("ontps", [128, 8, 128], BF16, psum=True)
        vsb = Tl("vsb", [128, 1024], BF16)
        gw = Tl("gw", [128, 1024], F32)
        sig = [Tl("sig%d" % i, [128, 128], F32) for i in range(2)]
        lf = [Tl("lf%d" % i, [128, 128], F32) for i in range(2)]
        kk = [Tl("kk%d" % i, [128, 128], F32) for i in range(2)]
        bb = [Tl("bb%d" % i, [128, 128], F32) for i in range(2)]
        Ep = [Tl("Ep%d" % i, [128, 128], F32) for i in range(2)]
        Em = [Tl("Em%d" % i, [128, 128], F32) for i in range(2)]
        qT = [Tl("qT%d" % i, [128, 128], BF16) for i in range(2)]
        kT = [Tl("kT%d" % i, [128, 128], BF16) for i in range(2)]
        AT = [Tl("AT%d" % i, [128, 128], BF16) for i in range(2)]
        qpad = [Tl("qpad%d" % i, [128, 640], BF16) for i in range(2)]
        kmask = [Tl("kmask%d" % i, [128, 4, 128], BF16) for i in range(2)]
        kmb = [[Buf() for _ in range(4)] for _ in range(2)]
        s1 = [Tl("s1_%d" % i, [128, 128], F32) for i in range(2)]
        ss = Tl("ssq", [128, 16], F32)
        junk = Tl("junk", [128, 128], F32)
        on_all = Tl("on_all", [128, 8, 128], BF16)
        onT = Tl("onT", [128, 8, 128], BF16)
        state = [[Tl("st_%d_%d" % (s, h), [128, 128], F32) for h in range(8)] for s in range(NSEQ)]
        stbf = [[Tl("stb_%d_%d" % (s, h), [128, 128], BF16) for h in range(8)] for s in range(NSEQ)]
        for i in range(2):
            MS("pool", qpad[i][:], 0.0, [qpad[i]])
        for s in range(NSEQ):
            for h in range(8):
                MS("pool", state[s][h][:], 0.0, [state[s][h]])
                MS("pool", stbf[s][h][:], 0.0, [stbf[s][h]])
        it = 0
        for s in range(NSEQ):
            load_mod_bc(scp, l, s, 1, plus1=True)
            load_mod_bc(shb, l, s, 0)
            load_mod_bc(gbc, l, s, 2)
            for t in range(NT):
                g = s * NT + t
                xt = xts[g % 2]
                DMA("sp", xt[:], src[g * 128:(g + 1) * 128, :], [srcb[g]], [xt])
                make_hT(xt, scp, shb, hT, hT, trp, work)
                for cch in range(4):
                    pp = vg[cch % 2]
                    for kc in range(8):
                        MM(pp[:], hT[:, kc, :], w_in[:, kc, 2048 + cch * 512:2048 + (cch + 1) * 512], kc == 0, kc == 7, [hT, w_in], [pp])
                    if cch < 2:
                        CP("dve", vsb[:, cch * 512:(cch + 1) * 512], pp[:], [pp], [vsb])
                    else:
                        ACT(gw[:, (cch - 2) * 512:(cch - 1) * 512], pp[:], AF.Silu, [pp], [gw])
                TT("pool", gw[:], gw[:], normw[:], ALU.mult, [gw, normw], [gw])
                for h in range(8):
                    p2 = it % 2
                    it += 1
                    qps, zps = qz[:, 2 * p2, :], qz[:, 2 * p2 + 1, :]
                    qb_, zb_ = qzb[p2]
                    for kc in range(8):
                        MM(qps, w_in[:, kc, h * 128:(h + 1) * 128], hT[:, kc, :], kc == 0, kc == 7, [hT, w_in], [qb_])
                    for kc in range(8):
                        MM(zps, w_in[:, kc, 1024 + h * 128:1024 + (h + 1) * 128], hT[:, kc, :], kc == 0, kc == 7, [hT, w_in], [zb_])
                    ACT(sig[p2][:], zps, AF.Sigmoid, [zb_], [sig[p2]])
                    TS("dve", sig[p2][:], sig[p2][:], oml[:, h:h + 1], lbc[:, h:h + 1], ALU.mult, ALU.add, [sig[p2], oml, lbc], [sig[p2]])
                    ACT(lf[p2][:], sig[p2][:], AF.Ln, [sig[p2]], [lf[p2]])
                    dbg('f', sig[p2][:], [sig[p2]]); dbg('lf', lf[p2][:], [lf[p2]])
                    TS("pool", kk[p2][:], sig[p2][:], -1.0, 1.0, ALU.mult, ALU.add, [sig[p2]], [kk[p2]])
                    P.op("dve", lambda e, p2=p2: e.tensor_tensor_scan(out=bb[p2][:], data0=scanmask[:], data1=lf[p2][:], initial=0.0,
                                                                     op0=ALU.mult, op1=ALU.add), nrm([scanmask, lf[p2]]), nrm([bb[p2]]))
                    ACT(Ep[p2][:], bb[p2][:], AF.Exp, [bb[p2]], [Ep[p2]])
                    ACT(Em[p2][:], bb[p2][:], AF.Exp, [bb[p2]], [Em[p2]], scale=-1.0)
                    TT("dve", qT[p2][:], qps, Ep[p2][:], ALU.mult, [qb_, Ep[p2]], [qT[p2]])
                    CP("pool", qpad[p2][:].rearrange("p (c x) -> p c x", x=160)[:, :, 0:32],
                       qT[p2][:].rearrange("p (c j) -> p c j", j=32), [qT[p2]], [qpad[p2]])
                    TT("pool", kT[p2][:], kk[p2][:], Em[p2][:], ALU.mult, [kk[p2], Em[p2]], [kT[p2]])
                    dbg('bb', bb[p2][:], [bb[p2]]); dbg('qT', qT[p2][:], [qT[p2]]); dbg('kT', kT[p2][:], [kT[p2]]); dbg('qpad', qpad[p2][:], [qpad[p2]])
                    stp, ops_, up = hd[p2][:, 0, :], vg[p2][:, 0:128], [hd[p2][:, 2, :], hd[p2][:, 3, :]]
                    stb_, ob_, ub_ = hdb[p2][0], vg[p2], [hdb[p2][2], hdb[p2][3]]
                    MM(stp, kT[p2][:], qT[p2][:], True, True, [kT[p2], qT[p2]], [stb_])
                    TR(ktp[:, p2, :], kT[p2][:], identb[:], [kT[p2], identb], [ktb[p2]])
                    TT("dve", AT[p2][:], stp, blockmask[:], ALU.mult, [stb_, blockmask], [AT[p2]])
                    for c in range(4):
                        ACT(kmask[p2][:, c, :], ktp[:, p2, :], AF.Copy, [ktb[p2], chunkind], [kmb[p2][c]], scale=chunkind[:, c:c + 1])
                    vh = vsb[:, h * 128:(h + 1) * 128]
                    MM(ops_, AT[p2][:], vh, True, False, [AT[p2], vsb], [ob_])
                    stt, stb16 = state[s][h], stbf[s][h]
                    for c in range(4):
                        MM(ops_, qpad[p2][:, c * 128:(c + 1) * 128], stb16[:], False, c == 3, [qpad[p2], stb16], [ob_])
                        MM(up[c % 2], kmask[p2][:, c, :], vh, True, True, [kmb[p2][c], vsb], [ub_[c % 2]])
                        ebl = Ep[p2][:, 32 * c + 31:32 * c + 32]
                        TS("pool", s1[p2][:], stt[:], ebl, None, ALU.mult, None, [stt, Ep[p2]], [s1[p2]])
                        STT(stt[:], up[c % 2], ebl, s1[p2][:], ALU.mult, ALU.add, [ub_[c % 2], Ep[p2], s1[p2]], [stt])
                        ACT(stb16[:], stt[:], AF.Copy, [stt], [stb16])
                    dbg('AT', AT[p2][:], [AT[p2]]); dbg('ops', ops_, [ob_], psum=True); dbg('kmask', kmask[p2][:], kmb[p2]); dbg('state', stt[:], [stt])
                    ACT(junk[:], ops_, AF.Square, [ob_], [junk, ss], accum_out=ss[:, h:h + 1])
                    ACT(ss[:, 8 + h:9 + h], ss[:, h:h + 1], AF.Sqrt, [ss], [ss], scale=1.0 / 128.0, bias=EPS)
                    P.op("dve", lambda e, h=h: e.reciprocal(out=ss[:, 8 + h:9 + h], in_=ss[:, 8 + h:9 + h]), nrm([ss]), nrm([ss]))
                    STT(on_all[:, h, :], ops_, ss[:, 8 + h:9 + h], gw[:, h * 128:(h + 1) * 128], ALU.mult, ALU.mult, [ob_, ss, gw], [on_all])
                    TR(ontp[:, h, :], on_all[:, h, :], identb[:], [on_all, identb], [ontp])
                dbg('on_all', on_all[:], [on_all]); dbg('gw', gw[:], [gw]); dbg('vsb', vsb[:], [vsb]); dbg('hT', hT[:], [hT]); dbg('ss', ss[:], [ss])
                CP("dve", onT[:, 0:4, :], ontp[:, 0:4, :], [ontp], [onT])
                ACT(onT[:, 4:8, :], ontp[:, 4:8, :], AF.Copy, [ontp], [onT])
                for half in range(2):
                    for h in range(8):
                        MM(vg[half][:], onT[:, h, :], w_out[:, h, half * 512:(half + 1) * 512], h == 0, h == 7, [onT, w_out], [vg[half]])
                dbg('y0', vg[0][:], [vg[0]], psum=True)
                resid_ln(xt, [(vg[0][:], vg[0]), (vg[1][:], vg[1])], gbc, lng, lnb, rr, stat, dst[g * 128:(g + 1) * 128, :], dstb[g])
        P.pop()

    def phase_moe_dense(l, src, srcb, dst, dstb):
        P.push()
        GT = min(16, NT)
        NG = (NSEQ * NT) // GT
        SGT = min(4, GT)
        wr = Tl("wr", [128, 8, 36], F32)
        DMA("sp", wr[:], W["router_w"][l], [], [wr])
        rbias = Tl("rbias", [128, 36], F32)
        DMA("sp", rbias[:], W["router_b"][l:l + 1, :].partition_broadcast(128), [], [rbias])
        lng = Tl("lng", [128, 1024], F32)
        lnb = Tl("lnb", [128, 1024], F32)
        DMA("sp", lng[:], W["ln_g"][l, 1:2, :].partition_broadcast(128), [], [lng])
        DMA("sp", lnb[:], W["ln_b"][l, 1:2, :].partition_broadcast(128), [], [lnb])
        scp = Tl("scp", [128, 1024], F32)
        shb = Tl("shb", [128, 1024], F32)
        gbc = Tl("gbc", [128, 1024], F32)
        xts = [Tl("xt%d" % i, [128, 1024], F32) for i in range(2)]
        work = Tl("work", [128, 1024], F32)
        rr = Tl("rr", [128, 1024], F32)
        stat = Tl("stat", [128, 32], F32)
        h2T = Tl("h2T", [128, 8, GT * 128], BF16)
        h2Tf = Tl("h2Tf", [128, 8, 128], F32)
        yacc = Tl("yacc", [128, GT, 1024], F32)
        yaccb = [Buf() for _ in range(GT)]
        gates = Tl("gates", [128, GT, 32], F32)
        gT = [Tl("gT%d" % i, [128, 4, 512], BF16) for i in range(2)]
        slt = [Tl("slt%d" % i, [128, 512], F32) for i in range(2)]
        wb = [dict(w1=Tl("w1_%d" % i, [128, 8, 512], BF16), w3=Tl("w3_%d" % i, [128, 8, 512], BF16),
                   w2=Tl("w2_%d" % i, [128, 4, 1024], BF16)) for i in range(2)]
        trp = Tl("trp", [128, 4, 128], F32, psum=True)
        lgp = Tl("lgp", [128, 512], F32, psum=True)
        hp1 = [Tl("hp1_%d" % i, [128, 512], F32, psum=True) for i in range(2)]
        hp3 = [Tl("hp3_%d" % i, [128, 512], F32, psum=True) for i in range(2)]
        yp = [Tl("yp%d" % i, [128, 512], F32, psum=True) for i in range(2)]
        lg = Tl("lg", [128, 36], F32)
        sm = Tl("rsm", [128, 16], F32)
        oh = Tl("oh", [128, 4], F32)
        ejunk = Tl("ejunk", [128, 4], F32)
        m1 = Tl("m1", [128, 4], F32)
        m2 = Tl("m2", [128, 4], F32)
        dd = Tl("dd", [128, 4], F32)
        c1 = Tl("c1", [128, 4], F32)
        c2 = Tl("c2", [128, 4], F32)
        mk1 = Tl("mk1", [128, 4, 8], F32)
        mk2 = Tl("mk2", [128, 4, 8], F32)
        el2 = Tl("el2", [128, 4, 8], F32)

        def bc48(ap):
            return ap.unsqueeze(2).to_broadcast([128, 4, 8])

        for gidx in range(NG):
            g0 = gidx * GT
            s = g0 // NT
            load_mod_bc(scp, l, s, 4, plus1=True)
            load_mod_bc(shb, l, s, 3)
            load_mod_bc(gbc, l, s, 5)
            if MOE_CUT != -3:
                MS("pool", yacc[:], 0.0, yaccb)
            for i in range(GT):
                g = g0 + i
                xt = xts[g % 2]
                DMA("sp", xt[:], src[g * 128:(g + 1) * 128, :], [srcb[g]], [xt])
                if MOE_CUT >= -1:
                    make_hT(xt, scp, shb, h2T[:, :, i * 128:(i + 1) * 128], h2T, trp, work, h2Tf if MOE_CUT >= 0 else None, h2Tf)
                if MOE_CUT <= 0:
                    continue
                for kc in range(8):
                    MM(lgp[:, 0:36], h2Tf[:, kc, :], wr[:, kc, :], kc == 0, kc == 7, [h2Tf, wr], [lgp])
                TT("dve", lg[:], lgp[:, 0:36], rbias[:], ALU.add, [lgp, rbias], [lg])
                if MOE_CUT == 1:
                    continue
                gl = lg[:, 0:4]
                el = lg[:, 4:36].rearrange("p (g j) -> p g j", j=8)
                P.op("dve", lambda e, gl=gl: e.tensor_reduce(out=sm[:, 0:1], in_=gl, axis=AX.X, op=ALU.max), nrm([lg]), nrm([sm]))
                TS("dve", oh[:], gl, sm[:, 0:1], None, ALU.is_equal, None, [lg, sm], [oh])
                TS("dve", sm[:, 1:2], sm[:, 0:1], -1.0, None, ALU.mult, None, [sm], [sm])
                ACT(ejunk[:], gl, AF.Exp, [lg, sm], [ejunk, sm], bias=sm[:, 1:2], accum_out=sm[:, 2:3])
                P.op("dve", lambda e: e.reciprocal(out=sm[:, 3:4], in_=sm[:, 2:3]), nrm([sm]), nrm([sm]))
                P.op("dve", lambda e, el=el: e.tensor_reduce(out=m1[:], in_=el, axis=AX.X, op=ALU.max), nrm([lg]), nrm([m1]))
                TT("dve", mk1[:], el, bc48(m1[:]), ALU.is_equal, [lg, m1], [mk1])
                STT(el2[:], mk1[:], -1.0e30, el, ALU.mult, ALU.add, [mk1, lg], [el2])
                P.op("dve", lambda e: e.tensor_reduce(out=m2[:], in_=el2[:], axis=AX.X, op=ALU.max), nrm([el2]), nrm([m2]))
                TT("dve", mk2[:], el2[:], bc48(m2[:]), ALU.is_equal, [el2, m2], [mk2])
                TT("dve", dd[:], m2[:], m1[:], ALU.subtract, [m1, m2], [dd])
                ACT(dd[:], dd[:], AF.Exp, [dd], [dd])
                TS("dve", c1[:], dd[:], 1.0, None, ALU.add, None, [dd], [c1])
                P.op("dve", lambda e: e.reciprocal(out=c1[:], in_=c1[:]), nrm([c1]), nrm([c1]))
                TT("dve", c2[:], dd[:], c1[:], ALU.mult, [dd, c1], [c2])
                TS("dve", oh[:], oh[:], sm[:, 3:4], None, ALU.mult, None, [oh, sm], [oh])
                TT("dve", c1[:], c1[:], oh[:], ALU.mult, [c1, oh], [c1])
                TT("dve", c2[:], c2[:], oh[:], ALU.mult, [c2, oh], [c2])
                TT("dve", mk1[:], mk1[:], bc48(c1[:]), ALU.mult, [mk1, c1], [mk1])
                TT("dve", mk2[:], mk2[:], bc48(c2[:]), ALU.mult, [mk2, c2], [mk2])
                TT("dve", gates[:, i, :].rearrange("p (g j) -> p g j", j=8), mk1[:], mk2[:], ALU.add, [mk1, mk2], [gates])
            if DEBUG:
                dbg("gates", gates[:], [gates])
            nsub = 0
            for e in range(NE if MOE_STAGE >= 2 else 0):
                wbe = wb[e % 2]
                DMA("pool", wbe["w1"][:], W["moe_w1"][l, e].rearrange("(kc p) n -> p kc n", p=128), [], [wbe["w1"]])
                DMA("pool", wbe["w3"][:], W["moe_w3"][l, e].rearrange("(kc p) n -> p kc n", p=128), [], [wbe["w3"]])
                DMA("pool", wbe["w2"][:], W["moe_w2"][l, e].rearrange("(kc p) n -> p kc n", p=128), [], [wbe["w2"]], max_dma_last_dim=4096)
                for sg in range(GT // SGT):
                    ncol = SGT * 128
                    c0 = sg * ncol
                    gt_ = gT[nsub % 2]
                    nsub += 1
                    for fc in range(4):
                        a1, a3 = hp1[fc % 2], hp3[fc % 2]
                        for kc in range(8):
                            MM(a1[:, 0:ncol], wbe["w1"][:, kc, fc * 128:(fc + 1) * 128], h2T[:, kc, c0:c0 + ncol], kc == 0, kc == 7, [wbe["w1"], h2T], [a1])
                        for kc in range(8):
                            MM(a3[:, 0:ncol], wbe["w3"][:, kc, fc * 128:(fc + 1) * 128], h2T[:, kc, c0:c0 + ncol], kc == 0, kc == 7, [wbe["w3"], h2T], [a3])
                        sl = slt[fc % 2]
                        ACT(sl[:, 0:ncol], a1[:, 0:ncol], AF.Silu, [a1], [sl])
                        TT("dve", gt_[:, fc, 0:ncol], sl[:, 0:ncol], a3[:, 0:ncol], ALU.mult, [sl, a3], [gt_])
                    for ti in range(SGT):
                        i = sg * SGT + ti
                        for half in range(2):
                            ypp = yp[half]
                            for fc in range(4):
                                MM(ypp[:], gt_[:, fc, ti * 128:(ti + 1) * 128], wbe["w2"][:, fc, half * 512:(half + 1) * 512], fc == 0, fc == 3, [gt_, wbe["w2"]], [ypp])
                            ya = yacc[:, i, half * 512:(half + 1) * 512]
                            STT(ya, ypp[:], gates[:, i, e:e + 1], ya, ALU.mult, ALU.add, [ypp, gates, yaccb[i]], [yaccb[i]])
            for i in range(GT):
                g = g0 + i
                xt = xts[g % 2]
                DMA("sp", xt[:], src[g * 128:(g + 1) * 128, :], [srcb[g]], [xt])
                resid_ln(xt, [(yacc[:, i, 0:512], yaccb[i]), (yacc[:, i, 512:1024], yaccb[i])], gbc, lng, lnb, rr, stat,
                         dst[g * 128:(g + 1) * 128, :], dstb[g])
        P.pop()

    def phase_moe(l, src, srcb, dst, dstb):
        P.push()
        NTT = NSEQ * NT
        TB = 512
        NBLK = (2 * NTOK) // TB + 32
        wr = Tl("wr", [128, 8, 36], F32)
        DMA("sp", wr[:], W["router_w"][l], [], [wr])
        rbias = Tl("rbias", [128, 36], F32)
        DMA("sp", rbias[:], W["router_b"][l:l + 1, :].partition_broadcast(128), [], [rbias])
        lng = Tl("lng", [128, 1024], F32)
        lnb = Tl("lnb", [128, 1024], F32)
        DMA("sp", lng[:], W["ln_g"][l, 1:2, :].partition_broadcast(128), [], [lng])
        DMA("sp", lnb[:], W["ln_b"][l, 1:2, :].partition_broadcast(128), [], [lnb])
        widx_c = Tl("widx_c", [128, 12], F32)
        DMA("sp", widx_c[:], CD["widx"], [], [widx_c])
        utri = Tl("utri", [128, 128], BF16)
        ones = Tl("ones", [128, 128], BF16)
        DMA("pool", utri[:], CD["utri"], [], [utri])
        MS("pool", ones[:], 1.0, [ones])
        scp = Tl("scp", [128, 1024], F32)
        shb = Tl("shb", [128, 1024], F32)
        gbc = Tl("gbc", [128, 1024], F32)
        xts = [Tl("xt%d" % i, [128, 1024], F32) for i in range(2)]
        work = Tl("work", [128, 1024], F32)
        rr = Tl("rr", [128, 1024], F32)
        stat = Tl("stat", [128, 32], F32)
        h2Tf = Tl("h2Tf", [128, 8, 128], F32)
        h2b = [Tl("h2b%d" % i, [128, 1024], BF16) for i in range(2)]
        m1all = Tl("m1all", [128, NTT, 32], F32)
        m2all = Tl("m2all", [128, NTT, 32], F32)
        rkall = Tl("rkall", [128, NTT, 32], F32)
        wab = Tl("wab", [128, NTT, 2], F32)
        slots = Tl("slots", [128, NTT, 2], I32)
        cum = Tl("cum", [128, 32], F32)
        MS("pool", cum[:], 0.0, [cum])
        mb16 = Tl("mb16", [128, 32], BF16)
        bankA = [Tl("mbk%d" % i, [128, 512], F32, psum=True) for i in range(7)]
        trp = Tl("trp", [128, 4, 128], F32, psum=True)
        lgp, rkp, csp = bankA[0], bankA[1], bankA[2]
        lg = Tl("lg", [128, 36], F32)
        sm = Tl("rsm", [128, 16], F32)
        oh = Tl("oh", [128, 4], F32)
        ejunk = Tl("ejunk", [128, 4], F32)
        m1 = Tl("m1", [128, 4], F32)
        m2 = Tl("m2", [128, 4], F32)
        dd = Tl("dd", [128, 4], F32)
        c1 = Tl("c1", [128, 4], F32)
        c2 = Tl("c2", [128, 4], F32)
        mk1 = Tl("mk1", [128, 4, 8], F32)
        mk2 = Tl("mk2", [128, 4, 8], F32)
        el2 = Tl("el2", [128, 4, 8], F32)
        h2_d = moe_h2_d
        h2db = [Buf() for _ in range(NTT)]

        def bc48(ap):
            return ap.unsqueeze(2).to_broadcast([128, 4, 8])

        for g in range(NTT):
            s = g // NT
            if g % NT == 0:
                load_mod_bc(scp, l, s, 4, plus1=True)
                load_mod_bc(shb, l, s, 3)
                load_mod_bc(gbc, l, s, 5)
            xt = xts[g % 2]
            DMA("sp", xt[:], src[g * 128:(g + 1) * 128, :], [srcb[g]], [xt])
            TT("dve", work[:], xt[:], scp[:], ALU.mult, [xt, scp], [work])
            TT("pool", work[:], work[:], shb[:], ALU.add, [work, shb], [work])
            hb = h2b[g % 2]
            ACT(hb[:], work[:], AF.Copy, [work], [hb])
            DMA("sp", h2_d[g * 128:(g + 1) * 128, :], hb[:], [hb], [h2db[g]])
            for half in range(2):
                for i in range(4):
                    kc = half * 4 + i
                    TR(trp[:, i, :], work[:, kc * 128:(kc + 1) * 128], identf[:], [work, identf], [trp])
                if half == 0:
                    CP("dve", h2Tf[:, 0:4, :], trp[:], [trp], [h2Tf])
                else:
                    ACT(h2Tf[:, 4:8, :], trp[:], AF.Copy, [trp], [h2Tf])
            for kc in range(8):
                MM(lgp[:, 0:36], h2Tf[:, kc, :], wr[:, kc, :], kc == 0, kc == 7, [h2Tf, wr], [lgp])
            TT("dve", lg[:], lgp[:, 0:36], rbias[:], ALU.add, [lgp, rbias], [lg])
            gl = lg[:, 0:4]
            el = lg[:, 4:36].rearrange("p (g j) -> p g j", j=8)
            P.op("dve", lambda e, gl=gl: e.tensor_reduce(out=sm[:, 0:1], in_=gl, axis=AX.X, op=ALU.max), nrm([lg]), nrm([sm]))
            TS("dve", oh[:], gl, sm[:, 0:1], None, ALU.is_equal, None, [lg, sm], [oh])
            TS("dve", sm[:, 1:2], sm[:, 0:1], -1.0, None, ALU.mult, None, [sm], [sm])
            ACT(ejunk[:], gl, AF.Exp, [lg, sm], [ejunk, sm], bias=sm[:, 1:2], accum_out=sm[:, 2:3])
            P.op("dve", lambda e: e.reciprocal(out=sm[:, 3:4], in_=sm[:, 2:3]), nrm([sm]), nrm([sm]))
            P.op("dve", lambda e, el=el: e.tensor_reduce(out=m1[:], in_=el, axis=AX.X, op=ALU.max), nrm([lg]), nrm([m1]))
            TT("dve", mk1[:], el, bc48(m1[:]), ALU.is_equal, [lg, m1], [mk1])
            STT(el2[:], mk1[:], -1.0e30, el, ALU.mult, ALU.add, [mk1, lg], [el2])
            P.op("dve", lambda e: e.tensor_reduce(out=m2[:], in_=el2[:], axis=AX.X, op=ALU.max), nrm([el2]), nrm([m2]))
            TT("dve", mk2[:], el2[:], bc48(m2[:]), ALU.is_equal, [el2, m2], [mk2])
            TT("dve", dd[:], m2[:], m1[:], ALU.subtract, [m1, m2], [dd])
            ACT(dd[:], dd[:], AF.Exp, [dd], [dd])
            TS("dve", c1[:], dd[:], 1.0, None, ALU.add, None, [dd], [c1])
            P.op("dve", lambda e: e.reciprocal(out=c1[:], in_=c1[:]), nrm([c1]), nrm([c1]))
            TT("dve", c2[:], dd[:], c1[:], ALU.mult, [dd, c1], [c2])
            m1g = m1all[:, g, :].rearrange("p (g j) -> p g j", j=8)
            m2g = m2all[:, g, :].rearrange("p (g j) -> p g j", j=8)
            TT("dve", m1g, mk1[:], bc48(oh[:]), ALU.mult, [mk1, oh], [m1all])
            TT("dve", m2g, mk2[:], bc48(oh[:]), ALU.mult, [mk2, oh], [m2all])
            TT("dve", c1[:], c1[:], oh[:], ALU.mult, [c1, oh], [c1])
            TT("dve", c2[:], c2[:], oh[:], ALU.mult, [c2, oh], [c2])
            P.op("dve", lambda e: e.tensor_reduce(out=sm[:, 4:5], in_=c1[:], axis=AX.X, op=ALU.add), nrm([c1]), nrm([sm]))
            P.op("dve", lambda e: e.tensor_reduce(out=sm[:, 5:6], in_=c2[:], axis=AX.X, op=ALU.add), nrm([c2]), nrm([sm]))
            TS("dve", wab[:, g, :], sm[:, 4:6], sm[:, 3:4], None, ALU.mult, None, [sm], [wab])
            TT("dve", mb16[:], m1all[:, g, :], m2all[:, g, :], ALU.add, [m1all, m2all], [mb16])
            MM(rkp[:, 0:32], utri[:], mb16[:], True, True, [utri, mb16], [rkp])
            MM(csp[:, 0:32], ones[:], mb16[:], True, True, [ones, mb16], [csp])
            TT("dve", rkall[:, g, :], rkp[:, 0:32], cum[:], ALU.add, [rkp, cum], [rkall])
            TT("dve", cum[:], cum[:], csp[:, 0:32], ALU.add, [cum, csp], [cum])

        if MOE_CUT >= 2:
            pass
        pad = Tl("pad", [128, 32], F32)
        padi = Tl("padi", [128, 32], I32)
        pend = Tl("pend", [128, 32], F32)
        pstart = Tl("pstart", [128, 32], F32)
        onesf = Tl("onesf", [128, 32], F32)
        MS("pool", onesf[:], 1.0, [onesf])
        CP("dve", padi[:], cum[:], [cum], [padi])
        TS("dve", padi[:], padi[:], TB - 1, None, ALU.add, None, [padi], [padi])
        TS("dve", padi[:], padi[:], 9, None, ALU.arith_shift_right, None, [padi], [padi])
        TS("dve", padi[:], padi[:], 9, None, ALU.logical_shift_left, None, [padi], [padi])
        CP("dve", pad[:], padi[:], [padi], [pad])
        P.op("dve", lambda e: e.tensor_tensor_scan(out=pend[:], data0=onesf[:], data1=pad[:], initial=0.0, op0=ALU.mult, op1=ALU.add),
             nrm([onesf, pad]), nrm([pend]))
        TT("dve", pstart[:], pend[:], pad[:], ALU.subtract, [pend, pad], [pstart])
        bstart = Tl("bstart", [128, NBLK], F32)
        DMA("sp", bstart[:], CD["bstart"][0:1, 0:NBLK].partition_broadcast(128), [], [bstart])
        cmp_ = Tl("cmpb", [128, NBLK, 32], F32)
        bexp = Tl("bexp", [128, NBLK], F32)
        TT("dve", cmp_[:], pend[:].unsqueeze(1).to_broadcast([128, NBLK, 32]), bstart[:].unsqueeze(2).to_broadcast([128, NBLK, 32]),
           ALU.is_le, [pend, bstart], [cmp_])
        P.op("dve", lambda e: e.tensor_reduce(out=bexp[:], in_=cmp_[:], axis=AX.X, op=ALU.add), nrm([cmp_]), nrm([bexp]))
        TS("dve", bexp[:], bexp[:], 31.0, None, ALU.min, None, [bexp], [bexp])
        widf = Tl("widf", [128, NBLK, 12], F32)
        widi = Tl("widi", [128, NBLK, 12], I32)
        TS("dve", bexp[:], bexp[:], float(l * 32), None, ALU.add, None, [bexp], [bexp])
        STT(widf[:, :, 0:8], bexp[:].unsqueeze(2).to_broadcast([128, NBLK, 8]), 1024.0,
            widx_c[:, 0:8].unsqueeze(1).to_broadcast([128, NBLK, 8]), ALU.mult, ALU.add, [bexp, widx_c], [widf])
        STT(widf[:, :, 8:12], bexp[:].unsqueeze(2).to_broadcast([128, NBLK, 4]), 512.0,
            widx_c[:, 8:12].unsqueeze(1).to_broadcast([128, NBLK, 4]), ALU.mult, ALU.add, [bexp, widx_c], [widf])
        CP("dve", widi[:], widf[:], [widf], [widi])

        dtmp = Tl("dtmp", [128, 32], F32)
        dtmp2 = Tl("dtmp2", [128, 32], F32)
        slf = Tl("slf", [128, 2], F32)
        xbufb = [Buf() for _ in range(2 * NTT)]
        for g in range(NTT if MOE_CUT >= 3 else 0):
            TT("dve", dtmp[:], rkall[:, g, :], pstart[:], ALU.add, [rkall, pstart], [dtmp])
            TT("dve", dtmp2[:], dtmp[:], m1all[:, g, :], ALU.mult, [dtmp, m1all], [dtmp2])
            P.op("dve", lambda e: e.tensor_reduce(out=slf[:, 0:1], in_=dtmp2[:], axis=AX.X, op=ALU.add), nrm([dtmp2]), nrm([slf]))
            TT("dve", dtmp2[:], dtmp[:], m2all[:, g, :], ALU.mult, [dtmp, m2all], [dtmp2])
            P.op("dve", lambda e: e.tensor_reduce(out=slf[:, 1:2], in_=dtmp2[:], axis=AX.X, op=ALU.add), nrm([dtmp2]), nrm([slf]))
            CP("dve", slots[:, g, :], slf[:], [slf], [slots])
            hb = h2b[g % 2]
            DMA("sp", hb[:], h2_d[g * 128:(g + 1) * 128, :], [h2db[g]], [hb])
            for k in range(2):
                P.dma("pool", lambda e, g=g, k=k, hb=hb: e.indirect_dma_start(
                    out=moe_xbuf_d[:, :], out_offset=bass.IndirectOffsetOnAxis(ap=slots[:, g, k:k + 1], axis=0),
                    in_=hb[:], in_offset=None), nrm([hb, slots]), [xbufb[2 * g + k]])

        wb = [dict(w1=Tl("w1_%d" % i, [128, 8, 512], BF16), w3=Tl("w3_%d" % i, [128, 8, 512], BF16),
                   w2=Tl("w2_%d" % i, [128, 4, 1024], BF16)) for i in range(2)]
        xg = [Tl("xg%d" % i, [128, 4, 1024], BF16) for i in range(2)]
        xT = [Tl("xTb%d" % i, [128, 8, 512], BF16) for i in range(2)]
        gT = [Tl("gT%d" % i, [128, 4, 512], BF16) for i in range(2)]
        slt = [Tl("slt%d" % i, [128, 512], F32) for i in range(2)]
        yt = [Tl("ytb%d" % i, [128, 1024], BF16) for i in range(2)]
        tpb = bankA[0]
        tpv = tpb[:].bitcast(BF16).rearrange("p (r c) -> p r c", c=512)
        hp1 = [bankA[1], bankA[2]]
        hp3 = [bankA[3], bankA[4]]
        yp = [bankA[5], bankA[6]]
        ybufb = [Buf() for _ in range(NBLK)]
        w1rows = W["moe_w1"].rearrange("l e k f -> (l e k) f")
        w3rows = W["moe_w3"].rearrange("l e k f -> (l e k) f")
        w2rows = W["moe_w2"].rearrange("l e k f -> (l e k) f")
        nyt = 0
        for b in range(NBLK if MOE_CUT >= 4 else 0):
            wbe = wb[b % 2]
            for kc in range(8):
                P.dma("pool", lambda e, b=b, kc=kc, wbe=wbe: e.indirect_dma_start(
                    out=wbe["w1"][:, kc, :], out_offset=None, in_=w1rows,
                    in_offset=bass.IndirectOffsetOnAxis(ap=widi[:, b, kc:kc + 1], axis=0)), nrm([widi]), nrm([wbe["w1"]]))
                P.dma("pool", lambda e, b=b, kc=kc, wbe=wbe: e.indirect_dma_start(
                    out=wbe["w3"][:, kc, :], out_offset=None, in_=w3rows,
                    in_offset=bass.IndirectOffsetOnAxis(ap=widi[:, b, kc:kc + 1], axis=0)), nrm([widi]), nrm([wbe["w3"]]))
            for fc in range(4):
                P.dma("pool", lambda e, b=b, fc=fc, wbe=wbe: e.indirect_dma_start(
                    out=wbe["w2"][:, fc, :], out_offset=None, in_=w2rows,
                    in_offset=bass.IndirectOffsetOnAxis(ap=widi[:, b, 8 + fc:9 + fc], axis=0)), nrm([widi]), nrm([wbe["w2"]]))
            xgb = xg[b % 2]
            DMA("sp", xgb[:], moe_xbuf_d[b * TB:(b + 1) * TB, :].rearrange("(j p) d -> p j d", p=128), xbufb, [xgb])
            xTb = xT[b % 2]
            for kc in range(8):
                for jj in range(4):
                    TR(tpv[:, kc % 2, jj * 128:(jj + 1) * 128], xgb[:, jj, kc * 128:(kc + 1) * 128], identb[:], [xgb, identb], [tpb])
                if kc % 2 == 0:
                    CP("dve", xTb[:, kc, :], tpv[:, kc % 2, :], [tpb], [xTb])
                else:
                    ACT(xTb[:, kc, :], tpv[:, kc % 2, :], AF.Copy, [tpb], [xTb])
            gt_ = gT[b % 2]
            for fc in range(4):
                a1, a3 = hp1[fc % 2], hp3[fc % 2]
                for kc in range(8):
                    MM(a1[:], wbe["w1"][:, kc, fc * 128:(fc + 1) * 128], xTb[:, kc, :], kc == 0, kc == 7, [wbe["w1"], xTb], [a1])
                for kc in range(8):
                    MM(a3[:], wbe["w3"][:, kc, fc * 128:(fc + 1) * 128], xTb[:, kc, :], kc == 0, kc == 7, [wbe["w3"], xTb], [a3])
                sl = slt[fc % 2]
                ACT(sl[:], a1[:], AF.Silu, [a1], [sl])
                TT("dve", gt_[:, fc, :], sl[:], a3[:], ALU.mult, [sl, a3], [gt_])
            for ti in range(4):
                ytt = yt[nyt % 2]
                nyt += 1
                for half in range(2):
                    ypp = yp[half]
                    for fc in range(4):
                        MM(ypp[:], gt_[:, fc, ti * 128:(ti + 1) * 128], wbe["w2"][:, fc, half * 512:(half + 1) * 512], fc == 0, fc == 3, [gt_, wbe["w2"]], [ypp])
                    if half == 0:
                        ACT(ytt[:, 0:512], ypp[:], AF.Copy, [ypp], [ytt])
                    else:
                        CP("dve", ytt[:, 512:1024], ypp[:], [ypp], [ytt])
                r0 = b * TB + ti * 128
                DMA("sp", moe_ybuf_d[r0:r0 + 128, :], ytt[:], [ytt], [ybufb[b]])

        ya = [Tl("yga%d" % i, [128, 1024], BF16) for i in range(2)]
        yb_ = [Tl("ygb%d" % i, [128, 1024], BF16) for i in range(2)]
        ycomb = Tl("ycomb", [128, 1024], F32)
        for g in range(NTT):
            s = g // NT
            if g % NT == 0:
                load_mod_bc(gbc, l, s, 5)
            xt = xts[g % 2]
            DMA("sp", xt[:], src[g * 128:(g + 1) * 128, :], [srcb[g]], [xt])
            ga, gb_ = ya[g % 2], yb_[g % 2]
            if MOE_CUT < 5:
                MS("pool", ga[:], 0.0, [ga])
                MS("pool", gb_[:], 0.0, [gb_])
            for k, dstt in (((0, ga), (1, gb_)) if MOE_CUT >= 5 else ()):
                P.dma("pool", lambda e, g=g, k=k, dstt=dstt: e.indirect_dma_start(
                    out=dstt[:], out_offset=None, in_=moe_ybuf_d[:, :],
                    in_offset=bass.IndirectOffsetOnAxis(ap=slots[:, g, k:k + 1], axis=0)), nrm(ybufb + [slots]), nrm([dstt]))
            TS("dve", ycomb[:], ga[:], wab[:, g, 0:1], None, ALU.mult, None, [ga, wab], [ycomb])
            STT(ycomb[:], gb_[:], wab[:, g, 1:2], ycomb[:], ALU.mult, ALU.add, [gb_, wab, ycomb], [ycomb])
            resid_ln(xt, [(ycomb[:, 0:512], ycomb), (ycomb[:, 512:1024], ycomb)], gbc, lng, lnb, rr, stat,
                     dst[g * 128:(g + 1) * 128, :], dstb[g])
        P.pop()

    def phase_attn(l, src, srcb, dst, dstb):
        import math
        j = l // 2
        lam_init = 0.8 - 0.6 * math.exp(-0.3 * l)
        QB = min(512, S)
        NQB = QB // 128
        NSB = S // QB
        P.push()
        banks = [Tl("bk%d" % i, [128, 512], F32, psum=True) for i in range(8)]

        class TrV:
            def __init__(self, bank):
                self.b = bank.b
                self.v = bank[:].rearrange("p (i t) -> p i t", t=128)

            def __getitem__(self, k):
                return self.v[k]
        trp_t = banks[0]
        w_out = Tl("aw_out", [128, 8, 1024], BF16)
        DMA("pool", w_out[:], W["attn_w_out"][j].rearrange("(kc p) n -> p kc n", p=128), [], [w_out], max_dma_last_dim=4096)
        lng = Tl("lng", [128, 1024], F32)
        lnb = Tl("lnb", [128, 1024], F32)
        DMA("sp", lng[:], W["ln_g"][l, 0:1, :].partition_broadcast(128), [], [lng])
        DMA("sp", lnb[:], W["ln_b"][l, 0:1, :].partition_broadcast(128), [], [lnb])
        subw = Tl("subw", [128, 128], F32)
        DMA("sp", subw[:], W["attn_subln_w"][j:j + 1, :].partition_broadcast(128), [], [subw])
        TS("dve", subw[:], subw[:], 1.0 - lam_init, None, ALU.mult, None, [subw], [subw])
        lamt = Tl("lamt", [128, 256], F32)
        lams = Tl("lams", [128, 8], F32)
        DMA("sp", lamt[:], W["attn_lambda"][j:j + 1].rearrange("o a d -> o (a d)").partition_broadcast(128), [], [lamt])
        TT("dve", lamt[:, 0:64], lamt[:, 0:64], lamt[:, 64:128], ALU.mult, [lamt], [lamt])
        TT("dve", lamt[:, 128:192], lamt[:, 128:192], lamt[:, 192:256], ALU.mult, [lamt], [lamt])
        P.op("dve", lambda e: e.tensor_reduce(out=lams[:, 0:1], in_=lamt[:, 0:64], axis=AX.X, op=ALU.add), nrm([lamt]), nrm([lams]))
        P.op("dve", lambda e: e.tensor_reduce(out=lams[:, 1:2], in_=lamt[:, 128:192], axis=AX.X, op=ALU.add), nrm([lamt]), nrm([lams]))
        ACT(lams[:, 0:2], lams[:, 0:2], AF.Exp, [lams], [lams])
        TT("dve", lams[:, 2:3], lams[:, 0:1], lams[:, 1:2], ALU.subtract, [lams], [lams])
        TS("dve", lams[:, 2:3], lams[:, 2:3], lam_init, None, ALU.add, None, [lams], [lams])
        lam = lams[:, 2:3]
        cosT = Tl("cosT", [128, S], F32)
        sinT = Tl("sinT", [128, S], F32)
        DMA("sp", cosT[:], CD["ropecos"], [], [cosT])
        DMA("sp", sinT[:], CD["ropesin"], [], [sinT])
        scp = Tl("scp", [128, 1024], F32)
        shb = Tl("shb", [128, 1024], F32)
        gbc = Tl("gbc", [128, 1024], F32)
        xts = [Tl("xt%d" % i, [128, 1024], F32) for i in range(2)]
        work = Tl("work", [128, 1024], F32)
        rr = Tl("rr", [128, 1024], F32)
        stat = Tl("stat", [128, 32], F32)
        hTall = Tl("hTall", [128, 8, S], BF16)
        qT = Tl("aqT", [128, S], BF16)
        kT = Tl("akT", [128, S], BF16)
        vext = Tl("vext", [128, NT, 129], BF16)
        MS("pool", vext[:, :, 128:129], 1.0, [vext])
        wsl = {nm: Tl("aw_" + nm, [128, 8, 128], BF16) for nm in ("q", "qs", "k", "ks", "v")}
        t1 = Tl("rt1", [128, 512], F32)
        t2 = Tl("rt2", [128, 512], F32)
        pT = [[Tl("pT%d_%d" % (i, m), [128, 512], BF16) for m in range(2)] for i in range(2)]
        rs = Tl("ars", [128, 8], F32)
        ot = Tl("aot", [128, 128], F32)
        ot2 = Tl("aot2", [128, 128], F32)
        o1s = Tl("ao1s", [128, 4, 128], F32)
        junk = Tl("ajunk", [128, 128], F32)
        onb16 = [Tl("aon%d" % i, [128, 128], BF16) for i in range(2)]
        ont = Tl("aont", [128, 1024], BF16)
        onT = Tl("aonT", [128, 8, 128], BF16)
        win = W["attn_w_in"][j]
        nst = 0
        for s in range(NSEQ):
            load_mod_bc(scp, l, s, 1, plus1=True)
            load_mod_bc(shb, l, s, 0)
            load_mod_bc(gbc, l, s, 2)
            for t in range(NT):
                g = s * NT + t
                xt = xts[g % 2]
                DMA("sp", xt[:], src[g * 128:(g + 1) * 128, :], [srcb[g]], [xt])
                make_hT(xt, scp, shb, hTall[:, :, t * 128:(t + 1) * 128], hTall, TrV(banks[0]), work)
            for h in range(8):
                def wv3(c0, n):
                    return win[:, c0:c0 + n].rearrange("(kc p) n -> p kc n", p=128)
                DMA("pool", wsl["q"][:], wv3(h * 128, 128), [], [wsl["q"]])
                DMA("pool", wsl["k"][:], wv3(1024 + h * 128, 128), [], [wsl["k"]])
                DMA("pool", wsl["v"][:], wv3(2048 + h * 128, 128), [], [wsl["v"]])
                for nm, base in (("qs", 0), ("ks", 1024)):
                    for m in range(2):
                        b0 = base + h * 128 + m * 64
                        DMA("pool", wsl[nm][:, :, m * 64:m * 64 + 32], wv3(b0 + 32, 32), [], [wsl[nm]])
                        DMA("pool", wsl[nm][:, :, m * 64 + 32:m * 64 + 64], wv3(b0, 32), [], [wsl[nm]])
                for nb in range(NSB):
                    cs = slice(nb * QB, (nb + 1) * QB)
                    for (wn, wsn, dstT, bi) in (("q", "qs", qT, 2), ("k", "ks", kT, 2)):
                        pa, pb = banks[bi], banks[bi + 1]
                        for kc in range(8):
                            MM(pa[:, 0:QB], wsl[wn][:, kc, :], hTall[:, kc, cs], kc == 0, kc == 7, [wsl[wn], hTall], [pa])
                        for kc in range(8):
                            MM(pb[:, 0:QB], wsl[wsn][:, kc, :], hTall[:, kc, cs], kc == 0, kc == 7, [wsl[wsn], hTall], [pb])
                        TT("dve", t1[:, 0:QB], pa[:, 0:QB], cosT[:, cs], ALU.mult, [pa, cosT], [t1])
                        TT("dve", t2[:, 0:QB], pb[:, 0:QB], sinT[:, cs], ALU.mult, [pb, sinT], [t2])
                        TT("pool", dstT[:, cs], t1[:, 0:QB], t2[:, 0:QB], ALU.add, [t1, t2], [dstT])
                for t in range(NT):
                    pv = banks[4 + (t % 2)]
                    for kc in range(8):
                        MM(pv[:, 0:128], hTall[:, kc, t * 128:(t + 1) * 128], wsl["v"][:, kc, :], kc == 0, kc == 7, [hTall, wsl["v"]], [pv])
                    CP("dve", vext[:, t, 0:128], pv[:, 0:128], [pv], [vext])
                for Q in range(NSB):
                    nkb = Q * NQB + NQB
                    for m in range(2):
                        for kb in range(nkb):
                            j0 = max(0, kb - Q * NQB)
                            csl = slice(j0 * 128, QB)
                            stb = banks[nst % 2]
                            pt = pT[nst % 2][0]
                            nst += 1
                            MM(stb[:, csl], kT[m * 64:(m + 1) * 64, kb * 128:(kb + 1) * 128], qT[m * 64:(m + 1) * 64, Q * QB + j0 * 128:(Q + 1) * QB],
                               True, True, [kT, qT], [stb])
                            ACT(pt[:, csl], stb[:, csl], AF.Exp, [stb], [pt], scale=0.125)
                            if kb >= Q * NQB:
                                dsl = slice(j0 * 128, (j0 + 1) * 128)
                                TT("pool", pt[:, dsl], pt[:, dsl], trimask[:], ALU.mult, [pt, trimask], [pt])
                            for jq in range(j0, NQB):
                                ab = banks[4 + jq]
                                MM(ab[:, 0:129], pt[:, jq * 128:(jq + 1) * 128], vext[:, kb, :], kb == 0, kb == Q * NQB + jq, [pt, vext], [ab])
                        for jq in range(NQB):
                            ab = banks[4 + jq]
                            if m == 0:
                                P.op("dve", lambda e, ab=ab: e.reciprocal(out=rs[:, 0:1], in_=ab[:, 128:129]), nrm([ab]), nrm([rs]))
                                TS("dve", o1s[:, jq, :], ab[:, 0:128], rs[:, 0:1], None, ALU.mult, None, [ab, rs], [o1s])
                            else:
                                t = Q * NQB + jq
                                g = s * NT + t
                                P.op("dve", lambda e, ab=ab: e.reciprocal(out=rs[:, 1:2], in_=ab[:, 128:129]), nrm([ab]), nrm([rs]))
                                TS("dve", rs[:, 1:2], rs[:, 1:2], lam, None, ALU.mult, None, [rs, lams], [rs])
                                TS("dve", ot2[:], ab[:, 0:128], rs[:, 1:2], None, ALU.mult, None, [ab, rs], [ot2])
                                TT("dve", ot[:], o1s[:, jq, :], ot2[:], ALU.subtract, [o1s, ot2], [ot])
                                ACT(junk[:], ot[:], AF.Square, [ot], [junk, rs], accum_out=rs[:, 2:3])
                                ACT(rs[:, 3:4], rs[:, 2:3], AF.Sqrt, [rs], [rs], scale=1.0 / 128.0, bias=EPS)
                                P.op("dve", lambda e: e.reciprocal(out=rs[:, 3:4], in_=rs[:, 3:4]), nrm([rs]), nrm([rs]))
                                ob = onb16[jq % 2]
                                STT(ob[:], ot[:], rs[:, 3:4], subw[:], ALU.mult, ALU.mult, [ot, rs, subw], [ob])
                                DMA("sp", on_d[g * 128:(g + 1) * 128, h * 128:(h + 1) * 128], ob[:], [ob], [onb[g]])
            for t in range(NT):
                g = s * NT + t
                xt = xts[g % 2]
                DMA("sp", xt[:], src[g * 128:(g + 1) * 128, :], [srcb[g]], [xt])
                DMA("sp", ont[:], on_d[g * 128:(g + 1) * 128, :], [onb[g]], [ont])
                tb = banks[1]
                tbv = tb[:].bitcast(BF16).rearrange("p (h t) -> p h t", t=128)
                for h in range(8):
                    TR(tbv[:, h, :], ont[:, h * 128:(h + 1) * 128], identb[:], [ont, identb], [tb])
                CP("dve", onT[:, 0:4, :], tbv[:, 0:4, :], [tb], [onT])
                ACT(onT[:, 4:8, :], tbv[:, 4:8, :], AF.Copy, [tb], [onT])
                for half in range(2):
                    yb = banks[2 + half]
                    for h in range(8):
                        MM(yb[:], onT[:, h, :], w_out[:, h, half * 512:(half + 1) * 512], h == 0, h == 7, [onT, w_out], [yb])
                resid_ln(xt, [(banks[2][:], banks[2]), (banks[3][:], banks[3])], gbc, lng, lnb, rr, stat, dst[g * 128:(g + 1) * 128, :], dstb[g])
        P.pop()

    P.push()
    ztile = Tl("ztile", [128, 8192], BF16)
    MS("pool", ztile[:], 0.0, [ztile])
    zb = Buf("xbufzero")
    nrows = MOE_NBLK * 512
    r0 = 0
    while r0 < nrows:
        nr = min(1024, nrows - r0)
        DMA("sp", moe_xbuf_d[r0:r0 + nr, :].rearrange("(p j) d -> p (j d)", p=128), ztile[:, 0:(nr // 128) * 1024], [ztile], [zb])
        r0 += nr
    P.pop()
    phase_mod()
    P.barrier()
    cur, curb = x_d, xb["x"]
    for li, l in enumerate(layers):
        last = (li == len(layers) - 1)
        if "mix" in sub:
            dst, dstb = (out_d, xb["out"]) if (last and "moe" not in sub) else (xs[0], xb[0])
            if l % 2 == 0:
                phase_hgrn(l, cur, curb, dst, dstb)
            else:
                phase_attn(l, cur, curb, dst, dstb)
            cur, curb = dst, dstb
        if "moe" in sub:
            dst, dstb = (out_d, xb["out"]) if last else (xs[1], xb[1])
            phase_moe(l, cur, curb, dst, dstb)
            cur, curb = dst, dstb
    P.finish()
    P.DBG = DBG
    return nc, consts, P


_CACHE = {}


def run(inputs, NSEQ, S, layers, sub=("mix", "moe"), n_cores=8, NE=32):
    from concourse.bass_utils import run_bass_kernel_spmd
    nc, consts, P = build(NSEQ, S, layers, sub, NE)
    x = np.ascontiguousarray(inputs["x"], dtype=np.float32).reshape(n_cores, NSEQ * S, D)
    c = np.ascontiguousarray(inputs["c"], dtype=np.float32).reshape(n_cores, NSEQ, D)
    inputs = dict(inputs)
    rw = np.concatenate([np.asarray(inputs["router_g_w"], np.float32), np.asarray(inputs["router_e_w"], np.float32)], axis=2)
    inputs["router_w"] = np.ascontiguousarray(rw.reshape(rw.shape[0], 8, 128, 36).transpose(0, 2, 1, 3))
    inputs["router_b"] = np.concatenate([np.asarray(inputs["router_g_b"], np.float32), np.asarray(inputs["router_e_b"], np.float32)], axis=1)
    in_maps = []
    for i in range(n_cores):
        m = {"x": x[i], "cT": np.ascontiguousarray(c[i].reshape(NSEQ, 8, 128).transpose(2, 1, 0))}
        for nm, shp in wnames():
            m[nm] = np.ascontiguousarray(np.asarray(inputs[nm])[:shp[0]], dtype=np.float32)
        for nm, arr in consts.items():
            m[nm] = arr
        in_maps.append(m)
    res = run_bass_kernel_spmd(nc, in_maps, core_ids=list(range(n_cores)))
    out = np.stack([np.asarray(r["out"]) for r in res.results], 0)
    global LAST_DBG
    LAST_DBG = {k: np.asarray(res.results[0]["dbg_" + k]) for k in P.DBG}
    return out.reshape(n_cores * NSEQ, S, D)


def kernel(**inputs):
    out = run(inputs, 2, 4096, [0, 1, 2, 3])
    return out.astype(np.float32)
```

```python
import contextlib
import numpy as np
import concourse.bass as bass
import concourse.mybir as mybir

F32 = mybir.dt.float32
BF16 = mybir.dt.bfloat16
I32 = mybir.dt.int32
U32 = mybir.dt.uint32
AF = mybir.ActivationFunctionType
ALU = mybir.AluOpType
AX = mybir.AxisListType

QUEUES = ("pe", "act", "dve", "pool", "sp")
N_DMA_CH = 12


class Buf:
    __slots__ = ("name", "w", "r", "excl")

    def __init__(self, name="", excl=False):
        self.name = name
        self.excl = excl
        self.w = None
        self.r = []


class Prog:
    def __init__(self, nc, same_engine_sync=True):
        self.nc = nc
        self.stack = contextlib.ExitStack()
        self.ops = {q: [] for q in QUEUES}
        self.cnt = {q: 0 for q in QUEUES}
        self.sems = {}
        self.seen = {q: {} for q in QUEUES}
        self.same_engine_sync = same_engine_sync
        for q in QUEUES:
            self.sems["e_" + q] = self.stack.enter_context(nc.semaphore("e_" + q))
        self.dma_ch = {}
        self.dma_rr = {}
        for q in ("sp", "act", "pool"):
            chs = []
            for i in range(N_DMA_CH):
                key = "d_%s_%d" % (q, i)
                self.sems[key] = self.stack.enter_context(nc.semaphore(key))
                chs.append([key, 0])
            self.dma_ch[q] = chs
            self.dma_rr[q] = 0
        self.n_inst = 0
        self.uid = 0
        self.scopes = [self.stack]

    def sb(self, name, shape, dtype):
        self.uid += 1
        return self.scopes[-1].enter_context(self.nc.sbuf_tensor("sb%d_%s" % (self.uid, name), list(shape), dtype))

    def ps(self, name, shape, dtype=F32):
        self.uid += 1
        return self.scopes[-1].enter_context(self.nc.psum_tensor("ps%d_%s" % (self.uid, name), list(shape), dtype))

    def push(self):
        self.scopes.append(contextlib.ExitStack())

    def pop(self):
        self.barrier()
        self.scopes.pop().close()

    def barrier(self):
        targets = []
        for q in QUEUES:
            if self.cnt[q] > 0:
                targets.append(("e_" + q, self.cnt[q], q))
        for q, chs in self.dma_ch.items():
            for key, val in chs:
                if val > 0:
                    targets.append((key, val, None))
        sems = self.sems
        for q in QUEUES:
            seen = self.seen[q]
            waits = []
            for key, val, wq in targets:
                if wq == q and q != "sp":
                    continue
                if seen.get(key, 0) >= val:
                    continue
                seen[key] = val
                waits.append((key, val))

            def emit(eng, waits=waits):
                for k, v in waits:
                    eng.wait_ge(sems[k], v)

            self.ops[q].append(emit)
            self.n_inst += len(waits)

    def _collect(self, q, reads, writes, is_dma):
        waits = {}

        def need(tok):
            key, val, wq, wdma = tok
            if (not wdma) and (not is_dma) and wq == q:
                if q == "pe" or not self.same_engine_sync:
                    return
            if waits.get(key, 0) < val:
                waits[key] = val

        for b in reads:
            if b.w is not None:
                need(b.w)
        for b in writes:
            if b.w is not None:
                need(b.w)
            for t in b.r:
                need(t)
        out = []
        seen = self.seen[q]
        for key, val in waits.items():
            if seen.get(key, 0) >= val:
                continue
            seen[key] = val
            out.append((key, val))
        return out

    def _commit(self, tok, reads, writes):
        for b in reads:
            b.r.append(tok)
        for b in writes:
            b.w = tok
            b.r = []

    def op(self, q, fn, reads=(), writes=()):
        ex = [b for b in reads if b.excl]
        if ex:
            writes = list(writes) + [b for b in ex if b not in writes]
        waits = self._collect(q, reads, writes, False)
        self.cnt[q] += 1
        key = "e_" + q
        val = self.cnt[q]
        tok = (key, val, q, False)
        sems = self.sems

        def emit(eng, waits=waits, fn=fn, key=key):
            for k, v in waits:
                eng.wait_ge(sems[k], v)
            fn(eng).then_inc(sems[key], 1)

        self.ops[q].append(emit)
        self.n_inst += 1 + len(waits)
        self._commit(tok, reads, writes)
        return tok

    def dma(self, q, fn, reads=(), writes=()):
        waits = self._collect(q, reads, writes, True)
        chs = self.dma_ch[q]
        i = self.dma_rr[q]
        self.dma_rr[q] = (i + 1) % len(chs)
        ch = chs[i]
        key = ch[0]
        prev = ch[1]
        ch[1] += 16
        val = ch[1]
        seen = self.seen[q]
        if prev > 0 and seen.get(key, 0) < prev:
            seen[key] = prev
            waits = waits + [(key, prev)]
        tok = (key, val, q, True)
        sems = self.sems

        def emit(eng, waits=waits, fn=fn, key=key):
            for k, v in waits:
                eng.wait_ge(sems[k], v)
            fn(eng).then_inc(sems[key], 16)

        self.ops[q].append(emit)
        self.n_inst += 1 + len(waits)
        self._commit(tok, reads, writes)
        return tok

    def finish(self):
        nc = self.nc
        final = []
        for q, chs in self.dma_ch.items():
            for key, val in chs:
                if val > 0:
                    final.append((key, val))
        for q in QUEUES:
            if q != "sp" and self.cnt[q] > 0:
                final.append(("e_" + q, self.cnt[q]))
        sems = self.sems
        ops = self.ops
        with nc.Block() as block:
            @block.tensor
            def _(eng):
                for f in ops["pe"]:
                    f(eng)

            @block.scalar
            def _(eng):
                for f in ops["act"]:
                    f(eng)

            @block.vector
            def _(eng):
                for f in ops["dve"]:
                    f(eng)

            @block.gpsimd
            def _(eng):
                for f in ops["pool"]:
                    f(eng)

            @block.sync
            def _(eng):
                for f in ops["sp"]:
                    f(eng)
                for k, v in final:
                    eng.wait_ge(sems[k], v)
        self.stack.close()

DEBUG = False
MOE_STAGE = 2
MOE_CUT = 5
D = 1024
DEPTH = 4
ALPHA = (2 * DEPTH) ** 0.25
EPS = 1e-5
WDEPTH = 4


def wnames():
    L = WDEPTH
    return [("ada_w", [L, 1024, 6144]), ("ada_b", [L, 6144]), ("ln_g", [L, 2, 1024]), ("ln_b", [L, 2, 1024]),
            ("hgrn_w_in", [2, 1024, 4096]), ("hgrn_w_out", [2, 1024, 1024]), ("hgrn_lb", [2, 1024]),
            ("hgrn_norm_w", [2, 128]), ("attn_w_in", [2, 1024, 3072]), ("attn_w_out", [2, 1024, 1024]),
            ("attn_lambda", [2, 4, 64]), ("attn_subln_w", [2, 128]), ("router_w", [L, 128, 8, 36]), ("router_b", [L, 36]),
            ("moe_w1", [L, 32, 1024, 512]), ("moe_w3", [L, 32, 1024, 512]), ("moe_w2", [L, 32, 512, 1024])]


def make_consts(S):
    c = {}
    c["identf"] = np.eye(128, dtype=np.float32)
    s = np.arange(128)[:, None]
    t = np.arange(128)[None, :]
    c["blockmask"] = ((s // 32 == t // 32) & (s <= t)).astype(np.float32)
    c["trimask"] = (s <= t).astype(np.float32)
    sm = np.ones((128, 128), np.float32)
    sm[:, ::32] = 0.0
    c["scanmask"] = sm
    ci = np.zeros((128, 4), np.float32)
    for k in range(4):
        ci[k * 32:(k + 1) * 32, k] = 1.0
    c["chunkind"] = ci
    half = 32
    inv_freq = (np.float32(10000.0) ** (-np.arange(half, dtype=np.float32) / np.float32(half))).astype(np.float32)
    ang = (np.arange(S, dtype=np.float32)[:, None] * inv_freq[None, :]).astype(np.float32)
    cos = np.cos(ang).astype(np.float32).T
    sin = np.sin(ang).astype(np.float32).T
    c["utri"] = (s < t).astype(np.float32)
    wi = np.zeros((128, 12), np.float32)
    for kc in range(8):
        wi[:, kc] = kc * 128 + np.arange(128)
    for fc in range(4):
        wi[:, 8 + fc] = fc * 128 + np.arange(128)
    c["widx"] = wi
    c["bstart"] = (np.arange(128, dtype=np.float32) * 512.0)[None, :]
    c["ropecos"] = np.ascontiguousarray(np.concatenate([cos, cos, cos, cos], 0))
    c["ropesin"] = np.ascontiguousarray(np.concatenate([-sin, sin, -sin, sin], 0))
    return c


def build(NSEQ, S, layers, sub=("mix", "moe"), NE=32):
    NT = S // 128
    NTOK = NSEQ * S
    nc = bass.Bass("TRN2", target_bir_lowering=False)
    dtn = nc.dram_tensor
    x_d = dtn("x", [NTOK, D], F32, kind="ExternalInput").ap()
    c_d = dtn("cT", [128, 8, NSEQ], F32, kind="ExternalInput").ap()
    W = {}
    for nm, shp in wnames():
        W[nm] = dtn(nm, shp, F32, kind="ExternalInput").ap()
    consts = make_consts(S)
    CD = {}
    for nm, arr in consts.items():
        CD[nm] = dtn(nm, list(arr.shape), F32, kind="ExternalInput").ap()
    out_d = dtn("out", [NTOK, D], F32, kind="ExternalOutput").ap()
    xs = [dtn("xs0", [NTOK, D], F32, kind="Internal").ap(), dtn("xs1", [NTOK, D], F32, kind="Internal").ap()]
    mod_d = dtn("mod_d", [WDEPTH, NSEQ, 6144], F32, kind="Internal").ap()
    on_d = dtn("on_d", [NTOK, D], BF16, kind="Internal").ap()
    MOE_NBLK = (2 * NTOK) // 512 + 32
    moe_h2_d = dtn("moe_h2", [NTOK, D], BF16, kind="Internal").ap()
    moe_xbuf_d = dtn("moe_xbuf", [MOE_NBLK * 512, D], BF16, kind="Internal").ap()
    moe_ybuf_d = dtn("moe_ybuf", [MOE_NBLK * 512, D], BF16, kind="Internal").ap()

    P = Prog(nc)
    DBG = {}

    def dbg(name, ap, bufs, psum=False):
        if not DEBUG or name in DBG:
            return
        shp = list(ap.shape)
        d = dtn("dbg_" + name, shp, F32, kind="ExternalOutput").ap()
        DBG[name] = d
        if psum:
            tmp = P.sb("dbgtmp_" + name, shp, F32)
            tb = Buf()
            P.op("dve", lambda e: e.tensor_copy(out=tmp[:], in_=ap), nrm(bufs), [tb])
            P.dma("pool", lambda e: e.dma_start(out=d, in_=tmp[:]), [tb], [])
        else:
            P.dma("pool", lambda e: e.dma_start(out=d, in_=ap), nrm(bufs), [])

    def nrm(l):
        return [b.b if hasattr(b, "b") else b for b in l]

    def MM(out, lhsT, rhs, start, stop, r, w):
        P.op("pe", lambda e: e.matmul(out, lhsT=lhsT, rhs=rhs, start=start, stop=stop), nrm(r), nrm(w))

    def TR(out, in_, ident, r, w):
        P.op("pe", lambda e: e.transpose(out=out, in_=in_, identity=ident), nrm(r), nrm(w))

    def ACT(out, in_, func, r, w, **kw):
        P.op("act", lambda e: e.activation(out=out, in_=in_, func=func, **kw), nrm(r), nrm(w))

    def TT(q, out, in0, in1, op, r, w):
        P.op(q, lambda e: e.tensor_tensor(out=out, in0=in0, in1=in1, op=op), nrm(r), nrm(w))

    def TS(q, out, in0, s1, s2, op0, op1, r, w):
        if op1 is None:
            P.op(q, lambda e: e.tensor_scalar(out=out, in0=in0, scalar1=s1, scalar2=None, op0=op0), nrm(r), nrm(w))
        else:
            P.op(q, lambda e: e.tensor_scalar(out=out, in0=in0, scalar1=s1, scalar2=s2, op0=op0, op1=op1), nrm(r), nrm(w))

    def STT(out, in0, scalar, in1, op0, op1, r, w):
        P.op("dve", lambda e: e.scalar_tensor_tensor(out=out, in0=in0, scalar=scalar, in1=in1, op0=op0, op1=op1), nrm(r), nrm(w))

    def CP(q, out, in_, r, w):
        P.op(q, lambda e: e.tensor_copy(out=out, in_=in_), nrm(r), nrm(w))

    def MS(q, ap, val, w):
        P.op(q, lambda e: e.memset(ap, val), [], nrm(w))

    def DMA(q, out, in_, r, w, **kw):
        P.dma(q, lambda e: e.dma_start(out=out, in_=in_, **kw), nrm(r), nrm(w))

    class Tl:
        def __init__(self, name, shape, dtype, psum=False):
            self.t = P.ps(name, shape, dtype) if psum else P.sb(name, shape, dtype)
            self.b = Buf(name, excl=psum)

        def __getitem__(self, k):
            return self.t[k]

    xb = {"x": [Buf() for _ in range(NSEQ * NT)], 0: [Buf() for _ in range(NSEQ * NT)],
          1: [Buf() for _ in range(NSEQ * NT)], "out": [Buf() for _ in range(NSEQ * NT)]}
    modb = Buf("mod_d")
    onb = [Buf() for _ in range(NSEQ * NT)]

    identf = Tl("identf", [128, 128], F32)
    identb = Tl("identb", [128, 128], BF16)
    blockmask = Tl("blockmask", [128, 128], F32)
    trimask = Tl("trimask", [128, 128], BF16)
    scanmask = Tl("scanmask", [128, 128], F32)
    chunkind = Tl("chunkind", [128, 4], F32)
    DMA("sp", identf[:], CD["identf"], [], [identf])
    DMA("pool", identb[:], CD["identf"], [], [identb])
    DMA("sp", blockmask[:], CD["blockmask"], [], [blockmask])
    DMA("pool", trimask[:], CD["trimask"], [], [trimask])
    DMA("sp", scanmask[:], CD["scanmask"], [], [scanmask])
    DMA("sp", chunkind[:], CD["chunkind"], [], [chunkind])

    def phase_mod():
        P.push()
        condT = Tl("condT", [128, 8, NSEQ], F32)
        DMA("sp", condT[:], c_d, [], [condT])
        ACT(condT[:], condT[:], AF.Silu, [condT], [condT])
        stg = [Tl("adastg%d" % i, [128, 8, 512], F32) for i in range(2)]
        adab = Tl("adab", [NSEQ, 6144], F32)
        modrow = Tl("modrow", [NSEQ, 6144], F32)
        mps = [Tl("modps%d" % i, [128, 512], F32, psum=True) for i in range(2)]
        n = 0
        for l in layers:
            DMA("sp", adab[:], W["ada_b"][l:l + 1, :].partition_broadcast(NSEQ) if NSEQ > 1 else W["ada_b"][l:l + 1, :], [], [adab])
            for j in range(12):
                st = stg[n % 2]
                pp = mps[n % 2]
                n += 1
                DMA("sp", st[:], W["ada_w"][l, :, j * 512:(j + 1) * 512].rearrange("(kc p) n -> p kc n", p=128), [], [st])
                for kc in range(8):
                    MM(pp[0:NSEQ, :], condT[:, kc, :], st[:, kc, :], kc == 0, kc == 7, [condT, st], [pp])
                TT("dve", modrow[:, j * 512:(j + 1) * 512], pp[0:NSEQ, :], adab[:, j * 512:(j + 1) * 512], ALU.add, [pp, adab], [modrow])
            DMA("sp", mod_d[l], modrow[:], [modrow], [modb])
        P.pop()

    def load_mod_bc(tl, l, s, j, plus1=False):
        DMA("sp", tl[:], mod_d[l, s:s + 1, j * 1024:(j + 1) * 1024].partition_broadcast(128), [modb], [tl])
        if plus1:
            TS("pool", tl[:], tl[:], 1.0, None, ALU.add, None, [tl], [tl])

    def make_hT(xt, scp, sh, hT_out_ap, hTbuf, trp, work, hTf_ap=None, hTfbuf=None):
        TT("dve", work[:], xt[:], scp[:], ALU.mult, [xt, scp], [work])
        TT("pool", work[:], work[:], sh[:], ALU.add, [work, sh], [work])
        for half in range(2):
            for i in range(4):
                kc = half * 4 + i
                TR(trp[:, i, :], work[:, kc * 128:(kc + 1) * 128], identf[:], [work, identf], [trp])
            if half == 0:
                ACT(hT_out_ap[:, 0:4, :], trp[:], AF.Copy, [trp], [hTbuf])
            else:
                CP("dve", hT_out_ap[:, 4:8, :], trp[:], [trp], [hTbuf])
            if hTf_ap is not None:
                if half == 0:
                    CP("dve", hTf_ap[:, 0:4, :], trp[:], [trp], [hTfbuf])
                else:
                    ACT(hTf_ap[:, 4:8, :], trp[:], AF.Copy, [trp], [hTfbuf])

    def resid_ln(xt, yps_list, gbc, lng, lnb, r, stat, dst_ap, dst_buf):
        for half in range(2):
            yap, ybuf = yps_list[half]
            sl = slice(half * 512, (half + 1) * 512)
            TT("dve", r[:, sl], yap, gbc[:, sl], ALU.mult, [ybuf, gbc], [r])
        STT(r[:], xt[:], ALPHA, r[:], ALU.mult, ALU.add, [xt, r], [r])
        for c4 in range(2):
            P.op("dve", lambda e, c4=c4: e.bn_stats(out=stat[:, c4 * 6:(c4 + 1) * 6], in_=r[:, c4 * 512:(c4 + 1) * 512]), nrm([r]), nrm([stat]))
        P.op("dve", lambda e: e.bn_aggr(out=stat[:, 12:14], in_=stat[:, 0:12]), nrm([stat]), nrm([stat]))
        TS("dve", stat[:, 14:15], stat[:, 13:14], EPS, None, ALU.add, None, [stat], [stat])
        ACT(stat[:, 14:15], stat[:, 14:15], AF.Sqrt, [stat], [stat])
        P.op("dve", lambda e: e.reciprocal(out=stat[:, 15:16], in_=stat[:, 14:15]), nrm([stat]), nrm([stat]))
        STT(stat[:, 16:17], stat[:, 12:13], -1.0, stat[:, 15:16], ALU.mult, ALU.mult, [stat], [stat])
        ACT(r[:], r[:], AF.Identity, [r, stat], [r], scale=stat[:, 15:16], bias=stat[:, 16:17])
        TT("dve", r[:], r[:], lng[:], ALU.mult, [r, lng], [r])
        TT("pool", r[:], r[:], lnb[:], ALU.add, [r, lnb], [r])
        DMA("sp", dst_ap, r[:], [r], [dst_buf])

    def phase_hgrn(l, src, srcb, dst, dstb):
        j = l // 2
        P.push()
        w_in = Tl("hw_in", [128, 8, 4096], BF16)
        w_out = Tl("hw_out", [128, 8, 1024], BF16)
        for kc in range(8):
            DMA("pool", w_in[:, kc, :], W["hgrn_w_in"][j, kc * 128:(kc + 1) * 128, :], [], [w_in], max_dma_last_dim=4096)
        DMA("pool", w_out[:], W["hgrn_w_out"][j].rearrange("(kc p) n -> p kc n", p=128), [], [w_out], max_dma_last_dim=4096)
        lbraw = Tl("lbraw", [128, 2, 8], F32)
        lbc = Tl("lbc", [128, 8], F32)
        oml = Tl("oml", [128, 8], F32)
        DMA("sp", lbraw[:], W["hgrn_lb"].rearrange("j (h p) -> p j h", p=128), [], [lbraw], allow_slow_non_contiguous=True)
        if j == 0:
            TT("dve", lbc[:], lbraw[:, 0, :], lbraw[:, 0, :], ALU.subtract, [lbraw], [lbc])
        else:
            TT("dve", lbc[:], lbraw[:, 1, :], lbraw[:, 0, :], ALU.subtract, [lbraw], [lbc])
            ACT(lbc[:], lbc[:], AF.Sigmoid, [lbc], [lbc])
        TS("dve", oml[:], lbc[:], -1.0, 1.0, ALU.mult, ALU.add, [lbc], [oml])
        normw = Tl("normw", [128, 1024], F32)
        for h in range(8):
            DMA("sp", normw[:, h * 128:(h + 1) * 128], W["hgrn_norm_w"][j:j + 1, :].partition_broadcast(128), [], [normw])
        lng = Tl("lng", [128, 1024], F32)
        lnb = Tl("lnb", [128, 1024], F32)
        DMA("sp", lng[:], W["ln_g"][l, 0:1, :].partition_broadcast(128), [], [lng])
        DMA("sp", lnb[:], W["ln_b"][l, 0:1, :].partition_broadcast(128), [], [lnb])
        scp = Tl("scp", [128, 1024], F32)
        shb = Tl("shb", [128, 1024], F32)
        gbc = Tl("gbc", [128, 1024], F32)
        xts = [Tl("xt%d" % i, [128, 1024], F32) for i in range(2)]
        work = Tl("work", [128, 1024], F32)
        rr = Tl("rr", [128, 1024], F32)
        stat = Tl("stat", [128, 32], F32)
        hT = Tl("hT", [128, 8, 128], BF16)
        trp = Tl("trp", [128, 4, 128], F32, psum=True)
        qz = [Tl("qzps%d" % i, [128, 4, 128], F32, psum=True) for i in range(2)]
        qzb = [[Buf(), Buf()], [Buf(), Buf()]]
        vg = [Tl("vgps%d" % i, [128, 512], F32, psum=True) for i in range(2)]
        hd = [Tl("hdps%d" % i, [128, 4, 128], F32, psum=True) for i in range(2)]
        hdb = [[Buf() for _ in range(4)] for _ in range(2)]
        ktv = [qz[i][:, 2, :].bitcast(BF16)[:, 0:128] for i in range(2)]
        ktb = [Buf(), Buf()]

        class OnV:
            b = trp.b
            v = trp[:].bitcast(BF16).rearrange("p a (b c) -> p (a b) c", c=128)

            def __getitem__(self, k):
                return self.v[k]
        ontp = OnV()
        vsb = Tl("vsb", [128, 1024], BF16)
        gw = Tl("gw", [128, 1024], F32)
        sig = [Tl("sig%d" % i, [128, 128], F32) for i in range(2)]
        lf = [Tl("lf%d" % i, [128, 128], F32) for i in range(2)]
        kk = [Tl("kk%d" % i, [128, 128], F32) for i in range(2)]
        bb = [Tl("bb%d" % i, [128, 128], F32) for i in range(2)]
        Ep = [Tl("Ep%d" % i, [128, 128], F32) for i in range(2)]
        Em = [Tl("Em%d" % i, [128, 128], F32) for i in range(2)]
        qT = [Tl("qT%d" % i, [128, 128], BF16) for i in range(2)]
        kT = [Tl("kT%d" % i, [128, 128], BF16) for i in range(2)]
        AT = [Tl("AT%d" % i, [128, 128], BF16) for i in range(2)]
        qpad = [Tl("qpad%d" % i, [128, 640], BF16) for i in range(2)]
        kmask = [Tl("kmask%d" % i, [128, 4, 128], BF16) for i in range(2)]
        kmb = [[Buf() for _ in range(4)] for _ in range(2)]
        s1 = [Tl("s1_%d" % i, [128, 128], F32) for i in range(2)]
        ss = Tl("ssq", [128, 16], F32)
        junk = Tl("junk", [128, 128], F32)
        on_all = Tl("on_all", [128, 8, 128], BF16)
        onT = Tl("onT", [128, 8, 128], BF16)
        state = [[Tl("st_%d_%d" % (s, h), [128, 128], F32) for h in range(8)] for s in range(NSEQ)]
        stbf = [[Tl("stb_%d_%d" % (s, h), [128, 128], BF16) for h in range(8)] for s in range(NSEQ)]
        for i in range(2):
            MS("pool", qpad[i][:], 0.0, [qpad[i]])
        for s in range(NSEQ):
            for h in range(8):
                MS("pool", state[s][h][:], 0.0, [state[s][h]])
                MS("pool", stbf[s][h][:], 0.0, [stbf[s][h]])
        it = 0
        for s in range(NSEQ):
            load_mod_bc(scp, l, s, 1, plus1=True)
            load_mod_bc(shb, l, s, 0)
            load_mod_bc(gbc, l, s, 2)
            for t in range(NT):
                g = s * NT + t
                xt = xts[g % 2]
                DMA("sp", xt[:], src[g * 128:(g + 1) * 128, :], [srcb[g]], [xt])
                make_hT(xt, scp, shb, hT, hT, trp, work)
                for cch in range(4):
                    pp = vg[cch % 2]
                    for kc in range(8):
                        MM(pp[:], hT[:, kc, :], w_in[:, kc, 2048 + cch * 512:2048 + (cch + 1) * 512], kc == 0, kc == 7, [hT, w_in], [pp])
                    if cch < 2:
                        CP("dve", vsb[:, cch * 512:(cch + 1) * 512], pp[:], [pp], [vsb])
                    else:
                        ACT(gw[:, (cch - 2) * 512:(cch - 1) * 512], pp[:], AF.Silu, [pp], [gw])
                TT("pool", gw[:], gw[:], normw[:], ALU.mult, [gw, normw], [gw])
                def head_gen(h, p2, s=s):
                    qps, zps = qz[p2][:, 0, :], qz[p2][:, 1, :]
                    qb_, zb_ = qzb[p2]
                    for kc in range(8):
                        MM(qps, w_in[:, kc, h * 128:(h + 1) * 128], hT[:, kc, :], kc == 0, kc == 7, [hT, w_in], [qb_])
                    for kc in range(8):
                        MM(zps, w_in[:, kc, 1024 + h * 128:1024 + (h + 1) * 128], hT[:, kc, :], kc == 0, kc == 7, [hT, w_in], [zb_])
                    yield
                    ACT(sig[p2][:], zps, AF.Sigmoid, [zb_], [sig[p2]])
                    yield
                    TS("dve", sig[p2][:], sig[p2][:], oml[:, h:h + 1], lbc[:, h:h + 1], ALU.mult, ALU.add, [sig[p2], oml, lbc], [sig[p2]])
                    yield
                    ACT(lf[p2][:], sig[p2][:], AF.Ln, [sig[p2]], [lf[p2]])
                    dbg('f', sig[p2][:], [sig[p2]]); dbg('lf', lf[p2][:], [lf[p2]])
                    TS("pool", kk[p2][:], sig[p2][:], -1.0, 1.0, ALU.mult, ALU.add, [sig[p2]], [kk[p2]])
                    yield
                    P.op("dve", lambda e, p2=p2: e.tensor_tensor_scan(out=bb[p2][:], data0=scanmask[:], data1=lf[p2][:], initial=0.0,
                                                                     op0=ALU.mult, op1=ALU.add), nrm([scanmask, lf[p2]]), nrm([bb[p2]]))
                    yield
                    ACT(Ep[p2][:], bb[p2][:], AF.Exp, [bb[p2]], [Ep[p2]])
                    ACT(Em[p2][:], bb[p2][:], AF.Exp, [bb[p2]], [Em[p2]], scale=-1.0)
                    yield
                    TT("dve", qT[p2][:], qps, Ep[p2][:], ALU.mult, [qb_, Ep[p2]], [qT[p2]])
                    CP("pool", qpad[p2][:].rearrange("p (c x) -> p c x", x=160)[:, :, 0:32],
                       qT[p2][:].rearrange("p (c j) -> p c j", j=32), [qT[p2]], [qpad[p2]])
                    TT("pool", kT[p2][:], kk[p2][:], Em[p2][:], ALU.mult, [kk[p2], Em[p2]], [kT[p2]])
                    yield
                    dbg('bb', bb[p2][:], [bb[p2]]); dbg('qT', qT[p2][:], [qT[p2]]); dbg('kT', kT[p2][:], [kT[p2]]); dbg('qpad', qpad[p2][:], [qpad[p2]])
                    stp, ops_, up = hd[p2][:, 0, :], vg[p2][:, 0:128], [hd[p2][:, 2, :], hd[p2][:, 3, :]]
                    stb_, ob_, ub_ = hdb[p2][0], vg[p2], [hdb[p2][2], hdb[p2][3]]
                    MM(stp, kT[p2][:], qT[p2][:], True, True, [kT[p2], qT[p2]], [stb_])
                    TR(ktv[p2], kT[p2][:], identb[:], [kT[p2], identb], [ktb[p2]])
                    yield
                    TT("dve", AT[p2][:], stp, blockmask[:], ALU.mult, [stb_, blockmask], [AT[p2]])
                    for c in range(4):
                        ACT(kmask[p2][:, c, :], ktv[p2], AF.Copy, [ktb[p2], chunkind], [kmb[p2][c]], scale=chunkind[:, c:c + 1])
                    yield
                    vh = vsb[:, h * 128:(h + 1) * 128]
                    MM(ops_, AT[p2][:], vh, True, False, [AT[p2], vsb], [ob_])
                    yield
                    stt, stb16 = state[s][h], stbf[s][h]
                    for c in range(4):
                        MM(ops_, qpad[p2][:, c * 128:(c + 1) * 128], stb16[:], False, c == 3, [qpad[p2], stb16], [ob_])
                        MM(up[c % 2], kmask[p2][:, c, :], vh, True, True, [kmb[p2][c], vsb], [ub_[c % 2]])
                        yield
                        ebl = Ep[p2][:, 32 * c + 31:32 * c + 32]
                        TS("pool", s1[p2][:], stt[:], ebl, None, ALU.mult, None, [stt, Ep[p2]], [s1[p2]])
                        yield
                        STT(stt[:], up[c % 2], ebl, s1[p2][:], ALU.mult, ALU.add, [ub_[c % 2], Ep[p2], s1[p2]], [stt])
                        yield
                        ACT(stb16[:], stt[:], AF.Copy, [stt], [stb16])
                        yield
                    dbg('AT', AT[p2][:], [AT[p2]]); dbg('ops', ops_, [ob_], psum=True); dbg('kmask', kmask[p2][:], kmb[p2]); dbg('state', stt[:], [stt])
                    ACT(junk[:], ops_, AF.Square, [ob_], [junk, ss], accum_out=ss[:, h:h + 1])
                    yield
                    ACT(ss[:, 8 + h:9 + h], ss[:, h:h + 1], AF.Sqrt, [ss], [ss], scale=1.0 / 128.0, bias=EPS)
                    yield
                    P.op("dve", lambda e, h=h: e.reciprocal(out=ss[:, 8 + h:9 + h], in_=ss[:, 8 + h:9 + h]), nrm([ss]), nrm([ss]))
                    yield
                    STT(on_all[:, h, :], ops_, ss[:, 8 + h:9 + h], gw[:, h * 128:(h + 1) * 128], ALU.mult, ALU.mult, [ob_, ss, gw], [on_all])
                    yield
                    TR(ontp[:, h, :], on_all[:, h, :], identb[:], [on_all, identb], [ontp])
                for hp in range(0, 8, 2):
                    alive = [head_gen(hp, 0), head_gen(hp + 1, 1)]
                    while alive:
                        for gg in list(alive):
                            try:
                                next(gg)
                            except StopIteration:
                                alive.remove(gg)
                dbg('on_all', on_all[:], [on_all]); dbg('gw', gw[:], [gw]); dbg('vsb', vsb[:], [vsb]); dbg('hT', hT[:], [hT]); dbg('ss', ss[:], [ss])
                CP("dve", onT[:, 0:4, :], ontp[:, 0:4, :], [ontp], [onT])
                ACT(onT[:, 4:8, :], ontp[:, 4:8, :], AF.Copy, [ontp], [onT])
                for half in range(2):
                    for h in range(8):
                        MM(vg[half][:], onT[:, h, :], w_out[:, h, half * 512:(half + 1) * 512], h == 0, h == 7, [onT, w_out], [vg[half]])
                dbg('y0', vg[0][:], [vg[0]], psum=True)
                resid_ln(xt, [(vg[0][:], vg[0]), (vg[1][:], vg[1])], gbc, lng, lnb, rr, stat, dst[g * 128:(g + 1) * 128, :], dstb[g])
        P.pop()

    def phase_moe_dense(l, src, srcb, dst, dstb):
        P.push()
        GT = min(16, NT)
        NG = (NSEQ * NT) // GT
        SGT = min(4, GT)
        wr = Tl("wr", [128, 8, 36], F32)
        DMA("sp", wr[:], W["router_w"][l], [], [wr])
        rbias = Tl("rbias", [128, 36], F32)
        DMA("sp", rbias[:], W["router_b"][l:l + 1, :].partition_broadcast(128), [], [rbias])
        lng = Tl("lng", [128, 1024], F32)
        lnb = Tl("lnb", [128, 1024], F32)
        DMA("sp", lng[:], W["ln_g"][l, 1:2, :].partition_broadcast(128), [], [lng])
        DMA("sp", lnb[:], W["ln_b"][l, 1:2, :].partition_broadcast(128), [], [lnb])
        scp = Tl("scp", [128, 1024], F32)
        shb = Tl("shb", [128, 1024], F32)
        gbc = Tl("gbc", [128, 1024], F32)
        xts = [Tl("xt%d" % i, [128, 1024], F32) for i in range(2)]
        work = Tl("work", [128, 1024], F32)
        rr = Tl("rr", [128, 1024], F32)
        stat = Tl("stat", [128, 32], F32)
        h2T = Tl("h2T", [128, 8, GT * 128], BF16)
        h2Tf = Tl("h2Tf", [128, 8, 128], F32)
        yacc = Tl("yacc", [128, GT, 1024], F32)
        yaccb = [Buf() for _ in range(GT)]
        gates = Tl("gates", [128, GT, 32], F32)
        gT = [Tl("gT%d" % i, [128, 4, 512], BF16) for i in range(2)]
        slt = [Tl("slt%d" % i, [128, 512], F32) for i in range(2)]
        wb = [dict(w1=Tl("w1_%d" % i, [128, 8, 512], BF16), w3=Tl("w3_%d" % i, [128, 8, 512], BF16),
                   w2=Tl("w2_%d" % i, [128, 4, 1024], BF16)) for i in range(2)]
        trp = Tl("trp", [128, 4, 128], F32, psum=True)
        lgp = Tl("lgp", [128, 512], F32, psum=True)
        hp1 = [Tl("hp1_%d" % i, [128, 512], F32, psum=True) for i in range(2)]
        hp3 = [Tl("hp3_%d" % i, [128, 512], F32, psum=True) for i in range(2)]
        yp = [Tl("yp%d" % i, [128, 512], F32, psum=True) for i in range(2)]
        lg = Tl("lg", [128, 36], F32)
        sm = Tl("rsm", [128, 16], F32)
        oh = Tl("oh", [128, 4], F32)
        ejunk = Tl("ejunk", [128, 4], F32)
        m1 = Tl("m1", [128, 4], F32)
        m2 = Tl("m2", [128, 4], F32)
        dd = Tl("dd", [128, 4], F32)
        c1 = Tl("c1", [128, 4], F32)
        c2 = Tl("c2", [128, 4], F32)
        mk1 = Tl("mk1", [128, 4, 8], F32)
        mk2 = Tl("mk2", [128, 4, 8], F32)
        el2 = Tl("el2", [128, 4, 8], F32)

        def bc48(ap):
            return ap.unsqueeze(2).to_broadcast([128, 4, 8])

        for gidx in range(NG):
            g0 = gidx * GT
            s = g0 // NT
            load_mod_bc(scp, l, s, 4, plus1=True)
            load_mod_bc(shb, l, s, 3)
            load_mod_bc(gbc, l, s, 5)
            if MOE_CUT != -3:
                MS("pool", yacc[:], 0.0, yaccb)
            for i in range(GT):
                g = g0 + i
                xt = xts[g % 2]
                DMA("sp", xt[:], src[g * 128:(g + 1) * 128, :], [srcb[g]], [xt])
                if MOE_CUT >= -1:
                    make_hT(xt, scp, shb, h2T[:, :, i * 128:(i + 1) * 128], h2T, trp, work, h2Tf if MOE_CUT >= 0 else None, h2Tf)
                if MOE_CUT <= 0:
                    continue
                for kc in range(8):
                    MM(lgp[:, 0:36], h2Tf[:, kc, :], wr[:, kc, :], kc == 0, kc == 7, [h2Tf, wr], [lgp])
                TT("dve", lg[:], lgp[:, 0:36], rbias[:], ALU.add, [lgp, rbias], [lg])
                if MOE_CUT == 1:
                    continue
                gl = lg[:, 0:4]
                el = lg[:, 4:36].rearrange("p (g j) -> p g j", j=8)
                P.op("dve", lambda e, gl=gl: e.tensor_reduce(out=sm[:, 0:1], in_=gl, axis=AX.X, op=ALU.max), nrm([lg]), nrm([sm]))
                TS("dve", oh[:], gl, sm[:, 0:1], None, ALU.is_equal, None, [lg, sm], [oh])
                TS("dve", sm[:, 1:2], sm[:, 0:1], -1.0, None, ALU.mult, None, [sm], [sm])
                ACT(ejunk[:], gl, AF.Exp, [lg, sm], [ejunk, sm], bias=sm[:, 1:2], accum_out=sm[:, 2:3])
                P.op("dve", lambda e: e.reciprocal(out=sm[:, 3:4], in_=sm[:, 2:3]), nrm([sm]), nrm([sm]))
                P.op("dve", lambda e, el=el: e.tensor_reduce(out=m1[:], in_=el, axis=AX.X, op=ALU.max), nrm([lg]), nrm([m1]))
                TT("dve", mk1[:], el, bc48(m1[:]), ALU.is_equal, [lg, m1], [mk1])
                STT(el2[:], mk1[:], -1.0e30, el, ALU.mult, ALU.add, [mk1, lg], [el2])
                P.op("dve", lambda e: e.tensor_reduce(out=m2[:], in_=el2[:], axis=AX.X, op=ALU.max), nrm([el2]), nrm([m2]))
                TT("dve", mk2[:], el2[:], bc48(m2[:]), ALU.is_equal, [el2, m2], [mk2])
                TT("dve", dd[:], m2[:], m1[:], ALU.subtract, [m1, m2], [dd])
                ACT(dd[:], dd[:], AF.Exp, [dd], [dd])
                TS("dve", c1[:], dd[:], 1.0, None, ALU.add, None, [dd], [c1])
                P.op("dve", lambda e: e.reciprocal(out=c1[:], in_=c1[:]), nrm([c1]), nrm([c1]))
                TT("dve", c2[:], dd[:], c1[:], ALU.mult, [dd, c1], [c2])
                TS("dve", oh[:], oh[:], sm[:, 3:4], None, ALU.mult, None, [oh, sm], [oh])
                TT("dve", c1[:], c1[:], oh[:], ALU.mult, [c1, oh], [c1])
                TT("dve", c2[:], c2[:], oh[:], ALU.mult, [c2, oh], [c2])
                TT("dve", mk1[:], mk1[:], bc48(c1[:]), ALU.mult, [mk1, c1], [mk1])
                TT("dve", mk2[:], mk2[:], bc48(c2[:]), ALU.mult, [mk2, c2], [mk2])
                TT("dve", gates[:, i, :].rearrange("p (g j) -> p g j", j=8), mk1[:], mk2[:], ALU.add, [mk1, mk2], [gates])
            if DEBUG:
                dbg("gates", gates[:], [gates])
            nsub = 0
            for e in range(NE if MOE_STAGE >= 2 else 0):
                wbe = wb[e % 2]
                DMA("pool", wbe["w1"][:], W["moe_w1"][l, e].rearrange("(kc p) n -> p kc n", p=128), [], [wbe["w1"]])
                DMA("pool", wbe["w3"][:], W["moe_w3"][l, e].rearrange("(kc p) n -> p kc n", p=128), [], [wbe["w3"]])
                DMA("pool", wbe["w2"][:], W["moe_w2"][l, e].rearrange("(kc p) n -> p kc n", p=128), [], [wbe["w2"]], max_dma_last_dim=4096)
                for sg in range(GT // SGT):
                    ncol = SGT * 128
                    c0 = sg * ncol
                    gt_ = gT[nsub % 2]
                    nsub += 1
                    for fc in range(4):
                        a1, a3 = hp1[fc % 2], hp3[fc % 2]
                        for kc in range(8):
                            MM(a1[:, 0:ncol], wbe["w1"][:, kc, fc * 128:(fc + 1) * 128], h2T[:, kc, c0:c0 + ncol], kc == 0, kc == 7, [wbe["w1"], h2T], [a1])
                        for kc in range(8):
                            MM(a3[:, 0:ncol], wbe["w3"][:, kc, fc * 128:(fc + 1) * 128], h2T[:, kc, c0:c0 + ncol], kc == 0, kc == 7, [wbe["w3"], h2T], [a3])
                        sl = slt[fc % 2]
                        ACT(sl[:, 0:ncol], a1[:, 0:ncol], AF.Silu, [a1], [sl])
                        TT("dve", gt_[:, fc, 0:ncol], sl[:, 0:ncol], a3[:, 0:ncol], ALU.mult, [sl, a3], [gt_])
                    for ti in range(SGT):
                        i = sg * SGT + ti
                        for half in range(2):
                            ypp = yp[half]
                            for fc in range(4):
                                MM(ypp[:], gt_[:, fc, ti * 128:(ti + 1) * 128], wbe["w2"][:, fc, half * 512:(half + 1) * 512], fc == 0, fc == 3, [gt_, wbe["w2"]], [ypp])
                            ya = yacc[:, i, half * 512:(half + 1) * 512]
                            STT(ya, ypp[:], gates[:, i, e:e + 1], ya, ALU.mult, ALU.add, [ypp, gates, yaccb[i]], [yaccb[i]])
            for i in range(GT):
                g = g0 + i
                xt = xts[g % 2]
                DMA("sp", xt[:], src[g * 128:(g + 1) * 128, :], [srcb[g]], [xt])
                resid_ln(xt, [(yacc[:, i, 0:512], yaccb[i]), (yacc[:, i, 512:1024], yaccb[i])], gbc, lng, lnb, rr, stat,
                         dst[g * 128:(g + 1) * 128, :], dstb[g])
        P.pop()

    def phase_moe(l, src, srcb, dst, dstb):
        P.push()
        NTT = NSEQ * NT
        TB = 512
        NBLK = (2 * NTOK) // TB + 32
        wr = Tl("wr", [128, 8, 36], F32)
        DMA("sp", wr[:], W["router_w"][l], [], [wr])
        rbias = Tl("rbias", [128, 36], F32)
        DMA("sp", rbias[:], W["router_b"][l:l + 1, :].partition_broadcast(128), [], [rbias])
        lng = Tl("lng", [128, 1024], F32)
        lnb = Tl("lnb", [128, 1024], F32)
        DMA("sp", lng[:], W["ln_g"][l, 1:2, :].partition_broadcast(128), [], [lng])
        DMA("sp", lnb[:], W["ln_b"][l, 1:2, :].partition_broadcast(128), [], [lnb])
        widx_c = Tl("widx_c", [128, 12], F32)
        DMA("sp", widx_c[:], CD["widx"], [], [widx_c])
        utri = Tl("utri", [128, 128], BF16)
        ones = Tl("ones", [128, 128], BF16)
        DMA("pool", utri[:], CD["utri"], [], [utri])
        MS("pool", ones[:], 1.0, [ones])
        scp = Tl("scp", [128, 1024], F32)
        shb = Tl("shb", [128, 1024], F32)
        gbc = Tl("gbc", [128, 1024], F32)
        xts = [Tl("xt%d" % i, [128, 1024], F32) for i in range(2)]
        work = Tl("work", [128, 1024], F32)
        rr = Tl("rr", [128, 1024], F32)
        stat = Tl("stat", [128, 32], F32)
        h2Tf = Tl("h2Tf", [128, 8, 128], F32)
        h2b = [Tl("h2b%d" % i, [128, 1024], BF16) for i in range(2)]
        m1all = Tl("m1all", [128, NTT, 32], F32)
        m2all = Tl("m2all", [128, NTT, 32], F32)
        rkall = Tl("rkall", [128, NTT, 32], F32)
        wab = Tl("wab", [128, NTT, 2], F32)
        slots = Tl("slots", [128, NTT, 2], I32)
        cum = Tl("cum", [128, 32], F32)
        MS("pool", cum[:], 0.0, [cum])
        mb16 = Tl("mb16", [128, 32], BF16)
        bankA = [Tl("mbk%d" % i, [128, 512], F32, psum=True) for i in range(7)]
        trp = Tl("trp", [128, 4, 128], F32, psum=True)
        lgp, rkp, csp = bankA[0], bankA[1], bankA[2]
        lg = Tl("lg", [128, 36], F32)
        sm = Tl("rsm", [128, 16], F32)
        oh = Tl("oh", [128, 4], F32)
        ejunk = Tl("ejunk", [128, 4], F32)
        m1 = Tl("m1", [128, 4], F32)
        m2 = Tl("m2", [128, 4], F32)
        dd = Tl("dd", [128, 4], F32)
        c1 = Tl("c1", [128, 4], F32)
        c2 = Tl("c2", [128, 4], F32)
        mk1 = Tl("mk1", [128, 4, 8], F32)
        mk2 = Tl("mk2", [128, 4, 8], F32)
        el2 = Tl("el2", [128, 4, 8], F32)
        h2_d = moe_h2_d
        h2db = [Buf() for _ in range(NTT)]

        def bc48(ap):
            return ap.unsqueeze(2).to_broadcast([128, 4, 8])

        for g in range(NTT):
            s = g // NT
            if g % NT == 0:
                load_mod_bc(scp, l, s, 4, plus1=True)
                load_mod_bc(shb, l, s, 3)
                load_mod_bc(gbc, l, s, 5)
            xt = xts[g % 2]
            DMA("sp", xt[:], src[g * 128:(g + 1) * 128, :], [srcb[g]], [xt])
            TT("dve", work[:], xt[:], scp[:], ALU.mult, [xt, scp], [work])
            TT("pool", work[:], work[:], shb[:], ALU.add, [work, shb], [work])
            hb = h2b[g % 2]
            ACT(hb[:], work[:], AF.Copy, [work], [hb])
            DMA("sp", h2_d[g * 128:(g + 1) * 128, :], hb[:], [hb], [h2db[g]])
            for half in range(2):
                for i in range(4):
                    kc = half * 4 + i
                    TR(trp[:, i, :], work[:, kc * 128:(kc + 1) * 128], identf[:], [work, identf], [trp])
                if half == 0:
                    CP("dve", h2Tf[:, 0:4, :], trp[:], [trp], [h2Tf])
                else:
                    ACT(h2Tf[:, 4:8, :], trp[:], AF.Copy, [trp], [h2Tf])
            for kc in range(8):
                MM(lgp[:, 0:36], h2Tf[:, kc, :], wr[:, kc, :], kc == 0, kc == 7, [h2Tf, wr], [lgp])
            TT("dve", lg[:], lgp[:, 0:36], rbias[:], ALU.add, [lgp, rbias], [lg])
            gl = lg[:, 0:4]
            el = lg[:, 4:36].rearrange("p (g j) -> p g j", j=8)
            P.op("dve", lambda e, gl=gl: e.tensor_reduce(out=sm[:, 0:1], in_=gl, axis=AX.X, op=ALU.max), nrm([lg]), nrm([sm]))
            TS("dve", oh[:], gl, sm[:, 0:1], None, ALU.is_equal, None, [lg, sm], [oh])
            TS("dve", sm[:, 1:2], sm[:, 0:1], -1.0, None, ALU.mult, None, [sm], [sm])
            ACT(ejunk[:], gl, AF.Exp, [lg, sm], [ejunk, sm], bias=sm[:, 1:2], accum_out=sm[:, 2:3])
            P.op("dve", lambda e: e.reciprocal(out=sm[:, 3:4], in_=sm[:, 2:3]), nrm([sm]), nrm([sm]))
            P.op("dve", lambda e, el=el: e.tensor_reduce(out=m1[:], in_=el, axis=AX.X, op=ALU.max), nrm([lg]), nrm([m1]))
            TT("dve", mk1[:], el, bc48(m1[:]), ALU.is_equal, [lg, m1], [mk1])
            STT(el2[:], mk1[:], -1.0e30, el, ALU.mult, ALU.add, [mk1, lg], [el2])
            P.op("dve", lambda e: e.tensor_reduce(out=m2[:], in_=el2[:], axis=AX.X, op=ALU.max), nrm([el2]), nrm([m2]))
            TT("dve", mk2[:], el2[:], bc48(m2[:]), ALU.is_equal, [el2, m2], [mk2])
            TT("dve", dd[:], m2[:], m1[:], ALU.subtract, [m1, m2], [dd])
            ACT(dd[:], dd[:], AF.Exp, [dd], [dd])
            TS("dve", c1[:], dd[:], 1.0, None, ALU.add, None, [dd], [c1])
            P.op("dve", lambda e: e.reciprocal(out=c1[:], in_=c1[:]), nrm([c1]), nrm([c1]))
            TT("dve", c2[:], dd[:], c1[:], ALU.mult, [dd, c1], [c2])
            m1g = m1all[:, g, :].rearrange("p (g j) -> p g j", j=8)
            m2g = m2all[:, g, :].rearrange("p (g j) -> p g j", j=8)
            TT("dve", m1g, mk1[:], bc48(oh[:]), ALU.mult, [mk1, oh], [m1all])
            TT("dve", m2g, mk2[:], bc48(oh[:]), ALU.mult, [mk2, oh], [m2all])
            TT("dve", c1[:], c1[:], oh[:], ALU.mult, [c1, oh], [c1])
            TT("dve", c2[:], c2[:], oh[:], ALU.mult, [c2, oh], [c2])
            P.op("dve", lambda e: e.tensor_reduce(out=sm[:, 4:5], in_=c1[:], axis=AX.X, op=ALU.add), nrm([c1]), nrm([sm]))
            P.op("dve", lambda e: e.tensor_reduce(out=sm[:, 5:6], in_=c2[:], axis=AX.X, op=ALU.add), nrm([c2]), nrm([sm]))
            TS("dve", wab[:, g, :], sm[:, 4:6], sm[:, 3:4], None, ALU.mult, None, [sm], [wab])
            TT("dve", mb16[:], m1all[:, g, :], m2all[:, g, :], ALU.add, [m1all, m2all], [mb16])
            MM(rkp[:, 0:32], utri[:], mb16[:], True, True, [utri, mb16], [rkp])
            MM(csp[:, 0:32], ones[:], mb16[:], True, True, [ones, mb16], [csp])
            TT("dve", rkall[:, g, :], rkp[:, 0:32], cum[:], ALU.add, [rkp, cum], [rkall])
            TT("dve", cum[:], cum[:], csp[:, 0:32], ALU.add, [cum, csp], [cum])

        if MOE_CUT >= 2:
            pass
        pad = Tl("pad", [128, 32], F32)
        padi = Tl("padi", [128, 32], I32)
        pend = Tl("pend", [128, 32], F32)
        pstart = Tl("pstart", [128, 32], F32)
        onesf = Tl("onesf", [128, 32], F32)
        MS("pool", onesf[:], 1.0, [onesf])
        CP("dve", padi[:], cum[:], [cum], [padi])
        TS("dve", padi[:], padi[:], TB - 1, None, ALU.add, None, [padi], [padi])
        TS("dve", padi[:], padi[:], 9, None, ALU.arith_shift_right, None, [padi], [padi])
        TS("dve", padi[:], padi[:], 9, None, ALU.logical_shift_left, None, [padi], [padi])
        CP("dve", pad[:], padi[:], [padi], [pad])
        P.op("dve", lambda e: e.tensor_tensor_scan(out=pend[:], data0=onesf[:], data1=pad[:], initial=0.0, op0=ALU.mult, op1=ALU.add),
             nrm([onesf, pad]), nrm([pend]))
        TT("dve", pstart[:], pend[:], pad[:], ALU.subtract, [pend, pad], [pstart])
        bstart = Tl("bstart", [128, NBLK], F32)
        DMA("sp", bstart[:], CD["bstart"][0:1, 0:NBLK].partition_broadcast(128), [], [bstart])
        cmp_ = Tl("cmpb", [128, NBLK, 32], F32)
        bexp = Tl("bexp", [128, NBLK], F32)
        TT("dve", cmp_[:], pend[:].unsqueeze(1).to_broadcast([128, NBLK, 32]), bstart[:].unsqueeze(2).to_broadcast([128, NBLK, 32]),
           ALU.is_le, [pend, bstart], [cmp_])
        P.op("dve", lambda e: e.tensor_reduce(out=bexp[:], in_=cmp_[:], axis=AX.X, op=ALU.add), nrm([cmp_]), nrm([bexp]))
        TS("dve", bexp[:], bexp[:], 31.0, None, ALU.min, None, [bexp], [bexp])
        widf = Tl("widf", [128, NBLK, 12], F32)
        widi = Tl("widi", [128, NBLK, 12], I32)
        TS("dve", bexp[:], bexp[:], float(l * 32), None, ALU.add, None, [bexp], [bexp])
        STT(widf[:, :, 0:8], bexp[:].unsqueeze(2).to_broadcast([128, NBLK, 8]), 1024.0,
            widx_c[:, 0:8].unsqueeze(1).to_broadcast([128, NBLK, 8]), ALU.mult, ALU.add, [bexp, widx_c], [widf])
        STT(widf[:, :, 8:12], bexp[:].unsqueeze(2).to_broadcast([128, NBLK, 4]), 512.0,
            widx_c[:, 8:12].unsqueeze(1).to_broadcast([128, NBLK, 4]), ALU.mult, ALU.add, [bexp, widx_c], [widf])
        CP("dve", widi[:], widf[:], [widf], [widi])

        dtmp = Tl("dtmp", [128, 32], F32)
        dtmp2 = Tl("dtmp2", [128, 32], F32)
        slf = Tl("slf", [128, 2], F32)
        xbufb = [Buf() for _ in range(2 * NTT)]
        for g in range(NTT if MOE_CUT >= 3 else 0):
            TT("dve", dtmp[:], rkall[:, g, :], pstart[:], ALU.add, [rkall, pstart], [dtmp])
            TT("dve", dtmp2[:], dtmp[:], m1all[:, g, :], ALU.mult, [dtmp, m1all], [dtmp2])
            P.op("dve", lambda e: e.tensor_reduce(out=slf[:, 0:1], in_=dtmp2[:], axis=AX.X, op=ALU.add), nrm([dtmp2]), nrm([slf]))
            TT("dve", dtmp2[:], dtmp[:], m2all[:, g, :], ALU.mult, [dtmp, m2all], [dtmp2])
            P.op("dve", lambda e: e.tensor_reduce(out=slf[:, 1:2], in_=dtmp2[:], axis=AX.X, op=ALU.add), nrm([dtmp2]), nrm([slf]))
            CP("dve", slots[:, g, :], slf[:], [slf], [slots])
            hb = h2b[g % 2]
            DMA("sp", hb[:], h2_d[g * 128:(g + 1) * 128, :], [h2db[g]], [hb])
            for k in range(2):
                P.dma("pool", lambda e, g=g, k=k, hb=hb: e.indirect_dma_start(
                    out=moe_xbuf_d[:, :], out_offset=bass.IndirectOffsetOnAxis(ap=slots[:, g, k:k + 1], axis=0),
                    in_=hb[:], in_offset=None), nrm([hb, slots]), [xbufb[2 * g + k]])

        wb = [dict(w1=Tl("w1_%d" % i, [128, 8, 512], BF16), w3=Tl("w3_%d" % i, [128, 8, 512], BF16),
                   w2=Tl("w2_%d" % i, [128, 4, 1024], BF16)) for i in range(2)]
        xg = [Tl("xg%d" % i, [128, 4, 1024], BF16) for i in range(2)]
        xT = [Tl("xTb%d" % i, [128, 8, 512], BF16) for i in range(2)]
        gT = [Tl("gT%d" % i, [128, 4, 512], BF16) for i in range(2)]
        slt = [Tl("slt%d" % i, [128, 512], F32) for i in range(2)]
        yt = [Tl("ytb%d" % i, [128, 1024], BF16) for i in range(2)]
        tpb = bankA[0]
        tpv = tpb[:].bitcast(BF16).rearrange("p (r c) -> p r c", c=512)
        hp1 = [bankA[1], bankA[2]]
        hp3 = [bankA[3], bankA[4]]
        yp = [bankA[5], bankA[6]]
        ybufb = [Buf() for _ in range(NBLK)]
        w1rows = W["moe_w1"].rearrange("l e k f -> (l e k) f")
        w3rows = W["moe_w3"].rearrange("l e k f -> (l e k) f")
        w2rows = W["moe_w2"].rearrange("l e k f -> (l e k) f")
        nyt = 0
        for b in range(NBLK if MOE_CUT >= 4 else 0):
            wbe = wb[b % 2]
            for kc in range(8):
                P.dma("pool", lambda e, b=b, kc=kc, wbe=wbe: e.indirect_dma_start(
                    out=wbe["w1"][:, kc, :], out_offset=None, in_=w1rows,
                    in_offset=bass.IndirectOffsetOnAxis(ap=widi[:, b, kc:kc + 1], axis=0)), nrm([widi]), nrm([wbe["w1"]]))
                P.dma("pool", lambda e, b=b, kc=kc, wbe=wbe: e.indirect_dma_start(
                    out=wbe["w3"][:, kc, :], out_offset=None, in_=w3rows,
                    in_offset=bass.IndirectOffsetOnAxis(ap=widi[:, b, kc:kc + 1], axis=0)), nrm([widi]), nrm([wbe["w3"]]))
            for fc in range(4):
                P.dma("pool", lambda e, b=b, fc=fc, wbe=wbe: e.indirect_dma_start(
                    out=wbe["w2"][:, fc, :], out_offset=None, in_=w2rows,
                    in_offset=bass.IndirectOffsetOnAxis(ap=widi[:, b, 8 + fc:9 + fc], axis=0)), nrm([widi]), nrm([wbe["w2"]]))
            xgb = xg[b % 2]
            DMA("sp", xgb[:], moe_xbuf_d[b * TB:(b + 1) * TB, :].rearrange("(j p) d -> p j d", p=128), xbufb, [xgb])
            xTb = xT[b % 2]
            for kc in range(8):
                for jj in range(4):
                    TR(tpv[:, kc % 2, jj * 128:(jj + 1) * 128], xgb[:, jj, kc * 128:(kc + 1) * 128], identb[:], [xgb, identb], [tpb])
                if kc % 2 == 0:
                    CP("dve", xTb[:, kc, :], tpv[:, kc % 2, :], [tpb], [xTb])
                else:
                    ACT(xTb[:, kc, :], tpv[:, kc % 2, :], AF.Copy, [tpb], [xTb])
            gt_ = gT[b % 2]
            for fc in range(4):
                a1, a3 = hp1[fc % 2], hp3[fc % 2]
                for kc in range(8):
                    MM(a1[:], wbe["w1"][:, kc, fc * 128:(fc + 1) * 128], xTb[:, kc, :], kc == 0, kc == 7, [wbe["w1"], xTb], [a1])
                for kc in range(8):
                    MM(a3[:], wbe["w3"][:, kc, fc * 128:(fc + 1) * 128], xTb[:, kc, :], kc == 0, kc == 7, [wbe["w3"], xTb], [a3])
                sl = slt[fc % 2]
                ACT(sl[:], a1[:], AF.Silu, [a1], [sl])
                TT("dve", gt_[:, fc, :], sl[:], a3[:], ALU.mult, [sl, a3], [gt_])
            for ti in range(4):
                ytt = yt[nyt % 2]
                nyt += 1
                for half in range(2):
                    ypp = yp[half]
                    for fc in range(4):
                        MM(ypp[:], gt_[:, fc, ti * 128:(ti + 1) * 128], wbe["w2"][:, fc, half * 512:(half + 1) * 512], fc == 0, fc == 3, [gt_, wbe["w2"]], [ypp])
                    if half == 0:
                        ACT(ytt[:, 0:512], ypp[:], AF.Copy, [ypp], [ytt])
                    else:
                        CP("dve", ytt[:, 512:1024], ypp[:], [ypp], [ytt])
                r0 = b * TB + ti * 128
                DMA("sp", moe_ybuf_d[r0:r0 + 128, :], ytt[:], [ytt], [ybufb[b]])

        ya = [Tl("yga%d" % i, [128, 1024], BF16) for i in range(2)]
        yb_ = [Tl("ygb%d" % i, [128, 1024], BF16) for i in range(2)]
        ycomb = Tl("ycomb", [128, 1024], F32)
        for g in range(NTT):
            s = g // NT
            if g % NT == 0:
                load_mod_bc(gbc, l, s, 5)
            xt = xts[g % 2]
            DMA("sp", xt[:], src[g * 128:(g + 1) * 128, :], [srcb[g]], [xt])
            ga, gb_ = ya[g % 2], yb_[g % 2]
            if MOE_CUT < 5:
                MS("pool", ga[:], 0.0, [ga])
                MS("pool", gb_[:], 0.0, [gb_])
            for k, dstt in (((0, ga), (1, gb_)) if MOE_CUT >= 5 else ()):
                P.dma("pool", lambda e, g=g, k=k, dstt=dstt: e.indirect_dma_start(
                    out=dstt[:], out_offset=None, in_=moe_ybuf_d[:, :],
                    in_offset=bass.IndirectOffsetOnAxis(ap=slots[:, g, k:k + 1], axis=0)), nrm(ybufb + [slots]), nrm([dstt]))
            TS("dve", ycomb[:], ga[:], wab[:, g, 0:1], None, ALU.mult, None, [ga, wab], [ycomb])
            STT(ycomb[:], gb_[:], wab[:, g, 1:2], ycomb[:], ALU.mult, ALU.add, [gb_, wab, ycomb], [ycomb])
            resid_ln(xt, [(ycomb[:, 0:512], ycomb), (ycomb[:, 512:1024], ycomb)], gbc, lng, lnb, rr, stat,
                     dst[g * 128:(g + 1) * 128, :], dstb[g])
        P.pop()

    def phase_attn(l, src, srcb, dst, dstb):
        import math
        j = l // 2
        lam_init = 0.8 - 0.6 * math.exp(-0.3 * l)
        QB = min(512, S)
        NQB = QB // 128
        NSB = S // QB
        P.push()
        banks = [Tl("bk%d" % i, [128, 512], F32, psum=True) for i in range(8)]

        class TrV:
            def __init__(self, bank):
                self.b = bank.b
                self.v = bank[:].rearrange("p (i t) -> p i t", t=128)

            def __getitem__(self, k):
                return self.v[k]
        trp_t = banks[0]
        w_out = Tl("aw_out", [128, 8, 1024], BF16)
        DMA("pool", w_out[:], W["attn_w_out"][j].rearrange("(kc p) n -> p kc n", p=128), [], [w_out], max_dma_last_dim=4096)
        lng = Tl("lng", [128, 1024], F32)
        lnb = Tl("lnb", [128, 1024], F32)
        DMA("sp", lng[:], W["ln_g"][l, 0:1, :].partition_broadcast(128), [], [lng])
        DMA("sp", lnb[:], W["ln_b"][l, 0:1, :].partition_broadcast(128), [], [lnb])
        subw = Tl("subw", [128, 128], F32)
        DMA("sp", subw[:], W["attn_subln_w"][j:j + 1, :].partition_broadcast(128), [], [subw])
        TS("dve", subw[:], subw[:], 1.0 - lam_init, None, ALU.mult, None, [subw], [subw])
        lamt = Tl("lamt", [128, 256], F32)
        lams = Tl("lams", [128, 8], F32)
        DMA("sp", lamt[:], W["attn_lambda"][j:j + 1].rearrange("o a d -> o (a d)").partition_broadcast(128), [], [lamt])
        TT("dve", lamt[:, 0:64], lamt[:, 0:64], lamt[:, 64:128], ALU.mult, [lamt], [lamt])
        TT("dve", lamt[:, 128:192], lamt[:, 128:192], lamt[:, 192:256], ALU.mult, [lamt], [lamt])
        P.op("dve", lambda e: e.tensor_reduce(out=lams[:, 0:1], in_=lamt[:, 0:64], axis=AX.X, op=ALU.add), nrm([lamt]), nrm([lams]))
        P.op("dve", lambda e: e.tensor_reduce(out=lams[:, 1:2], in_=lamt[:, 128:192], axis=AX.X, op=ALU.add), nrm([lamt]), nrm([lams]))
        ACT(lams[:, 0:2], lams[:, 0:2], AF.Exp, [lams], [lams])
        TT("dve", lams[:, 2:3], lams[:, 0:1], lams[:, 1:2], ALU.subtract, [lams], [lams])
        TS("dve", lams[:, 2:3], lams[:, 2:3], lam_init, None, ALU.add, None, [lams], [lams])
        lam = lams[:, 2:3]
        cosT = Tl("cosT", [128, S], F32)
        sinT = Tl("sinT", [128, S], F32)
        DMA("sp", cosT[:], CD["ropecos"], [], [cosT])
        DMA("sp", sinT[:], CD["ropesin"], [], [sinT])
        scp = Tl("scp", [128, 1024], F32)
        shb = Tl("shb", [128, 1024], F32)
        gbc = Tl("gbc", [128, 1024], F32)
        xts = [Tl("xt%d" % i, [128, 1024], F32) for i in range(2)]
        work = Tl("work", [128, 1024], F32)
        rr = Tl("rr", [128, 1024], F32)
        stat = Tl("stat", [128, 32], F32)
        hTall = Tl("hTall", [128, 8, S], BF16)
        qT = Tl("aqT", [128, S], BF16)
        kT = Tl("akT", [128, S], BF16)
        vext = Tl("vext", [128, NT, 129], BF16)
        MS("pool", vext[:, :, 128:129], 1.0, [vext])
        wsl = {nm: Tl("aw_" + nm, [128, 8, 128], BF16) for nm in ("q", "qs", "k", "ks", "v")}
        t1 = Tl("rt1", [128, 512], F32)
        t2 = Tl("rt2", [128, 512], F32)
        pT = [[Tl("pT%d_%d" % (i, m), [128, 512], BF16) for m in range(2)] for i in range(2)]
        rs = Tl("ars", [128, 8], F32)
        ot = Tl("aot", [128, 128], F32)
        ot2 = Tl("aot2", [128, 128], F32)
        o1s = Tl("ao1s", [128, 4, 128], F32)
        junk = Tl("ajunk", [128, 128], F32)
        onb16 = [Tl("aon%d" % i, [128, 128], BF16) for i in range(2)]
        ont = Tl("aont", [128, 1024], BF16)
        onT = Tl("aonT", [128, 8, 128], BF16)
        win = W["attn_w_in"][j]
        nst = 0
        for s in range(NSEQ):
            load_mod_bc(scp, l, s, 1, plus1=True)
            load_mod_bc(shb, l, s, 0)
            load_mod_bc(gbc, l, s, 2)
            for t in range(NT):
                g = s * NT + t
                xt = xts[g % 2]
                DMA("sp", xt[:], src[g * 128:(g + 1) * 128, :], [srcb[g]], [xt])
                make_hT(xt, scp, shb, hTall[:, :, t * 128:(t + 1) * 128], hTall, TrV(banks[0]), work)
            for h in range(8):
                def wv3(c0, n):
                    return win[:, c0:c0 + n].rearrange("(kc p) n -> p kc n", p=128)
                DMA("pool", wsl["q"][:], wv3(h * 128, 128), [], [wsl["q"]])
                DMA("pool", wsl["k"][:], wv3(1024 + h * 128, 128), [], [wsl["k"]])
                DMA("pool", wsl["v"][:], wv3(2048 + h * 128, 128), [], [wsl["v"]])
                for nm, base in (("qs", 0), ("ks", 1024)):
                    for m in range(2):
                        b0 = base + h * 128 + m * 64
                        DMA("pool", wsl[nm][:, :, m * 64:m * 64 + 32], wv3(b0 + 32, 32), [], [wsl[nm]])
                        DMA("pool", wsl[nm][:, :, m * 64 + 32:m * 64 + 64], wv3(b0, 32), [], [wsl[nm]])
                for nb in range(NSB):
                    cs = slice(nb * QB, (nb + 1) * QB)
                    for (wn, wsn, dstT, bi) in (("q", "qs", qT, 2), ("k", "ks", kT, 2)):
                        pa, pb = banks[bi], banks[bi + 1]
                        for kc in range(8):
                            MM(pa[:, 0:QB], wsl[wn][:, kc, :], hTall[:, kc, cs], kc == 0, kc == 7, [wsl[wn], hTall], [pa])
                        for kc in range(8):
                            MM(pb[:, 0:QB], wsl[wsn][:, kc, :], hTall[:, kc, cs], kc == 0, kc == 7, [wsl[wsn], hTall], [pb])
                        TT("dve", t1[:, 0:QB], pa[:, 0:QB], cosT[:, cs], ALU.mult, [pa, cosT], [t1])
                        TT("dve", t2[:, 0:QB], pb[:, 0:QB], sinT[:, cs], ALU.mult, [pb, sinT], [t2])
                        TT("pool", dstT[:, cs], t1[:, 0:QB], t2[:, 0:QB], ALU.add, [t1, t2], [dstT])
                for t in range(NT):
                    pv = banks[4 + (t % 2)]
                    for kc in range(8):
                        MM(pv[:, 0:128], hTall[:, kc, t * 128:(t + 1) * 128], wsl["v"][:, kc, :], kc == 0, kc == 7, [hTall, wsl["v"]], [pv])
                    CP("dve", vext[:, t, 0:128], pv[:, 0:128], [pv], [vext])
                steps = []
                for Q in range(NSB):
                    for m in range(2):
                        for kb in range(Q * NQB + NQB):
                            steps.append((Q, m, kb))

                def emit_scores(i):
                    Q, m, kb = steps[i]
                    j0 = max(0, kb - Q * NQB)
                    csl = slice(j0 * 128, QB)
                    stb = banks[i % 2]
                    MM(stb[:, csl], kT[m * 64:(m + 1) * 64, kb * 128:(kb + 1) * 128], qT[m * 64:(m + 1) * 64, Q * QB + j0 * 128:(Q + 1) * QB],
                       True, True, [kT, qT], [stb])

                emit_scores(0)
                for i, (Q, m, kb) in enumerate(steps):
                    if i + 1 < len(steps):
                        emit_scores(i + 1)
                    j0 = max(0, kb - Q * NQB)
                    csl = slice(j0 * 128, QB)
                    stb = banks[i % 2]
                    pt = pT[i % 2][0]
                    ACT(pt[:, csl], stb[:, csl], AF.Exp, [stb], [pt], scale=0.125)
                    if kb >= Q * NQB:
                        dsl = slice(j0 * 128, (j0 + 1) * 128)
                        TT("pool", pt[:, dsl], pt[:, dsl], trimask[:], ALU.mult, [pt, trimask], [pt])
                    for jq in range(j0, NQB):
                        ab = banks[4 + jq]
                        MM(ab[:, 0:129], pt[:, jq * 128:(jq + 1) * 128], vext[:, kb, :], kb == 0, kb == Q * NQB + jq, [pt, vext], [ab])
                    if kb == Q * NQB + NQB - 1:
                        for jq in range(NQB):
                            ab = banks[4 + jq]
                            if m == 0:
                                P.op("dve", lambda e, ab=ab: e.reciprocal(out=rs[:, 0:1], in_=ab[:, 128:129]), nrm([ab]), nrm([rs]))
                                TS("dve", o1s[:, jq, :], ab[:, 0:128], rs[:, 0:1], None, ALU.mult, None, [ab, rs], [o1s])
                            else:
                                t = Q * NQB + jq
                                g = s * NT + t
                                P.op("dve", lambda e, ab=ab: e.reciprocal(out=rs[:, 1:2], in_=ab[:, 128:129]), nrm([ab]), nrm([rs]))
                                TS("dve", rs[:, 1:2], rs[:, 1:2], lam, None, ALU.mult, None, [rs, lams], [rs])
                                TS("dve", ot2[:], ab[:, 0:128], rs[:, 1:2], None, ALU.mult, None, [ab, rs], [ot2])
                                TT("dve", ot[:], o1s[:, jq, :], ot2[:], ALU.subtract, [o1s, ot2], [ot])
                                ACT(junk[:], ot[:], AF.Square, [ot], [junk, rs], accum_out=rs[:, 2:3])
                                ACT(rs[:, 3:4], rs[:, 2:3], AF.Sqrt, [rs], [rs], scale=1.0 / 128.0, bias=EPS)
                                P.op("dve", lambda e: e.reciprocal(out=rs[:, 3:4], in_=rs[:, 3:4]), nrm([rs]), nrm([rs]))
                                ob = onb16[jq % 2]
                                STT(ob[:], ot[:], rs[:, 3:4], subw[:], ALU.mult, ALU.mult, [ot, rs, subw], [ob])
                                DMA("sp", on_d[g * 128:(g + 1) * 128, h * 128:(h + 1) * 128], ob[:], [ob], [onb[g]])
            for t in range(NT):
                g = s * NT + t
                xt = xts[g % 2]
                DMA("sp", xt[:], src[g * 128:(g + 1) * 128, :], [srcb[g]], [xt])
                DMA("sp", ont[:], on_d[g * 128:(g + 1) * 128, :], [onb[g]], [ont])
                tb = banks[1]
                tbv = tb[:].bitcast(BF16).rearrange("p (h t) -> p h t", t=128)
                for h in range(8):
                    TR(tbv[:, h, :], ont[:, h * 128:(h + 1) * 128], identb[:], [ont, identb], [tb])
                CP("dve", onT[:, 0:4, :], tbv[:, 0:4, :], [tb], [onT])
                ACT(onT[:, 4:8, :], tbv[:, 4:8, :], AF.Copy, [tb], [onT])
                for half in range(2):
                    yb = banks[2 + half]
                    for h in range(8):
                        MM(yb[:], onT[:, h, :], w_out[:, h, half * 512:(half + 1) * 512], h == 0, h == 7, [onT, w_out], [yb])
                resid_ln(xt, [(banks[2][:], banks[2]), (banks[3][:], banks[3])], gbc, lng, lnb, rr, stat, dst[g * 128:(g + 1) * 128, :], dstb[g])
        P.pop()

    P.push()
    ztile = Tl("ztile", [128, 8192], BF16)
    MS("pool", ztile[:], 0.0, [ztile])
    zb = Buf("xbufzero")
    nrows = MOE_NBLK * 512
    r0 = 0
    while r0 < nrows:
        nr = min(1024, nrows - r0)
        DMA("sp", moe_xbuf_d[r0:r0 + nr, :].rearrange("(p j) d -> p (j d)", p=128), ztile[:, 0:(nr // 128) * 1024], [ztile], [zb])
        r0 += nr
    P.pop()
    phase_mod()
    P.barrier()
    cur, curb = x_d, xb["x"]
    for li, l in enumerate(layers):
        last = (li == len(layers) - 1)
        if "mix" in sub:
            dst, dstb = (out_d, xb["out"]) if (last and "moe" not in sub) else (xs[0], xb[0])
            if l % 2 == 0:
                phase_hgrn(l, cur, curb, dst, dstb)
            else:
                phase_attn(l, cur, curb, dst, dstb)
            cur, curb = dst, dstb
        if "moe" in sub:
            dst, dstb = (out_d, xb["out"]) if last else (xs[1], xb[1])
            phase_moe(l, cur, curb, dst, dstb)
            cur, curb = dst, dstb
    P.finish()
    P.DBG = DBG
    return nc, consts, P


_CACHE = {}


def run(inputs, NSEQ, S, layers, sub=("mix", "moe"), n_cores=8, NE=32):
    from concourse.bass_utils import run_bass_kernel_spmd
    nc, consts, P = build(NSEQ, S, layers, sub, NE)
    x = np.ascontiguousarray(inputs["x"], dtype=np.float32).reshape(n_cores, NSEQ * S, D)
    c = np.ascontiguousarray(inputs["c"], dtype=np.float32).reshape(n_cores, NSEQ, D)
    inputs = dict(inputs)
    rw = np.concatenate([np.asarray(inputs["router_g_w"], np.float32), np.asarray(inputs["router_e_w"], np.float32)], axis=2)
    inputs["router_w"] = np.ascontiguousarray(rw.reshape(rw.shape[0], 8, 128, 36).transpose(0, 2, 1, 3))
    inputs["router_b"] = np.concatenate([np.asarray(inputs["router_g_b"], np.float32), np.asarray(inputs["router_e_b"], np.float32)], axis=1)
    in_maps = []
    for i in range(n_cores):
        m = {"x": x[i], "cT": np.ascontiguousarray(c[i].reshape(NSEQ, 8, 128).transpose(2, 1, 0))}
        for nm, shp in wnames():
            m[nm] = np.ascontiguousarray(np.asarray(inputs[nm])[:shp[0]], dtype=np.float32)
        for nm, arr in consts.items():
            m[nm] = arr
        in_maps.append(m)
    res = run_bass_kernel_spmd(nc, in_maps, core_ids=list(range(n_cores)))
    out = np.stack([np.asarray(r["out"]) for r in res.results], 0)
    global LAST_DBG
    LAST_DBG = {k: np.asarray(res.results[0]["dbg_" + k]) for k in P.DBG}
    return out.reshape(n_cores * NSEQ, S, D)


def kernel(**inputs):
    out = run(inputs, 2, 4096, [0, 1, 2, 3])
    return out.astype(np.float32)
```

```python
import contextlib
import numpy as np
import concourse.bass as bass
import concourse.mybir as mybir

F32 = mybir.dt.float32
BF16 = mybir.dt.bfloat16
I32 = mybir.dt.int32
U32 = mybir.dt.uint32
AF = mybir.ActivationFunctionType
ALU = mybir.AluOpType
AX = mybir.AxisListType

QUEUES = ("pe", "act", "dve", "pool", "sp")
N_DMA_CH = 12


class Buf:
    __slots__ = ("name", "w", "r", "excl")

    def __init__(self, name="", excl=False):
        self.name = name
        self.excl = excl
        self.w = None
        self.r = []


class Prog:
    def __init__(self, nc, same_engine_sync=True):
        self.nc = nc
        self.stack = contextlib.ExitStack()
        self.ops = {q: [] for q in QUEUES}
        self.cnt = {q: 0 for q in QUEUES}
        self.sems = {}
        self.seen = {q: {} for q in QUEUES}
        self.same_engine_sync = same_engine_sync
        for q in QUEUES:
            self.sems["e_" + q] = self.stack.enter_context(nc.semaphore("e_" + q))
        self.dma_ch = {}
        self.dma_rr = {}
        for q in ("sp", "act", "pool"):
            chs = []
            for i in range(N_DMA_CH):
                key = "d_%s_%d" % (q, i)
                self.sems[key] = self.stack.enter_context(nc.semaphore(key))
                chs.append([key, 0])
            self.dma_ch[q] = chs
            self.dma_rr[q] = 0
        self.n_inst = 0
        self.uid = 0
        self.scopes = [self.stack]

    def sb(self, name, shape, dtype):
        self.uid += 1
        return self.scopes[-1].enter_context(self.nc.sbuf_tensor("sb%d_%s" % (self.uid, name), list(shape), dtype))

    def ps(self, name, shape, dtype=F32):
        self.uid += 1
        return self.scopes[-1].enter_context(self.nc.psum_tensor("ps%d_%s" % (self.uid, name), list(shape), dtype))

    def push(self):
        self.scopes.append(contextlib.ExitStack())

    def pop(self):
        self.barrier()
        self.scopes.pop().close()

    def barrier(self):
        targets = []
        for q in QUEUES:
            if self.cnt[q] > 0:
                targets.append(("e_" + q, self.cnt[q], q))
        for q, chs in self.dma_ch.items():
            for key, val in chs:
                if val > 0:
                    targets.append((key, val, None))
        sems = self.sems
        for q in QUEUES:
            seen = self.seen[q]
            waits = []
            for key, val, wq in targets:
                if wq == q and q != "sp":
                    continue
                if seen.get(key, 0) >= val:
                    continue
                seen[key] = val
                waits.append((key, val))

            def emit(eng, waits=waits):
                for k, v in waits:
                    eng.wait_ge(sems[k], v)

            self.ops[q].append(emit)
            self.n_inst += len(waits)

    def _collect(self, q, reads, writes, is_dma):
        waits = {}

        def need(tok):
            key, val, wq, wdma = tok
            if (not wdma) and (not is_dma) and wq == q:
                if q == "pe" or not self.same_engine_sync:
                    return
            if waits.get(key, 0) < val:
                waits[key] = val

        for b in reads:
            if b.w is not None:
                need(b.w)
        for b in writes:
            if b.w is not None:
                need(b.w)
            for t in b.r:
                need(t)
        out = []
        seen = self.seen[q]
        for key, val in waits.items():
            if seen.get(key, 0) >= val:
                continue
            seen[key] = val
            out.append((key, val))
        return out

    def _commit(self, tok, reads, writes):
        for b in reads:
            b.r.append(tok)
        for b in writes:
            b.w = tok
            b.r = []

    def op(self, q, fn, reads=(), writes=(), signal=True):
        ex = [b for b in reads if b.excl]
        if ex:
            writes = list(writes) + [b for b in ex if b not in writes]
        waits = self._collect(q, reads, writes, False)
        key = "e_" + q
        if signal:
            self.cnt[q] += 1
            val = self.cnt[q]
        else:
            val = self.cnt[q] + 1
        tok = (key, val, q, False)
        sems = self.sems

        def emit(eng, waits=waits, fn=fn, key=key, signal=signal):
            for k, v in waits[:-1]:
                eng.wait_ge(sems[k], v)
            ins = fn(eng)
            if waits:
                ins._wait_ge(sems[waits[-1][0]], waits[-1][1])
            if signal:
                ins.then_inc(sems[key], 1)

        self.ops[q].append(emit)
        self.n_inst += 1 + max(0, len(waits) - 1)
        self._commit(tok, reads, writes)
        return tok

    def dma(self, q, fn, reads=(), writes=()):
        waits = self._collect(q, reads, writes, True)
        chs = self.dma_ch[q]
        i = self.dma_rr[q]
        self.dma_rr[q] = (i + 1) % len(chs)
        ch = chs[i]
        key = ch[0]
        prev = ch[1]
        ch[1] += 16
        val = ch[1]
        seen = self.seen[q]
        if prev > 0 and seen.get(key, 0) < prev:
            seen[key] = prev
            waits = waits + [(key, prev)]
        tok = (key, val, q, True)
        sems = self.sems

        def emit(eng, waits=waits, fn=fn, key=key):
            for k, v in waits:
                eng.wait_ge(sems[k], v)
            fn(eng).then_inc(sems[key], 16)

        self.ops[q].append(emit)
        self.n_inst += 1 + len(waits)
        self._commit(tok, reads, writes)
        return tok

    def finish(self):
        nc = self.nc
        final = []
        for q, chs in self.dma_ch.items():
            for key, val in chs:
                if val > 0:
                    final.append((key, val))
        for q in QUEUES:
            if q != "sp" and self.cnt[q] > 0:
                final.append(("e_" + q, self.cnt[q]))
        sems = self.sems
        ops = self.ops
        with nc.Block() as block:
            @block.tensor
            def _(eng):
                for f in ops["pe"]:
                    f(eng)

            @block.scalar
            def _(eng):
                for f in ops["act"]:
                    f(eng)

            @block.vector
            def _(eng):
                for f in ops["dve"]:
                    f(eng)

            @block.gpsimd
            def _(eng):
                for f in ops["pool"]:
                    f(eng)

            @block.sync
            def _(eng):
                for f in ops["sp"]:
                    f(eng)
                for k, v in final:
                    eng.wait_ge(sems[k], v)
        self.stack.close()

DEBUG = False
MOE_STAGE = 2
MOE_CUT = 5
D = 1024
DEPTH = 4
ALPHA = (2 * DEPTH) ** 0.25
EPS = 1e-5
WDEPTH = 4


def wnames():
    L = WDEPTH
    return [("ada_w", [L, 1024, 6144]), ("ada_b", [L, 6144]), ("ln_g", [L, 2, 1024]), ("ln_b", [L, 2, 1024]),
            ("hgrn_w_in", [2, 1024, 4096]), ("hgrn_w_out", [2, 1024, 1024]), ("hgrn_lb", [2, 1024]),
            ("hgrn_norm_w", [2, 128]), ("attn_w_in", [2, 1024, 3072]), ("attn_w_out", [2, 1024, 1024]),
            ("attn_lambda", [2, 4, 64]), ("attn_subln_w", [2, 128]), ("router_w", [L, 128, 8, 36]), ("router_b", [L, 36]),
            ("moe_w1", [L, 32, 1024, 512]), ("moe_w3", [L, 32, 1024, 512]), ("moe_w2", [L, 32, 512, 1024])]


def make_consts(S):
    c = {}
    c["identf"] = np.eye(128, dtype=np.float32)
    s = np.arange(128)[:, None]
    t = np.arange(128)[None, :]
    c["blockmask"] = ((s // 32 == t // 32) & (s <= t)).astype(np.float32)
    c["trimask"] = (s <= t).astype(np.float32)
    sm = np.ones((128, 128), np.float32)
    sm[:, ::32] = 0.0
    c["scanmask"] = sm
    ci = np.zeros((128, 4), np.float32)
    for k in range(4):
        ci[k * 32:(k + 1) * 32, k] = 1.0
    c["chunkind"] = ci
    half = 32
    inv_freq = (np.float32(10000.0) ** (-np.arange(half, dtype=np.float32) / np.float32(half))).astype(np.float32)
    ang = (np.arange(S, dtype=np.float32)[:, None] * inv_freq[None, :]).astype(np.float32)
    cos = np.cos(ang).astype(np.float32).T
    sin = np.sin(ang).astype(np.float32).T
    c["utri"] = (s < t).astype(np.float32)
    wi = np.zeros((128, 12), np.float32)
    for kc in range(8):
        wi[:, kc] = kc * 128 + np.arange(128)
    for fc in range(4):
        wi[:, 8 + fc] = fc * 128 + np.arange(128)
    c["widx"] = wi
    c["bstart"] = (np.arange(128, dtype=np.float32) * 512.0)[None, :]
    c["ropecos"] = np.ascontiguousarray(np.concatenate([cos, cos, cos, cos], 0))
    c["ropesin"] = np.ascontiguousarray(np.concatenate([-sin, sin, -sin, sin], 0))
    return c


def build(NSEQ, S, layers, sub=("mix", "moe"), NE=32):
    NT = S // 128
    NTOK = NSEQ * S
    nc = bass.Bass("TRN2", target_bir_lowering=False)
    dtn = nc.dram_tensor
    x_d = dtn("x", [NTOK, D], F32, kind="ExternalInput").ap()
    c_d = dtn("cT", [128, 8, NSEQ], F32, kind="ExternalInput").ap()
    W = {}
    for nm, shp in wnames():
        W[nm] = dtn(nm, shp, F32, kind="ExternalInput").ap()
    consts = make_consts(S)
    CD = {}
    for nm, arr in consts.items():
        CD[nm] = dtn(nm, list(arr.shape), F32, kind="ExternalInput").ap()
    out_d = dtn("out", [NTOK, D], F32, kind="ExternalOutput").ap()
    xs = [dtn("xs0", [NTOK, D], F32, kind="Internal").ap(), dtn("xs1", [NTOK, D], F32, kind="Internal").ap()]
    mod_d = dtn("mod_d", [WDEPTH, NSEQ, 6144], F32, kind="Internal").ap()
    on_d = dtn("on_d", [NTOK, D], BF16, kind="Internal").ap()
    MOE_NBLK = (2 * NTOK) // 512 + 32
    moe_h2_d = dtn("moe_h2", [NTOK, D], BF16, kind="Internal").ap()
    moe_xbuf_d = dtn("moe_xbuf", [MOE_NBLK * 512, D], BF16, kind="Internal").ap()
    moe_ybuf_d = dtn("moe_ybuf", [MOE_NBLK * 512, D], BF16, kind="Internal").ap()

    P = Prog(nc)
    DBG = {}

    def dbg(name, ap, bufs, psum=False):
        if not DEBUG or name in DBG:
            return
        shp = list(ap.shape)
        d = dtn("dbg_" + name, shp, F32, kind="ExternalOutput").ap()
        DBG[name] = d
        if psum:
            tmp = P.sb("dbgtmp_" + name, shp, F32)
            tb = Buf()
            P.op("dve", lambda e: e.tensor_copy(out=tmp[:], in_=ap), nrm(bufs), [tb])
            P.dma("pool", lambda e: e.dma_start(out=d, in_=tmp[:]), [tb], [])
        else:
            P.dma("pool", lambda e: e.dma_start(out=d, in_=ap), nrm(bufs), [])

    def nrm(l):
        return [b.b if hasattr(b, "b") else b for b in l]

    def MM(out, lhsT, rhs, start, stop, r, w):
        P.op("pe", lambda e: e.matmul(out, lhsT=lhsT, rhs=rhs, start=start, stop=stop), nrm(r), nrm(w), signal=bool(stop))

    def TR(out, in_, ident, r, w):
        P.op("pe", lambda e: e.transpose(out=out, in_=in_, identity=ident), nrm(r), nrm(w))

    def ACT(out, in_, func, r, w, **kw):
        P.op("act", lambda e: e.activation(out=out, in_=in_, func=func, **kw), nrm(r), nrm(w))

    def TT(q, out, in0, in1, op, r, w):
        P.op(q, lambda e: e.tensor_tensor(out=out, in0=in0, in1=in1, op=op), nrm(r), nrm(w))

    def TS(q, out, in0, s1, s2, op0, op1, r, w):
        if op1 is None:
            P.op(q, lambda e: e.tensor_scalar(out=out, in0=in0, scalar1=s1, scalar2=None, op0=op0), nrm(r), nrm(w))
        else:
            P.op(q, lambda e: e.tensor_scalar(out=out, in0=in0, scalar1=s1, scalar2=s2, op0=op0, op1=op1), nrm(r), nrm(w))

    def STT(out, in0, scalar, in1, op0, op1, r, w):
        P.op("dve", lambda e: e.scalar_tensor_tensor(out=out, in0=in0, scalar=scalar, in1=in1, op0=op0, op1=op1), nrm(r), nrm(w))

    def CP(q, out, in_, r, w):
        P.op(q, lambda e: e.tensor_copy(out=out, in_=in_), nrm(r), nrm(w))

    def MS(q, ap, val, w):
        P.op(q, lambda e: e.memset(ap, val), [], nrm(w))

    def DMA(q, out, in_, r, w, **kw):
        P.dma(q, lambda e: e.dma_start(out=out, in_=in_, **kw), nrm(r), nrm(w))

    class Tl:
        def __init__(self, name, shape, dtype, psum=False):
            self.t = P.ps(name, shape, dtype) if psum else P.sb(name, shape, dtype)
            self.b = Buf(name, excl=psum)

        def __getitem__(self, k):
            return self.t[k]

    xb = {"x": [Buf() for _ in range(NSEQ * NT)], 0: [Buf() for _ in range(NSEQ * NT)],
          1: [Buf() for _ in range(NSEQ * NT)], "out": [Buf() for _ in range(NSEQ * NT)]}
    modb = Buf("mod_d")
    onb = [Buf() for _ in range(NSEQ * NT)]

    identf = Tl("identf", [128, 128], F32)
    identb = Tl("identb", [128, 128], BF16)
    blockmask = Tl("blockmask", [128, 128], F32)
    trimask = Tl("trimask", [128, 128], BF16)
    scanmask = Tl("scanmask", [128, 128], F32)
    chunkind = Tl("chunkind", [128, 4], F32)
    DMA("sp", identf[:], CD["identf"], [], [identf])
    DMA("pool", identb[:], CD["identf"], [], [identb])
    DMA("sp", blockmask[:], CD["blockmask"], [], [blockmask])
    DMA("pool", trimask[:], CD["trimask"], [], [trimask])
    DMA("sp", scanmask[:], CD["scanmask"], [], [scanmask])
    DMA("sp", chunkind[:], CD["chunkind"], [], [chunkind])

    def phase_mod():
        P.push()
        condT = Tl("condT", [128, 8, NSEQ], F32)
        DMA("sp", condT[:], c_d, [], [condT])
        ACT(condT[:], condT[:], AF.Silu, [condT], [condT])
        stg = [Tl("adastg%d" % i, [128, 8, 512], F32) for i in range(2)]
        adab = Tl("adab", [NSEQ, 6144], F32)
        modrow = Tl("modrow", [NSEQ, 6144], F32)
        mps = [Tl("modps%d" % i, [128, 512], F32, psum=True) for i in range(2)]
        n = 0
        for l in layers:
            DMA("sp", adab[:], W["ada_b"][l:l + 1, :].partition_broadcast(NSEQ) if NSEQ > 1 else W["ada_b"][l:l + 1, :], [], [adab])
            for j in range(12):
                st = stg[n % 2]
                pp = mps[n % 2]
                n += 1
                DMA("sp", st[:], W["ada_w"][l, :, j * 512:(j + 1) * 512].rearrange("(kc p) n -> p kc n", p=128), [], [st])
                for kc in range(8):
                    MM(pp[0:NSEQ, :], condT[:, kc, :], st[:, kc, :], kc == 0, kc == 7, [condT, st], [pp])
                TT("dve", modrow[:, j * 512:(j + 1) * 512], pp[0:NSEQ, :], adab[:, j * 512:(j + 1) * 512], ALU.add, [pp, adab], [modrow])
            DMA("sp", mod_d[l], modrow[:], [modrow], [modb])
        P.pop()

    def load_mod_bc(tl, l, s, j, plus1=False):
        DMA("sp", tl[:], mod_d[l, s:s + 1, j * 1024:(j + 1) * 1024].partition_broadcast(128), [modb], [tl])
        if plus1:
            TS("pool", tl[:], tl[:], 1.0, None, ALU.add, None, [tl], [tl])

    def make_hT(xt, scp, sh, hT_out_ap, hTbuf, trp, work, hTf_ap=None, hTfbuf=None):
        TT("dve", work[:], xt[:], scp[:], ALU.mult, [xt, scp], [work])
        TT("pool", work[:], work[:], sh[:], ALU.add, [work, sh], [work])
        for half in range(2):
            for i in range(4):
                kc = half * 4 + i
                TR(trp[:, i, :], work[:, kc * 128:(kc + 1) * 128], identf[:], [work, identf], [trp])
            if half == 0:
                ACT(hT_out_ap[:, 0:4, :], trp[:], AF.Copy, [trp], [hTbuf])
            else:
                CP("dve", hT_out_ap[:, 4:8, :], trp[:], [trp], [hTbuf])
            if hTf_ap is not None:
                if half == 0:
                    CP("dve", hTf_ap[:, 0:4, :], trp[:], [trp], [hTfbuf])
                else:
                    ACT(hTf_ap[:, 4:8, :], trp[:], AF.Copy, [trp], [hTfbuf])

    def resid_ln(xt, yps_list, gbc, lng, lnb, r, stat, dst_ap, dst_buf):
        for half in range(2):
            yap, ybuf = yps_list[half]
            sl = slice(half * 512, (half + 1) * 512)
            TT("dve", r[:, sl], yap, gbc[:, sl], ALU.mult, [ybuf, gbc], [r])
        STT(r[:], xt[:], ALPHA, r[:], ALU.mult, ALU.add, [xt, r], [r])
        for c4 in range(2):
            P.op("dve", lambda e, c4=c4: e.bn_stats(out=stat[:, c4 * 6:(c4 + 1) * 6], in_=r[:, c4 * 512:(c4 + 1) * 512]), nrm([r]), nrm([stat]))
        P.op("dve", lambda e: e.bn_aggr(out=stat[:, 12:14], in_=stat[:, 0:12]), nrm([stat]), nrm([stat]))
        TS("dve", stat[:, 14:15], stat[:, 13:14], EPS, None, ALU.add, None, [stat], [stat])
        ACT(stat[:, 14:15], stat[:, 14:15], AF.Sqrt, [stat], [stat])
        P.op("dve", lambda e: e.reciprocal(out=stat[:, 15:16], in_=stat[:, 14:15]), nrm([stat]), nrm([stat]))
        STT(stat[:, 16:17], stat[:, 12:13], -1.0, stat[:, 15:16], ALU.mult, ALU.mult, [stat], [stat])
        ACT(r[:], r[:], AF.Identity, [r, stat], [r], scale=stat[:, 15:16], bias=stat[:, 16:17])
        TT("dve", r[:], r[:], lng[:], ALU.mult, [r, lng], [r])
        TT("pool", r[:], r[:], lnb[:], ALU.add, [r, lnb], [r])
        DMA("sp", dst_ap, r[:], [r], [dst_buf])

    def phase_hgrn(l, src, srcb, dst, dstb):
        j = l // 2
        P.push()
        w_in = Tl("hw_in", [128, 8, 4096], BF16)
        w_out = Tl("hw_out", [128, 8, 1024], BF16)
        for kc in range(8):
            DMA("pool", w_in[:, kc, :], W["hgrn_w_in"][j, kc * 128:(kc + 1) * 128, :], [], [w_in], max_dma_last_dim=4096)
        DMA("pool", w_out[:], W["hgrn_w_out"][j].rearrange("(kc p) n -> p kc n", p=128), [], [w_out], max_dma_last_dim=4096)
        lbraw = Tl("lbraw", [128, 2, 8], F32)
        lbc = Tl("lbc", [128, 8], F32)
        oml = Tl("oml", [128, 8], F32)
        DMA("sp", lbraw[:], W["hgrn_lb"].rearrange("j (h p) -> p j h", p=128), [], [lbraw], allow_slow_non_contiguous=True)
        if j == 0:
            TT("dve", lbc[:], lbraw[:, 0, :], lbraw[:, 0, :], ALU.subtract, [lbraw], [lbc])
        else:
            TT("dve", lbc[:], lbraw[:, 1, :], lbraw[:, 0, :], ALU.subtract, [lbraw], [lbc])
            ACT(lbc[:], lbc[:], AF.Sigmoid, [lbc], [lbc])
        TS("dve", oml[:], lbc[:], -1.0, 1.0, ALU.mult, ALU.add, [lbc], [oml])
        normw = Tl("normw", [128, 1024], F32)
        for h in range(8):
            DMA("sp", normw[:, h * 128:(h + 1) * 128], W["hgrn_norm_w"][j:j + 1, :].partition_broadcast(128), [], [normw])
        lng = Tl("lng", [128, 1024], F32)
        lnb = Tl("lnb", [128, 1024], F32)
        DMA("sp", lng[:], W["ln_g"][l, 0:1, :].partition_broadcast(128), [], [lng])
        DMA("sp", lnb[:], W["ln_b"][l, 0:1, :].partition_broadcast(128), [], [lnb])
        scp = Tl("scp", [128, 1024], F32)
        shb = Tl("shb", [128, 1024], F32)
        gbc = Tl("gbc", [128, 1024], F32)
        xts = [Tl("xt%d" % i, [128, 1024], F32) for i in range(2)]
        work = Tl("work", [128, 1024], F32)
        rr = Tl("rr", [128, 1024], F32)
        stat = Tl("stat", [128, 32], F32)
        hT = Tl("hT", [128, 8, 128], BF16)
        trp = Tl("trp", [128, 4, 128], F32, psum=True)
        qz = [Tl("qzps%d" % i, [128, 4, 128], F32, psum=True) for i in range(2)]
        qzb = [[Buf(), Buf()], [Buf(), Buf()]]
        vg = [Tl("vgps%d" % i, [128, 512], F32, psum=True) for i in range(2)]
        hd = [Tl("hdps%d" % i, [128, 4, 128], F32, psum=True) for i in range(2)]
        hdb = [[Buf() for _ in range(4)] for _ in range(2)]
        ktv = [qz[i][:, 2, :].bitcast(BF16)[:, 0:128] for i in range(2)]
        ktb = [Buf(), Buf()]

        class OnV:
            b = trp.b
            v = trp[:].bitcast(BF16).rearrange("p a (b c) -> p (a b) c", c=128)

            def __getitem__(self, k):
                return self.v[k]
        ontp = OnV()
        vsb = Tl("vsb", [128, 1024], BF16)
        gw = Tl("gw", [128, 1024], F32)
        sig = [Tl("sig%d" % i, [128, 128], F32) for i in range(2)]
        lf = [Tl("lf%d" % i, [128, 128], F32) for i in range(2)]
        kk = [Tl("kk%d" % i, [128, 128], F32) for i in range(2)]
        bb = [Tl("bb%d" % i, [128, 128], F32) for i in range(2)]
        Ep = [Tl("Ep%d" % i, [128, 128], F32) for i in range(2)]
        Em = [Tl("Em%d" % i, [128, 128], F32) for i in range(2)]
        qT = [Tl("qT%d" % i, [128, 128], BF16) for i in range(2)]
        kT = [Tl("kT%d" % i, [128, 128], BF16) for i in range(2)]
        AT = [Tl("AT%d" % i, [128, 128], BF16) for i in range(2)]
        qpad = [Tl("qpad%d" % i, [128, 640], BF16) for i in range(2)]
        kmask = [Tl("kmask%d" % i, [128, 4, 128], BF16) for i in range(2)]
        kmb = [[Buf() for _ in range(4)] for _ in range(2)]
        s1 = [Tl("s1_%d" % i, [128, 128], F32) for i in range(2)]
        ss = Tl("ssq", [128, 16], F32)
        junk = Tl("junk", [128, 128], F32)
        on_all = Tl("on_all", [128, 8, 128], BF16)
        onT = Tl("onT", [128, 8, 128], BF16)
        state = [[Tl("st_%d_%d" % (s, h), [128, 128], F32) for h in range(8)] for s in range(NSEQ)]
        stbf = [[Tl("stb_%d_%d" % (s, h), [128, 128], BF16) for h in range(8)] for s in range(NSEQ)]
        for i in range(2):
            MS("pool", qpad[i][:], 0.0, [qpad[i]])
        for s in range(NSEQ):
            for h in range(8):
                MS("pool", state[s][h][:], 0.0, [state[s][h]])
                MS("pool", stbf[s][h][:], 0.0, [stbf[s][h]])
        it = 0
        for s in range(NSEQ):
            load_mod_bc(scp, l, s, 1, plus1=True)
            load_mod_bc(shb, l, s, 0)
            load_mod_bc(gbc, l, s, 2)
            for t in range(NT):
                g = s * NT + t
                xt = xts[g % 2]
                DMA("sp", xt[:], src[g * 128:(g + 1) * 128, :], [srcb[g]], [xt])
                make_hT(xt, scp, shb, hT, hT, trp, work)
                for cch in range(4):
                    pp = vg[cch % 2]
                    for kc in range(8):
                        MM(pp[:], hT[:, kc, :], w_in[:, kc, 2048 + cch * 512:2048 + (cch + 1) * 512], kc == 0, kc == 7, [hT, w_in], [pp])
                    if cch < 2:
                        CP("dve", vsb[:, cch * 512:(cch + 1) * 512], pp[:], [pp], [vsb])
                    else:
                        ACT(gw[:, (cch - 2) * 512:(cch - 1) * 512], pp[:], AF.Silu, [pp], [gw])
                TT("pool", gw[:], gw[:], normw[:], ALU.mult, [gw, normw], [gw])
                def head_gen(h, p2, s=s):
                    qps, zps = qz[p2][:, 0, :], qz[p2][:, 1, :]
                    qb_, zb_ = qzb[p2]
                    for kc in range(8):
                        MM(qps, w_in[:, kc, h * 128:(h + 1) * 128], hT[:, kc, :], kc == 0, kc == 7, [hT, w_in], [qb_])
                    for kc in range(8):
                        MM(zps, w_in[:, kc, 1024 + h * 128:1024 + (h + 1) * 128], hT[:, kc, :], kc == 0, kc == 7, [hT, w_in], [zb_])
                    yield
                    ACT(sig[p2][:], zps, AF.Sigmoid, [zb_], [sig[p2]])
                    yield
                    TS("dve", sig[p2][:], sig[p2][:], oml[:, h:h + 1], lbc[:, h:h + 1], ALU.mult, ALU.add, [sig[p2], oml, lbc], [sig[p2]])
                    yield
                    ACT(lf[p2][:], sig[p2][:], AF.Ln, [sig[p2]], [lf[p2]])
                    dbg('f', sig[p2][:], [sig[p2]]); dbg('lf', lf[p2][:], [lf[p2]])
                    TS("pool", kk[p2][:], sig[p2][:], -1.0, 1.0, ALU.mult, ALU.add, [sig[p2]], [kk[p2]])
                    yield
                    P.op("dve", lambda e, p2=p2: e.tensor_tensor_scan(out=bb[p2][:], data0=scanmask[:], data1=lf[p2][:], initial=0.0,
                                                                     op0=ALU.mult, op1=ALU.add), nrm([scanmask, lf[p2]]), nrm([bb[p2]]))
                    yield
                    ACT(Ep[p2][:], bb[p2][:], AF.Exp, [bb[p2]], [Ep[p2]])
                    ACT(Em[p2][:], bb[p2][:], AF.Exp, [bb[p2]], [Em[p2]], scale=-1.0)
                    yield
                    TT("dve", qT[p2][:], qps, Ep[p2][:], ALU.mult, [qb_, Ep[p2]], [qT[p2]])
                    CP("pool", qpad[p2][:].rearrange("p (c x) -> p c x", x=160)[:, :, 0:32],
                       qT[p2][:].rearrange("p (c j) -> p c j", j=32), [qT[p2]], [qpad[p2]])
                    TT("pool", kT[p2][:], kk[p2][:], Em[p2][:], ALU.mult, [kk[p2], Em[p2]], [kT[p2]])
                    yield
                    dbg('bb', bb[p2][:], [bb[p2]]); dbg('qT', qT[p2][:], [qT[p2]]); dbg('kT', kT[p2][:], [kT[p2]]); dbg('qpad', qpad[p2][:], [qpad[p2]])
                    stp, ops_, up = hd[p2][:, 0, :], vg[p2][:, 0:128], [hd[p2][:, 2, :], hd[p2][:, 3, :]]
                    stb_, ob_, ub_ = hdb[p2][0], vg[p2], [hdb[p2][2], hdb[p2][3]]
                    MM(stp, kT[p2][:], qT[p2][:], True, True, [kT[p2], qT[p2]], [stb_])
                    TR(ktv[p2], kT[p2][:], identb[:], [kT[p2], identb], [ktb[p2]])
                    yield
                    TT("dve", AT[p2][:], stp, blockmask[:], ALU.mult, [stb_, blockmask], [AT[p2]])
                    for c in range(4):
                        ACT(kmask[p2][:, c, :], ktv[p2], AF.Copy, [ktb[p2], chunkind], [kmb[p2][c]], scale=chunkind[:, c:c + 1])
                    yield
                    vh = vsb[:, h * 128:(h + 1) * 128]
                    MM(ops_, AT[p2][:], vh, True, False, [AT[p2], vsb], [ob_])
                    yield
                    stt, stb16 = state[s][h], stbf[s][h]
                    for c in range(4):
                        MM(ops_, qpad[p2][:, c * 128:(c + 1) * 128], stb16[:], False, c == 3, [qpad[p2], stb16], [ob_])
                        MM(up[c % 2], kmask[p2][:, c, :], vh, True, True, [kmb[p2][c], vsb], [ub_[c % 2]])
                        yield
                        ebl = Ep[p2][:, 32 * c + 31:32 * c + 32]
                        TS("pool", s1[p2][:], stt[:], ebl, None, ALU.mult, None, [stt, Ep[p2]], [s1[p2]])
                        yield
                        STT(stt[:], up[c % 2], ebl, s1[p2][:], ALU.mult, ALU.add, [ub_[c % 2], Ep[p2], s1[p2]], [stt])
                        yield
                        ACT(stb16[:], stt[:], AF.Copy, [stt], [stb16])
                        yield
                    dbg('AT', AT[p2][:], [AT[p2]]); dbg('ops', ops_, [ob_], psum=True); dbg('kmask', kmask[p2][:], kmb[p2]); dbg('state', stt[:], [stt])
                    ACT(junk[:], ops_, AF.Square, [ob_], [junk, ss], accum_out=ss[:, h:h + 1])
                    yield
                    ACT(ss[:, 8 + h:9 + h], ss[:, h:h + 1], AF.Sqrt, [ss], [ss], scale=1.0 / 128.0, bias=EPS)
                    yield
                    P.op("dve", lambda e, h=h: e.reciprocal(out=ss[:, 8 + h:9 + h], in_=ss[:, 8 + h:9 + h]), nrm([ss]), nrm([ss]))
                    yield
                    STT(on_all[:, h, :], ops_, ss[:, 8 + h:9 + h], gw[:, h * 128:(h + 1) * 128], ALU.mult, ALU.mult, [ob_, ss, gw], [on_all])
                    yield
                    TR(ontp[:, h, :], on_all[:, h, :], identb[:], [on_all, identb], [ontp])
                for hp in range(0, 8, 2):
                    alive = [head_gen(hp, 0), head_gen(hp + 1, 1)]
                    while alive:
                        for gg in list(alive):
                            try:
                                next(gg)
                            except StopIteration:
                                alive.remove(gg)
                dbg('on_all', on_all[:], [on_all]); dbg('gw', gw[:], [gw]); dbg('vsb', vsb[:], [vsb]); dbg('hT', hT[:], [hT]); dbg('ss', ss[:], [ss])
                CP("dve", onT[:, 0:4, :], ontp[:, 0:4, :], [ontp], [onT])
                ACT(onT[:, 4:8, :], ontp[:, 4:8, :], AF.Copy, [ontp], [onT])
                for half in range(2):
                    for h in range(8):
                        MM(vg[half][:], onT[:, h, :], w_out[:, h, half * 512:(half + 1) * 512], h == 0, h == 7, [onT, w_out], [vg[half]])
                dbg('y0', vg[0][:], [vg[0]], psum=True)
                resid_ln(xt, [(vg[0][:], vg[0]), (vg[1][:], vg[1])], gbc, lng, lnb, rr, stat, dst[g * 128:(g + 1) * 128, :], dstb[g])
        P.pop()

    def phase_moe_dense(l, src, srcb, dst, dstb):
        P.push()
        GT = min(16, NT)
        NG = (NSEQ * NT) // GT
        SGT = min(4, GT)
        wr = Tl("wr", [128, 8, 36], F32)
        DMA("sp", wr[:], W["router_w"][l], [], [wr])
        rbias = Tl("rbias", [128, 36], F32)
        DMA("sp", rbias[:], W["router_b"][l:l + 1, :].partition_broadcast(128), [], [rbias])
        lng = Tl("lng", [128, 1024], F32)
        lnb = Tl("lnb", [128, 1024], F32)
        DMA("sp", lng[:], W["ln_g"][l, 1:2, :].partition_broadcast(128), [], [lng])
        DMA("sp", lnb[:], W["ln_b"][l, 1:2, :].partition_broadcast(128), [], [lnb])
        scp = Tl("scp", [128, 1024], F32)
        shb = Tl("shb", [128, 1024], F32)
        gbc = Tl("gbc", [128, 1024], F32)
        xts = [Tl("xt%d" % i, [128, 1024], F32) for i in range(2)]
        work = Tl("work", [128, 1024], F32)
        rr = Tl("rr", [128, 1024], F32)
        stat = Tl("stat", [128, 32], F32)
        h2T = Tl("h2T", [128, 8, GT * 128], BF16)
        h2Tf = Tl("h2Tf", [128, 8, 128], F32)
        yacc = Tl("yacc", [128, GT, 1024], F32)
        yaccb = [Buf() for _ in range(GT)]
        gates = Tl("gates", [128, GT, 32], F32)
        gT = [Tl("gT%d" % i, [128, 4, 512], BF16) for i in range(2)]
        slt = [Tl("slt%d" % i, [128, 512], F32) for i in range(2)]
        wb = [dict(w1=Tl("w1_%d" % i, [128, 8, 512], BF16), w3=Tl("w3_%d" % i, [128, 8, 512], BF16),
                   w2=Tl("w2_%d" % i, [128, 4, 1024], BF16)) for i in range(2)]
        trp = Tl("trp", [128, 4, 128], F32, psum=True)
        lgp = Tl("lgp", [128, 512], F32, psum=True)
        hp1 = [Tl("hp1_%d" % i, [128, 512], F32, psum=True) for i in range(2)]
        hp3 = [Tl("hp3_%d" % i, [128, 512], F32, psum=True) for i in range(2)]
        yp = [Tl("yp%d" % i, [128, 512], F32, psum=True) for i in range(2)]
        lg = Tl("lg", [128, 36], F32)
        sm = Tl("rsm", [128, 16], F32)
        oh = Tl("oh", [128, 4], F32)
        ejunk = Tl("ejunk", [128, 4], F32)
        m1 = Tl("m1", [128, 4], F32)
        m2 = Tl("m2", [128, 4], F32)
        dd = Tl("dd", [128, 4], F32)
        c1 = Tl("c1", [128, 4], F32)
        c2 = Tl("c2", [128, 4], F32)
        mk1 = Tl("mk1", [128, 4, 8], F32)
        mk2 = Tl("mk2", [128, 4, 8], F32)
        el2 = Tl("el2", [128, 4, 8], F32)

        def bc48(ap):
            return ap.unsqueeze(2).to_broadcast([128, 4, 8])

        for gidx in range(NG):
            g0 = gidx * GT
            s = g0 // NT
            load_mod_bc(scp, l, s, 4, plus1=True)
            load_mod_bc(shb, l, s, 3)
            load_mod_bc(gbc, l, s, 5)
            if MOE_CUT != -3:
                MS("pool", yacc[:], 0.0, yaccb)
            for i in range(GT):
                g = g0 + i
                xt = xts[g % 2]
                DMA("sp", xt[:], src[g * 128:(g + 1) * 128, :], [srcb[g]], [xt])
                if MOE_CUT >= -1:
                    make_hT(xt, scp, shb, h2T[:, :, i * 128:(i + 1) * 128], h2T, trp, work, h2Tf if MOE_CUT >= 0 else None, h2Tf)
                if MOE_CUT <= 0:
                    continue
                for kc in range(8):
                    MM(lgp[:, 0:36], h2Tf[:, kc, :], wr[:, kc, :], kc == 0, kc == 7, [h2Tf, wr], [lgp])
                TT("dve", lg[:], lgp[:, 0:36], rbias[:], ALU.add, [lgp, rbias], [lg])
                if MOE_CUT == 1:
                    continue
                gl = lg[:, 0:4]
                el = lg[:, 4:36].rearrange("p (g j) -> p g j", j=8)
                P.op("dve", lambda e, gl=gl: e.tensor_reduce(out=sm[:, 0:1], in_=gl, axis=AX.X, op=ALU.max), nrm([lg]), nrm([sm]))
                TS("dve", oh[:], gl, sm[:, 0:1], None, ALU.is_equal, None, [lg, sm], [oh])
                TS("dve", sm[:, 1:2], sm[:, 0:1], -1.0, None, ALU.mult, None, [sm], [sm])
                ACT(ejunk[:], gl, AF.Exp, [lg, sm], [ejunk, sm], bias=sm[:, 1:2], accum_out=sm[:, 2:3])
                P.op("dve", lambda e: e.reciprocal(out=sm[:, 3:4], in_=sm[:, 2:3]), nrm([sm]), nrm([sm]))
                P.op("dve", lambda e, el=el: e.tensor_reduce(out=m1[:], in_=el, axis=AX.X, op=ALU.max), nrm([lg]), nrm([m1]))
                TT("dve", mk1[:], el, bc48(m1[:]), ALU.is_equal, [lg, m1], [mk1])
                STT(el2[:], mk1[:], -1.0e30, el, ALU.mult, ALU.add, [mk1, lg], [el2])
                P.op("dve", lambda e: e.tensor_reduce(out=m2[:], in_=el2[:], axis=AX.X, op=ALU.max), nrm([el2]), nrm([m2]))
                TT("dve", mk2[:], el2[:], bc48(m2[:]), ALU.is_equal, [el2, m2], [mk2])
                TT("dve", dd[:], m2[:], m1[:], ALU.subtract, [m1, m2], [dd])
                ACT(dd[:], dd[:], AF.Exp, [dd], [dd])
                TS("dve", c1[:], dd[:], 1.0, None, ALU.add, None, [dd], [c1])
                P.op("dve", lambda e: e.reciprocal(out=c1[:], in_=c1[:]), nrm([c1]), nrm([c1]))
                TT("dve", c2[:], dd[:], c1[:], ALU.mult, [dd, c1], [c2])
                TS("dve", oh[:], oh[:], sm[:, 3:4], None, ALU.mult, None, [oh, sm], [oh])
                TT("dve", c1[:], c1[:], oh[:], ALU.mult, [c1, oh], [c1])
                TT("dve", c2[:], c2[:], oh[:], ALU.mult, [c2, oh], [c2])
                TT("dve", mk1[:], mk1[:], bc48(c1[:]), ALU.mult, [mk1, c1], [mk1])
                TT("dve", mk2[:], mk2[:], bc48(c2[:]), ALU.mult, [mk2, c2], [mk2])
                TT("dve", gates[:, i, :].rearrange("p (g j) -> p g j", j=8), mk1[:], mk2[:], ALU.add, [mk1, mk2], [gates])
            if DEBUG:
                dbg("gates", gates[:], [gates])
            nsub = 0
            for e in range(NE if MOE_STAGE >= 2 else 0):
                wbe = wb[e % 2]
                DMA("pool", wbe["w1"][:], W["moe_w1"][l, e].rearrange("(kc p) n -> p kc n", p=128), [], [wbe["w1"]])
                DMA("pool", wbe["w3"][:], W["moe_w3"][l, e].rearrange("(kc p) n -> p kc n", p=128), [], [wbe["w3"]])
                DMA("pool", wbe["w2"][:], W["moe_w2"][l, e].rearrange("(kc p) n -> p kc n", p=128), [], [wbe["w2"]], max_dma_last_dim=4096)
                for sg in range(GT // SGT):
                    ncol = SGT * 128
                    c0 = sg * ncol
                    gt_ = gT[nsub % 2]
                    nsub += 1
                    for fc in range(4):
                        a1, a3 = hp1[fc % 2], hp3[fc % 2]
                        for kc in range(8):
                            MM(a1[:, 0:ncol], wbe["w1"][:, kc, fc * 128:(fc + 1) * 128], h2T[:, kc, c0:c0 + ncol], kc == 0, kc == 7, [wbe["w1"], h2T], [a1])
                        for kc in range(8):
                            MM(a3[:, 0:ncol], wbe["w3"][:, kc, fc * 128:(fc + 1) * 128], h2T[:, kc, c0:c0 + ncol], kc == 0, kc == 7, [wbe["w3"], h2T], [a3])
                        sl = slt[fc % 2]
                        ACT(sl[:, 0:ncol], a1[:, 0:ncol], AF.Silu, [a1], [sl])
                        TT("dve", gt_[:, fc, 0:ncol], sl[:, 0:ncol], a3[:, 0:ncol], ALU.mult, [sl, a3], [gt_])
                    for ti in range(SGT):
                        i = sg * SGT + ti
                        for half in range(2):
                            ypp = yp[half]
                            for fc in range(4):
                                MM(ypp[:], gt_[:, fc, ti * 128:(ti + 1) * 128], wbe["w2"][:, fc, half * 512:(half + 1) * 512], fc == 0, fc == 3, [gt_, wbe["w2"]], [ypp])
                            ya = yacc[:, i, half * 512:(half + 1) * 512]
                            STT(ya, ypp[:], gates[:, i, e:e + 1], ya, ALU.mult, ALU.add, [ypp, gates, yaccb[i]], [yaccb[i]])
            for i in range(GT):
                g = g0 + i
                xt = xts[g % 2]
                DMA("sp", xt[:], src[g * 128:(g + 1) * 128, :], [srcb[g]], [xt])
                resid_ln(xt, [(yacc[:, i, 0:512], yaccb[i]), (yacc[:, i, 512:1024], yaccb[i])], gbc, lng, lnb, rr, stat,
                         dst[g * 128:(g + 1) * 128, :], dstb[g])
        P.pop()

    def phase_moe(l, src, srcb, dst, dstb):
        P.push()
        NTT = NSEQ * NT
        TB = 512
        NBLK = (2 * NTOK) // TB + 32
        wr = Tl("wr", [128, 8, 36], F32)
        DMA("sp", wr[:], W["router_w"][l], [], [wr])
        rbias = Tl("rbias", [128, 36], F32)
        DMA("sp", rbias[:], W["router_b"][l:l + 1, :].partition_broadcast(128), [], [rbias])
        lng = Tl("lng", [128, 1024], F32)
        lnb = Tl("lnb", [128, 1024], F32)
        DMA("sp", lng[:], W["ln_g"][l, 1:2, :].partition_broadcast(128), [], [lng])
        DMA("sp", lnb[:], W["ln_b"][l, 1:2, :].partition_broadcast(128), [], [lnb])
        widx_c = Tl("widx_c", [128, 12], F32)
        DMA("sp", widx_c[:], CD["widx"], [], [widx_c])
        utri = Tl("utri", [128, 128], BF16)
        ones = Tl("ones", [128, 128], BF16)
        DMA("pool", utri[:], CD["utri"], [], [utri])
        MS("pool", ones[:], 1.0, [ones])
        scp = Tl("scp", [128, 1024], F32)
        shb = Tl("shb", [128, 1024], F32)
        gbc = Tl("gbc", [128, 1024], F32)
        xts = [Tl("xt%d" % i, [128, 1024], F32) for i in range(2)]
        work = Tl("work", [128, 1024], F32)
        rr = Tl("rr", [128, 1024], F32)
        stat = Tl("stat", [128, 32], F32)
        h2Tf = Tl("h2Tf", [128, 8, 128], F32)
        h2b = [Tl("h2b%d" % i, [128, 1024], BF16) for i in range(2)]
        m1all = Tl("m1all", [128, NTT, 32], F32)
        m2all = Tl("m2all", [128, NTT, 32], F32)
        rkall = Tl("rkall", [128, NTT, 32], F32)
        wab = Tl("wab", [128, NTT, 2], F32)
        slots = Tl("slots", [128, NTT, 2], I32)
        cum = Tl("cum", [128, 32], F32)
        MS("pool", cum[:], 0.0, [cum])
        mb16 = Tl("mb16", [128, 32], BF16)
        bankA = [Tl("mbk%d" % i, [128, 512], F32, psum=True) for i in range(7)]
        trp = Tl("trp", [128, 4, 128], F32, psum=True)
        lgp, rkp, csp = bankA[0], bankA[1], bankA[2]
        lg = Tl("lg", [128, 36], F32)
        sm = Tl("rsm", [128, 16], F32)
        oh = Tl("oh", [128, 4], F32)
        ejunk = Tl("ejunk", [128, 4], F32)
        m1 = Tl("m1", [128, 4], F32)
        m2 = Tl("m2", [128, 4], F32)
        dd = Tl("dd", [128, 4], F32)
        c1 = Tl("c1", [128, 4], F32)
        c2 = Tl("c2", [128, 4], F32)
        mk1 = Tl("mk1", [128, 4, 8], F32)
        mk2 = Tl("mk2", [128, 4, 8], F32)
        el2 = Tl("el2", [128, 4, 8], F32)
        h2_d = moe_h2_d
        h2db = [Buf() for _ in range(NTT)]

        def bc48(ap):
            return ap.unsqueeze(2).to_broadcast([128, 4, 8])

        for g in range(NTT):
            s = g // NT
            if g % NT == 0:
                load_mod_bc(scp, l, s, 4, plus1=True)
                load_mod_bc(shb, l, s, 3)
                load_mod_bc(gbc, l, s, 5)
            xt = xts[g % 2]
            DMA("sp", xt[:], src[g * 128:(g + 1) * 128, :], [srcb[g]], [xt])
            TT("dve", work[:], xt[:], scp[:], ALU.mult, [xt, scp], [work])
            TT("pool", work[:], work[:], shb[:], ALU.add, [work, shb], [work])
            hb = h2b[g % 2]
            ACT(hb[:], work[:], AF.Copy, [work], [hb])
            DMA("sp", h2_d[g * 128:(g + 1) * 128, :], hb[:], [hb], [h2db[g]])
            for half in range(2):
                for i in range(4):
                    kc = half * 4 + i
                    TR(trp[:, i, :], work[:, kc * 128:(kc + 1) * 128], identf[:], [work, identf], [trp])
                if half == 0:
                    CP("dve", h2Tf[:, 0:4, :], trp[:], [trp], [h2Tf])
                else:
                    ACT(h2Tf[:, 4:8, :], trp[:], AF.Copy, [trp], [h2Tf])
            for kc in range(8):
                MM(lgp[:, 0:36], h2Tf[:, kc, :], wr[:, kc, :], kc == 0, kc == 7, [h2Tf, wr], [lgp])
            TT("dve", lg[:], lgp[:, 0:36], rbias[:], ALU.add, [lgp, rbias], [lg])
            gl = lg[:, 0:4]
            el = lg[:, 4:36].rearrange("p (g j) -> p g j", j=8)
            P.op("dve", lambda e, gl=gl: e.tensor_reduce(out=sm[:, 0:1], in_=gl, axis=AX.X, op=ALU.max), nrm([lg]), nrm([sm]))
            TS("dve", oh[:], gl, sm[:, 0:1], None, ALU.is_equal, None, [lg, sm], [oh])
            TS("dve", sm[:, 1:2], sm[:, 0:1], -1.0, None, ALU.mult, None, [sm], [sm])
            ACT(ejunk[:], gl, AF.Exp, [lg, sm], [ejunk, sm], bias=sm[:, 1:2], accum_out=sm[:, 2:3])
            P.op("dve", lambda e: e.reciprocal(out=sm[:, 3:4], in_=sm[:, 2:3]), nrm([sm]), nrm([sm]))
            P.op("dve", lambda e, el=el: e.tensor_reduce(out=m1[:], in_=el, axis=AX.X, op=ALU.max), nrm([lg]), nrm([m1]))
            TT("dve", mk1[:], el, bc48(m1[:]), ALU.is_equal, [lg, m1], [mk1])
            STT(el2[:], mk1[:], -1.0e30, el, ALU.mult, ALU.add, [mk1, lg], [el2])
            P.op("dve", lambda e: e.tensor_reduce(out=m2[:], in_=el2[:], axis=AX.X, op=ALU.max), nrm([el2]), nrm([m2]))
            TT("dve", mk2[:], el2[:], bc48(m2[:]), ALU.is_equal, [el2, m2], [mk2])
            TT("dve", dd[:], m2[:], m1[:], ALU.subtract, [m1, m2], [dd])
            ACT(dd[:], dd[:], AF.Exp, [dd], [dd])
            TS("dve", c1[:], dd[:], 1.0, None, ALU.add, None, [dd], [c1])
            P.op("dve", lambda e: e.reciprocal(out=c1[:], in_=c1[:]), nrm([c1]), nrm([c1]))
            TT("dve", c2[:], dd[:], c1[:], ALU.mult, [dd, c1], [c2])
            m1g = m1all[:, g, :].rearrange("p (g j) -> p g j", j=8)
            m2g = m2all[:, g, :].rearrange("p (g j) -> p g j", j=8)
            TT("dve", m1g, mk1[:], bc48(oh[:]), ALU.mult, [mk1, oh], [m1all])
            TT("dve", m2g, mk2[:], bc48(oh[:]), ALU.mult, [mk2, oh], [m2all])
            TT("dve", c1[:], c1[:], oh[:], ALU.mult, [c1, oh], [c1])
            TT("dve", c2[:], c2[:], oh[:], ALU.mult, [c2, oh], [c2])
            P.op("dve", lambda e: e.tensor_reduce(out=sm[:, 4:5], in_=c1[:], axis=AX.X, op=ALU.add), nrm([c1]), nrm([sm]))
            P.op("dve", lambda e: e.tensor_reduce(out=sm[:, 5:6], in_=c2[:], axis=AX.X, op=ALU.add), nrm([c2]), nrm([sm]))
            TS("dve", wab[:, g, :], sm[:, 4:6], sm[:, 3:4], None, ALU.mult, None, [sm], [wab])
            TT("dve", mb16[:], m1all[:, g, :], m2all[:, g, :], ALU.add, [m1all, m2all], [mb16])
            MM(rkp[:, 0:32], utri[:], mb16[:], True, True, [utri, mb16], [rkp])
            MM(csp[:, 0:32], ones[:], mb16[:], True, True, [ones, mb16], [csp])
            TT("dve", rkall[:, g, :], rkp[:, 0:32], cum[:], ALU.add, [rkp, cum], [rkall])
            TT("dve", cum[:], cum[:], csp[:, 0:32], ALU.add, [cum, csp], [cum])

        if MOE_CUT >= 2:
            pass
        pad = Tl("pad", [128, 32], F32)
        padi = Tl("padi", [128, 32], I32)
        pend = Tl("pend", [128, 32], F32)
        pstart = Tl("pstart", [128, 32], F32)
        onesf = Tl("onesf", [128, 32], F32)
        MS("pool", onesf[:], 1.0, [onesf])
        CP("dve", padi[:], cum[:], [cum], [padi])
        TS("dve", padi[:], padi[:], TB - 1, None, ALU.add, None, [padi], [padi])
        TS("dve", padi[:], padi[:], 9, None, ALU.arith_shift_right, None, [padi], [padi])
        TS("dve", padi[:], padi[:], 9, None, ALU.logical_shift_left, None, [padi], [padi])
        CP("dve", pad[:], padi[:], [padi], [pad])
        P.op("dve", lambda e: e.tensor_tensor_scan(out=pend[:], data0=onesf[:], data1=pad[:], initial=0.0, op0=ALU.mult, op1=ALU.add),
             nrm([onesf, pad]), nrm([pend]))
        TT("dve", pstart[:], pend[:], pad[:], ALU.subtract, [pend, pad], [pstart])
        bstart = Tl("bstart", [128, NBLK], F32)
        DMA("sp", bstart[:], CD["bstart"][0:1, 0:NBLK].partition_broadcast(128), [], [bstart])
        cmp_ = Tl("cmpb", [128, NBLK, 32], F32)
        bexp = Tl("bexp", [128, NBLK], F32)
        TT("dve", cmp_[:], pend[:].unsqueeze(1).to_broadcast([128, NBLK, 32]), bstart[:].unsqueeze(2).to_broadcast([128, NBLK, 32]),
           ALU.is_le, [pend, bstart], [cmp_])
        P.op("dve", lambda e: e.tensor_reduce(out=bexp[:], in_=cmp_[:], axis=AX.X, op=ALU.add), nrm([cmp_]), nrm([bexp]))
        TS("dve", bexp[:], bexp[:], 31.0, None, ALU.min, None, [bexp], [bexp])
        widf = Tl("widf", [128, NBLK, 12], F32)
        widi = Tl("widi", [128, NBLK, 12], I32)
        TS("dve", bexp[:], bexp[:], float(l * 32), None, ALU.add, None, [bexp], [bexp])
        STT(widf[:, :, 0:8], bexp[:].unsqueeze(2).to_broadcast([128, NBLK, 8]), 1024.0,
            widx_c[:, 0:8].unsqueeze(1).to_broadcast([128, NBLK, 8]), ALU.mult, ALU.add, [bexp, widx_c], [widf])
        STT(widf[:, :, 8:12], bexp[:].unsqueeze(2).to_broadcast([128, NBLK, 4]), 512.0,
            widx_c[:, 8:12].unsqueeze(1).to_broadcast([128, NBLK, 4]), ALU.mult, ALU.add, [bexp, widx_c], [widf])
        CP("dve", widi[:], widf[:], [widf], [widi])

        dtmp = Tl("dtmp", [128, 32], F32)
        dtmp2 = Tl("dtmp2", [128, 32], F32)
        slf = Tl("slf", [128, 2], F32)
        xbufb = [Buf() for _ in range(2 * NTT)]
        for g in range(NTT if MOE_CUT >= 3 else 0):
            TT("dve", dtmp[:], rkall[:, g, :], pstart[:], ALU.add, [rkall, pstart], [dtmp])
            TT("dve", dtmp2[:], dtmp[:], m1all[:, g, :], ALU.mult, [dtmp, m1all], [dtmp2])
            P.op("dve", lambda e: e.tensor_reduce(out=slf[:, 0:1], in_=dtmp2[:], axis=AX.X, op=ALU.add), nrm([dtmp2]), nrm([slf]))
            TT("dve", dtmp2[:], dtmp[:], m2all[:, g, :], ALU.mult, [dtmp, m2all], [dtmp2])
            P.op("dve", lambda e: e.tensor_reduce(out=slf[:, 1:2], in_=dtmp2[:], axis=AX.X, op=ALU.add), nrm([dtmp2]), nrm([slf]))
            CP("dve", slots[:, g, :], slf[:], [slf], [slots])
            hb = h2b[g % 2]
            DMA("sp", hb[:], h2_d[g * 128:(g + 1) * 128, :], [h2db[g]], [hb])
            for k in range(2):
                P.dma("pool", lambda e, g=g, k=k, hb=hb: e.indirect_dma_start(
                    out=moe_xbuf_d[:, :], out_offset=bass.IndirectOffsetOnAxis(ap=slots[:, g, k:k + 1], axis=0),
                    in_=hb[:], in_offset=None), nrm([hb, slots]), [xbufb[2 * g + k]])

        wb = [dict(w1=Tl("w1_%d" % i, [128, 8, 512], BF16), w3=Tl("w3_%d" % i, [128, 8, 512], BF16),
                   w2=Tl("w2_%d" % i, [128, 4, 1024], BF16)) for i in range(2)]
        xg = [Tl("xg%d" % i, [128, 4, 1024], BF16) for i in range(2)]
        xT = [Tl("xTb%d" % i, [128, 8, 512], BF16) for i in range(2)]
        gT = [Tl("gT%d" % i, [128, 4, 512], BF16) for i in range(2)]
        slt = [Tl("slt%d" % i, [128, 512], F32) for i in range(2)]
        yt = [Tl("ytb%d" % i, [128, 1024], BF16) for i in range(2)]
        tpb = bankA[0]
        tpv = tpb[:].bitcast(BF16).rearrange("p (r c) -> p r c", c=512)
        hp1 = [bankA[1], bankA[2]]
        hp3 = [bankA[3], bankA[4]]
        yp = [bankA[5], bankA[6]]
        ybufb = [Buf() for _ in range(NBLK)]
        w1rows = W["moe_w1"].rearrange("l e k f -> (l e k) f")
        w3rows = W["moe_w3"].rearrange("l e k f -> (l e k) f")
        w2rows = W["moe_w2"].rearrange("l e k f -> (l e k) f")
        nyt = 0
        for b in range(NBLK if MOE_CUT >= 4 else 0):
            wbe = wb[b % 2]
            for kc in range(8):
                P.dma("pool", lambda e, b=b, kc=kc, wbe=wbe: e.indirect_dma_start(
                    out=wbe["w1"][:, kc, :], out_offset=None, in_=w1rows,
                    in_offset=bass.IndirectOffsetOnAxis(ap=widi[:, b, kc:kc + 1], axis=0)), nrm([widi]), nrm([wbe["w1"]]))
                P.dma("pool", lambda e, b=b, kc=kc, wbe=wbe: e.indirect_dma_start(
                    out=wbe["w3"][:, kc, :], out_offset=None, in_=w3rows,
                    in_offset=bass.IndirectOffsetOnAxis(ap=widi[:, b, kc:kc + 1], axis=0)), nrm([widi]), nrm([wbe["w3"]]))
            for fc in range(4):
                P.dma("pool", lambda e, b=b, fc=fc, wbe=wbe: e.indirect_dma_start(
                    out=wbe["w2"][:, fc, :], out_offset=None, in_=w2rows,
                    in_offset=bass.IndirectOffsetOnAxis(ap=widi[:, b, 8 + fc:9 + fc], axis=0)), nrm([widi]), nrm([wbe["w2"]]))
            xgb = xg[b % 2]
            DMA("sp", xgb[:], moe_xbuf_d[b * TB:(b + 1) * TB, :].rearrange("(j p) d -> p j d", p=128), xbufb, [xgb])
            xTb = xT[b % 2]
            for kc in range(8):
                for jj in range(4):
                    TR(tpv[:, kc % 2, jj * 128:(jj + 1) * 128], xgb[:, jj, kc * 128:(kc + 1) * 128], identb[:], [xgb, identb], [tpb])
                if kc % 2 == 0:
                    CP("dve", xTb[:, kc, :], tpv[:, kc % 2, :], [tpb], [xTb])
                else:
                    ACT(xTb[:, kc, :], tpv[:, kc % 2, :], AF.Copy, [tpb], [xTb])
            gt_ = gT[b % 2]
            for fc in range(4):
                a1, a3 = hp1[fc % 2], hp3[fc % 2]
                for kc in range(8):
                    MM(a1[:], wbe["w1"][:, kc, fc * 128:(fc + 1) * 128], xTb[:, kc, :], kc == 0, kc == 7, [wbe["w1"], xTb], [a1])
                for kc in range(8):
                    MM(a3[:], wbe["w3"][:, kc, fc * 128:(fc + 1) * 128], xTb[:, kc, :], kc == 0, kc == 7, [wbe["w3"], xTb], [a3])
                sl = slt[fc % 2]
                ACT(sl[:], a1[:], AF.Silu, [a1], [sl])
                TT("dve", gt_[:, fc, :], sl[:], a3[:], ALU.mult, [sl, a3], [gt_])
            for ti in range(4):
                ytt = yt[nyt % 2]
                nyt += 1
                for half in range(2):
                    ypp = yp[half]
                    for fc in range(4):
                        MM(ypp[:], gt_[:, fc, ti * 128:(ti + 1) * 128], wbe["w2"][:, fc, half * 512:(half + 1) * 512], fc == 0, fc == 3, [gt_, wbe["w2"]], [ypp])
                    if half == 0:
                        ACT(ytt[:, 0:512], ypp[:], AF.Copy, [ypp], [ytt])
                    else:
                        CP("dve", ytt[:, 512:1024], ypp[:], [ypp], [ytt])
                r0 = b * TB + ti * 128
                DMA("sp", moe_ybuf_d[r0:r0 + 128, :], ytt[:], [ytt], [ybufb[b]])

        ya = [Tl("yga%d" % i, [128, 1024], BF16) for i in range(2)]
        yb_ = [Tl("ygb%d" % i, [128, 1024], BF16) for i in range(2)]
        ycomb = Tl("ycomb", [128, 1024], F32)
        for g in range(NTT):
            s = g // NT
            if g % NT == 0:
                load_mod_bc(gbc, l, s, 5)
            xt = xts[g % 2]
            DMA("sp", xt[:], src[g * 128:(g + 1) * 128, :], [srcb[g]], [xt])
            ga, gb_ = ya[g % 2], yb_[g % 2]
            if MOE_CUT < 5:
                MS("pool", ga[:], 0.0, [ga])
                MS("pool", gb_[:], 0.0, [gb_])
            for k, dstt in (((0, ga), (1, gb_)) if MOE_CUT >= 5 else ()):
                P.dma("pool", lambda e, g=g, k=k, dstt=dstt: e.indirect_dma_start(
                    out=dstt[:], out_offset=None, in_=moe_ybuf_d[:, :],
                    in_offset=bass.IndirectOffsetOnAxis(ap=slots[:, g, k:k + 1], axis=0)), nrm(ybufb + [slots]), nrm([dstt]))
            TS("dve", ycomb[:], ga[:], wab[:, g, 0:1], None, ALU.mult, None, [ga, wab], [ycomb])
            STT(ycomb[:], gb_[:], wab[:, g, 1:2], ycomb[:], ALU.mult, ALU.add, [gb_, wab, ycomb], [ycomb])
            resid_ln(xt, [(ycomb[:, 0:512], ycomb), (ycomb[:, 512:1024], ycomb)], gbc, lng, lnb, rr, stat,
                     dst[g * 128:(g + 1) * 128, :], dstb[g])
        P.pop()

    def phase_attn(l, src, srcb, dst, dstb):
        import math
        j = l // 2
        lam_init = 0.8 - 0.6 * math.exp(-0.3 * l)
        QB = min(512, S)
        NQB = QB // 128
        NSB = S // QB
        P.push()
        banks = [Tl("bk%d" % i, [128, 512], F32, psum=True) for i in range(8)]

        class TrV:
            def __init__(self, bank):
                self.b = bank.b
                self.v = bank[:].rearrange("p (i t) -> p i t", t=128)

            def __getitem__(self, k):
                return self.v[k]
        trp_t = banks[0]
        w_out = Tl("aw_out", [128, 8, 1024], BF16)
        DMA("pool", w_out[:], W["attn_w_out"][j].rearrange("(kc p) n -> p kc n", p=128), [], [w_out], max_dma_last_dim=4096)
        lng = Tl("lng", [128, 1024], F32)
        lnb = Tl("lnb", [128, 1024], F32)
        DMA("sp", lng[:], W["ln_g"][l, 0:1, :].partition_broadcast(128), [], [lng])
        DMA("sp", lnb[:], W["ln_b"][l, 0:1, :].partition_broadcast(128), [], [lnb])
        subw = Tl("subw", [128, 128], F32)
        DMA("sp", subw[:], W["attn_subln_w"][j:j + 1, :].partition_broadcast(128), [], [subw])
        TS("dve", subw[:], subw[:], 1.0 - lam_init, None, ALU.mult, None, [subw], [subw])
        lamt = Tl("lamt", [128, 256], F32)
        lams = Tl("lams", [128, 8], F32)
        DMA("sp", lamt[:], W["attn_lambda"][j:j + 1].rearrange("o a d -> o (a d)").partition_broadcast(128), [], [lamt])
        TT("dve", lamt[:, 0:64], lamt[:, 0:64], lamt[:, 64:128], ALU.mult, [lamt], [lamt])
        TT("dve", lamt[:, 128:192], lamt[:, 128:192], lamt[:, 192:256], ALU.mult, [lamt], [lamt])
        P.op("dve", lambda e: e.tensor_reduce(out=lams[:, 0:1], in_=lamt[:, 0:64], axis=AX.X, op=ALU.add), nrm([lamt]), nrm([lams]))
        P.op("dve", lambda e: e.tensor_reduce(out=lams[:, 1:2], in_=lamt[:, 128:192], axis=AX.X, op=ALU.add), nrm([lamt]), nrm([lams]))
        ACT(lams[:, 0:2], lams[:, 0:2], AF.Exp, [lams], [lams])
        TT("dve", lams[:, 2:3], lams[:, 0:1], lams[:, 1:2], ALU.subtract, [lams], [lams])
        TS("dve", lams[:, 2:3], lams[:, 2:3], lam_init, None, ALU.add, None, [lams], [lams])
        lam = lams[:, 2:3]
        cosT = Tl("cosT", [128, S], F32)
        sinT = Tl("sinT", [128, S], F32)
        DMA("sp", cosT[:], CD["ropecos"], [], [cosT])
        DMA("sp", sinT[:], CD["ropesin"], [], [sinT])
        scp = Tl("scp", [128, 1024], F32)
        shb = Tl("shb", [128, 1024], F32)
        gbc = Tl("gbc", [128, 1024], F32)
        xts = [Tl("xt%d" % i, [128, 1024], F32) for i in range(2)]
        work = Tl("work", [128, 1024], F32)
        rr = Tl("rr", [128, 1024], F32)
        stat = Tl("stat", [128, 32], F32)
        hTall = Tl("hTall", [128, 8, S], BF16)
        qT = Tl("aqT", [128, S], BF16)
        kT = Tl("akT", [128, S], BF16)
        vext = Tl("vext", [128, NT, 129], BF16)
        MS("pool", vext[:, :, 128:129], 1.0, [vext])
        wsl = {nm: Tl("aw_" + nm, [128, 8, 128], BF16) for nm in ("q", "qs", "k", "ks", "v")}
        t1 = Tl("rt1", [128, 512], F32)
        t2 = Tl("rt2", [128, 512], F32)
        pT = [[Tl("pT%d_%d" % (i, m), [128, 512], BF16) for m in range(2)] for i in range(2)]
        rs = Tl("ars", [128, 8], F32)
        ot = Tl("aot", [128, 128], F32)
        ot2 = Tl("aot2", [128, 128], F32)
        o1s = Tl("ao1s", [128, 4, 128], F32)
        junk = Tl("ajunk", [128, 128], F32)
        onb16 = [Tl("aon%d" % i, [128, 128], BF16) for i in range(2)]
        ont = Tl("aont", [128, 1024], BF16)
        onT = Tl("aonT", [128, 8, 128], BF16)
        win = W["attn_w_in"][j]
        nst = 0
        for s in range(NSEQ):
            load_mod_bc(scp, l, s, 1, plus1=True)
            load_mod_bc(shb, l, s, 0)
            load_mod_bc(gbc, l, s, 2)
            for t in range(NT):
                g = s * NT + t
                xt = xts[g % 2]
                DMA("sp", xt[:], src[g * 128:(g + 1) * 128, :], [srcb[g]], [xt])
                make_hT(xt, scp, shb, hTall[:, :, t * 128:(t + 1) * 128], hTall, TrV(banks[0]), work)
            for h in range(8):
                def wv3(c0, n):
                    return win[:, c0:c0 + n].rearrange("(kc p) n -> p kc n", p=128)
                DMA("pool", wsl["q"][:], wv3(h * 128, 128), [], [wsl["q"]])
                DMA("pool", wsl["k"][:], wv3(1024 + h * 128, 128), [], [wsl["k"]])
                DMA("pool", wsl["v"][:], wv3(2048 + h * 128, 128), [], [wsl["v"]])
                for nm, base in (("qs", 0), ("ks", 1024)):
                    for m in range(2):
                        b0 = base + h * 128 + m * 64
                        DMA("pool", wsl[nm][:, :, m * 64:m * 64 + 32], wv3(b0 + 32, 32), [], [wsl[nm]])
                        DMA("pool", wsl[nm][:, :, m * 64 + 32:m * 64 + 64], wv3(b0, 32), [], [wsl[nm]])
                for nb in range(NSB):
                    cs = slice(nb * QB, (nb + 1) * QB)
                    for (wn, wsn, dstT, bi) in (("q", "qs", qT, 2), ("k", "ks", kT, 2)):
                        pa, pb = banks[bi], banks[bi + 1]
                        for kc in range(8):
                            MM(pa[:, 0:QB], wsl[wn][:, kc, :], hTall[:, kc, cs], kc == 0, kc == 7, [wsl[wn], hTall], [pa])
                        for kc in range(8):
                            MM(pb[:, 0:QB], wsl[wsn][:, kc, :], hTall[:, kc, cs], kc == 0, kc == 7, [wsl[wsn], hTall], [pb])
                        TT("dve", t1[:, 0:QB], pa[:, 0:QB], cosT[:, cs], ALU.mult, [pa, cosT], [t1])
                        TT("dve", t2[:, 0:QB], pb[:, 0:QB], sinT[:, cs], ALU.mult, [pb, sinT], [t2])
                        TT("pool", dstT[:, cs], t1[:, 0:QB], t2[:, 0:QB], ALU.add, [t1, t2], [dstT])
                for t in range(NT):
                    pv = banks[4 + (t % 2)]
                    for kc in range(8):
                        MM(pv[:, 0:128], hTall[:, kc, t * 128:(t + 1) * 128], wsl["v"][:, kc, :], kc == 0, kc == 7, [hTall, wsl["v"]], [pv])
                    CP("dve", vext[:, t, 0:128], pv[:, 0:128], [pv], [vext])
                steps = []
                for Q in range(NSB):
                    for m in range(2):
                        for kb in range(Q * NQB + NQB):
                            steps.append((Q, m, kb))

                def emit_scores(i):
                    Q, m, kb = steps[i]
                    j0 = max(0, kb - Q * NQB)
                    csl = slice(j0 * 128, QB)
                    stb = banks[i % 2]
                    MM(stb[:, csl], kT[m * 64:(m + 1) * 64, kb * 128:(kb + 1) * 128], qT[m * 64:(m + 1) * 64, Q * QB + j0 * 128:(Q + 1) * QB],
                       True, True, [kT, qT], [stb])

                emit_scores(0)
                for i, (Q, m, kb) in enumerate(steps):
                    if i + 1 < len(steps):
                        emit_scores(i + 1)
                    j0 = max(0, kb - Q * NQB)
                    csl = slice(j0 * 128, QB)
                    stb = banks[i % 2]
                    pt = pT[i % 2][0]
                    ACT(pt[:, csl], stb[:, csl], AF.Exp, [stb], [pt], scale=0.125)
                    if kb >= Q * NQB:
                        dsl = slice(j0 * 128, (j0 + 1) * 128)
                        TT("pool", pt[:, dsl], pt[:, dsl], trimask[:], ALU.mult, [pt, trimask], [pt])
                    for jq in range(j0, NQB):
                        ab = banks[4 + jq]
                        MM(ab[:, 0:129], pt[:, jq * 128:(jq + 1) * 128], vext[:, kb, :], kb == 0, kb == Q * NQB + jq, [pt, vext], [ab])
                    if kb == Q * NQB + NQB - 1:
                        for jq in range(NQB):
                            ab = banks[4 + jq]
                            if m == 0:
                                P.op("dve", lambda e, ab=ab: e.reciprocal(out=rs[:, 0:1], in_=ab[:, 128:129]), nrm([ab]), nrm([rs]))
                                TS("dve", o1s[:, jq, :], ab[:, 0:128], rs[:, 0:1], None, ALU.mult, None, [ab, rs], [o1s])
                            else:
                                t = Q * NQB + jq
                                g = s * NT + t
                                P.op("dve", lambda e, ab=ab: e.reciprocal(out=rs[:, 1:2], in_=ab[:, 128:129]), nrm([ab]), nrm([rs]))
                                TS("dve", rs[:, 1:2], rs[:, 1:2], lam, None, ALU.mult, None, [rs, lams], [rs])
                                TS("dve", ot2[:], ab[:, 0:128], rs[:, 1:2], None, ALU.mult, None, [ab, rs], [ot2])
                                TT("dve", ot[:], o1s[:, jq, :], ot2[:], ALU.subtract, [o1s, ot2], [ot])
                                ACT(junk[:], ot[:], AF.Square, [ot], [junk, rs], accum_out=rs[:, 2:3])
                                ACT(rs[:, 3:4], rs[:, 2:3], AF.Sqrt, [rs], [rs], scale=1.0 / 128.0, bias=EPS)
                                P.op("dve", lambda e: e.reciprocal(out=rs[:, 3:4], in_=rs[:, 3:4]), nrm([rs]), nrm([rs]))
                                ob = onb16[jq % 2]
                                STT(ob[:], ot[:], rs[:, 3:4], subw[:], ALU.mult, ALU.mult, [ot, rs, subw], [ob])
                                DMA("sp", on_d[g * 128:(g + 1) * 128, h * 128:(h + 1) * 128], ob[:], [ob], [onb[g]])
            for t in range(NT):
                g = s * NT + t
                xt = xts[g % 2]
                DMA("sp", xt[:], src[g * 128:(g + 1) * 128, :], [srcb[g]], [xt])
                DMA("sp", ont[:], on_d[g * 128:(g + 1) * 128, :], [onb[g]], [ont])
                tb = banks[1]
                tbv = tb[:].bitcast(BF16).rearrange("p (h t) -> p h t", t=128)
                for h in range(8):
                    TR(tbv[:, h, :], ont[:, h * 128:(h + 1) * 128], identb[:], [ont, identb], [tb])
                CP("dve", onT[:, 0:4, :], tbv[:, 0:4, :], [tb], [onT])
                ACT(onT[:, 4:8, :], tbv[:, 4:8, :], AF.Copy, [tb], [onT])
                for half in range(2):
                    yb = banks[2 + half]
                    for h in range(8):
                        MM(yb[:], onT[:, h, :], w_out[:, h, half * 512:(half + 1) * 512], h == 0, h == 7, [onT, w_out], [yb])
                resid_ln(xt, [(banks[2][:], banks[2]), (banks[3][:], banks[3])], gbc, lng, lnb, rr, stat, dst[g * 128:(g + 1) * 128, :], dstb[g])
        P.pop()

    P.push()
    ztile = Tl("ztile", [128, 8192], BF16)
    MS("pool", ztile[:], 0.0, [ztile])
    zb = Buf("xbufzero")
    nrows = MOE_NBLK * 512
    r0 = 0
    while r0 < nrows:
        nr = min(1024, nrows - r0)
        DMA("sp", moe_xbuf_d[r0:r0 + nr, :].rearrange("(p j) d -> p (j d)", p=128), ztile[:, 0:(nr // 128) * 1024], [ztile], [zb])
        r0 += nr
    P.pop()
    phase_mod()
    P.barrier()
    cur, curb = x_d, xb["x"]
    for li, l in enumerate(layers):
        last = (li == len(layers) - 1)
        if "mix" in sub:
            dst, dstb = (out_d, xb["out"]) if (last and "moe" not in sub) else (xs[0], xb[0])
            if l % 2 == 0:
                phase_hgrn(l, cur, curb, dst, dstb)
            else:
                phase_attn(l, cur, curb, dst, dstb)
            cur, curb = dst, dstb
        if "moe" in sub:
            dst, dstb = (out_d, xb["out"]) if last else (xs[1], xb[1])
            phase_moe(l, cur, curb, dst, dstb)
            cur, curb = dst, dstb
    P.finish()
    P.DBG = DBG
    return nc, consts, P


_CACHE = {}


def run(inputs, NSEQ, S, layers, sub=("mix", "moe"), n_cores=8, NE=32):
    from concourse.bass_utils import run_bass_kernel_spmd
    nc, consts, P = build(NSEQ, S, layers, sub, NE)
    x = np.ascontiguousarray(inputs["x"], dtype=np.float32).reshape(n_cores, NSEQ * S, D)
    c = np.ascontiguousarray(inputs["c"], dtype=np.float32).reshape(n_cores, NSEQ, D)
    inputs = dict(inputs)
    rw = np.concatenate([np.asarray(inputs["router_g_w"], np.float32), np.asarray(inputs["router_e_w"], np.float32)], axis=2)
    inputs["router_w"] = np.ascontiguousarray(rw.reshape(rw.shape[0], 8, 128, 36).transpose(0, 2, 1, 3))
    inputs["router_b"] = np.concatenate([np.asarray(inputs["router_g_b"], np.float32), np.asarray(inputs["router_e_b"], np.float32)], axis=1)
    in_maps = []
    for i in range(n_cores):
        m = {"x": x[i], "cT": np.ascontiguousarray(c[i].reshape(NSEQ, 8, 128).transpose(2, 1, 0))}
        for nm, shp in wnames():
            m[nm] = np.ascontiguousarray(np.asarray(inputs[nm])[:shp[0]], dtype=np.float32)
        for nm, arr in consts.items():
            m[nm] = arr
        in_maps.append(m)
    res = run_bass_kernel_spmd(nc, in_maps, core_ids=list(range(n_cores)))
    out = np.stack([np.asarray(r["out"]) for r in res.results], 0)
    global LAST_DBG
    LAST_DBG = {k: np.asarray(res.results[0]["dbg_" + k]) for k in P.DBG}
    return out.reshape(n_cores * NSEQ, S, D)


def kernel(**inputs):
    out = run(inputs, 2, 4096, [0, 1, 2, 3])
    return out.astype(np.float32)
```

```python
import contextlib
import numpy as np
import concourse.bass as bass
import concourse.mybir as mybir

F32 = mybir.dt.float32
BF16 = mybir.dt.bfloat16
I32 = mybir.dt.int32
U32 = mybir.dt.uint32
AF = mybir.ActivationFunctionType
ALU = mybir.AluOpType
AX = mybir.AxisListType

QUEUES = ("pe", "act", "dve", "pool", "sp")
N_DMA_CH = 12


class Buf:
    __slots__ = ("name", "w", "r", "excl")

    def __init__(self, name="", excl=False):
        self.name = name
        self.excl = excl
        self.w = None
        self.r = []


class Prog:
    def __init__(self, nc, same_engine_sync=True):
        self.nc = nc
        self.stack = contextlib.ExitStack()
        self.ops = {q: [] for q in QUEUES}
        self.cnt = {q: 0 for q in QUEUES}
        self.sems = {}
        self.seen = {q: {} for q in QUEUES}
        self.same_engine_sync = same_engine_sync
        for q in QUEUES:
            self.sems["e_" + q] = self.stack.enter_context(nc.semaphore("e_" + q))
        self.dma_ch = {}
        self.dma_rr = {}
        for q in ("sp", "act", "pool"):
            chs = []
            for i in range(N_DMA_CH):
                key = "d_%s_%d" % (q, i)
                self.sems[key] = self.stack.enter_context(nc.semaphore(key))
                chs.append([key, 0])
            self.dma_ch[q] = chs
            self.dma_rr[q] = 0
        self.n_inst = 0
        self.uid = 0
        self.scopes = [self.stack]

    def sb(self, name, shape, dtype):
        self.uid += 1
        return self.scopes[-1].enter_context(self.nc.sbuf_tensor("sb%d_%s" % (self.uid, name), list(shape), dtype))

    def ps(self, name, shape, dtype=F32):
        self.uid += 1
        return self.scopes[-1].enter_context(self.nc.psum_tensor("ps%d_%s" % (self.uid, name), list(shape), dtype))

    def push(self):
        self.scopes.append(contextlib.ExitStack())

    def pop(self):
        self.barrier()
        self.scopes.pop().close()

    def barrier(self):
        targets = []
        for q in QUEUES:
            if self.cnt[q] > 0:
                targets.append(("e_" + q, self.cnt[q], q))
        for q, chs in self.dma_ch.items():
            for key, val in chs:
                if val > 0:
                    targets.append((key, val, None))
        sems = self.sems
        for q in QUEUES:
            seen = self.seen[q]
            waits = []
            for key, val, wq in targets:
                if wq == q and q != "sp":
                    continue
                if seen.get(key, 0) >= val:
                    continue
                seen[key] = val
                waits.append((key, val))

            def emit(eng, waits=waits):
                for k, v in waits:
                    eng.wait_ge(sems[k], v)

            self.ops[q].append(emit)
            self.n_inst += len(waits)

    def _collect(self, q, reads, writes, is_dma):
        waits = {}

        def need(tok):
            key, val, wq, wdma = tok
            if (not wdma) and (not is_dma) and wq == q:
                if q == "pe" or not self.same_engine_sync:
                    return
            if waits.get(key, 0) < val:
                waits[key] = val

        for b in reads:
            if b.w is not None:
                need(b.w)
        for b in writes:
            if b.w is not None:
                need(b.w)
            for t in b.r:
                need(t)
        out = []
        seen = self.seen[q]
        for key, val in waits.items():
            if seen.get(key, 0) >= val:
                continue
            seen[key] = val
            out.append((key, val))
        return out

    def _commit(self, tok, reads, writes):
        for b in reads:
            b.r.append(tok)
        for b in writes:
            b.w = tok
            b.r = []

    def op(self, q, fn, reads=(), writes=(), signal=True):
        ex = [b for b in reads if b.excl]
        if ex:
            writes = list(writes) + [b for b in ex if b not in writes]
        waits = self._collect(q, reads, writes, False)
        key = "e_" + q
        if signal:
            self.cnt[q] += 1
            val = self.cnt[q]
        else:
            val = self.cnt[q] + 1
        tok = (key, val, q, False)
        sems = self.sems

        def emit(eng, waits=waits, fn=fn, key=key, signal=signal):
            for k, v in waits[:-1]:
                eng.wait_ge(sems[k], v)
            ins = fn(eng)
            if waits:
                ins._wait_ge(sems[waits[-1][0]], waits[-1][1])
            if signal:
                ins.then_inc(sems[key], 1)

        self.ops[q].append(emit)
        self.n_inst += 1 + max(0, len(waits) - 1)
        self._commit(tok, reads, writes)
        return tok

    def dma(self, q, fn, reads=(), writes=()):
        waits = self._collect(q, reads, writes, True)
        chs = self.dma_ch[q]
        i = self.dma_rr[q]
        self.dma_rr[q] = (i + 1) % len(chs)
        ch = chs[i]
        key = ch[0]
        prev = ch[1]
        ch[1] += 16
        val = ch[1]
        seen = self.seen[q]
        if prev > 0 and seen.get(key, 0) < prev:
            seen[key] = prev
            waits = waits + [(key, prev)]
        tok = (key, val, q, True)
        sems = self.sems

        def emit(eng, waits=waits, fn=fn, key=key):
            for k, v in waits:
                eng.wait_ge(sems[k], v)
            fn(eng).then_inc(sems[key], 16)

        self.ops[q].append(emit)
        self.n_inst += 1 + len(waits)
        self._commit(tok, reads, writes)
        return tok

    def finish(self):
        nc = self.nc
        final = []
        for q, chs in self.dma_ch.items():
            for key, val in chs:
                if val > 0:
                    final.append((key, val))
        for q in QUEUES:
            if q != "sp" and self.cnt[q] > 0:
                final.append(("e_" + q, self.cnt[q]))
        sems = self.sems
        ops = self.ops
        with nc.Block() as block:
            @block.tensor
            def _(eng):
                for f in ops["pe"]:
                    f(eng)

            @block.scalar
            def _(eng):
                for f in ops["act"]:
                    f(eng)

            @block.vector
            def _(eng):
                for f in ops["dve"]:
                    f(eng)

            @block.gpsimd
            def _(eng):
                for f in ops["pool"]:
                    f(eng)

            @block.sync
            def _(eng):
                for f in ops["sp"]:
                    f(eng)
                for k, v in final:
                    eng.wait_ge(sems[k], v)
        self.stack.close()

DEBUG = False
MOE_STAGE = 2
MOE_CUT = 5
D = 1024
DEPTH = 4
ALPHA = (2 * DEPTH) ** 0.25
EPS = 1e-5
WDEPTH = 4


def wnames():
    L = WDEPTH
    return [("ada_w", [L, 1024, 6144]), ("ada_b", [L, 6144]), ("ln_g", [L, 2, 1024]), ("ln_b", [L, 2, 1024]),
            ("hgrn_w_in", [2, 1024, 4096]), ("hgrn_w_out", [2, 1024, 1024]), ("hgrn_lb", [2, 1024]),
            ("hgrn_norm_w", [2, 128]), ("attn_w_in", [2, 1024, 3072]), ("attn_w_out", [2, 1024, 1024]),
            ("attn_lambda", [2, 4, 64]), ("attn_subln_w", [2, 128]), ("router_w", [L, 128, 8, 36]), ("router_b", [L, 36]),
            ("moe_w1", [L, 32, 1024, 512]), ("moe_w3", [L, 32, 1024, 512]), ("moe_w2", [L, 32, 512, 1024])]


def make_consts(S):
    c = {}
    c["identf"] = np.eye(128, dtype=np.float32)
    s = np.arange(128)[:, None]
    t = np.arange(128)[None, :]
    c["blockmask"] = ((s // 32 == t // 32) & (s <= t)).astype(np.float32)
    c["trimask"] = (s <= t).astype(np.float32)
    sm = np.ones((128, 128), np.float32)
    sm[:, ::32] = 0.0
    c["scanmask"] = sm
    ci = np.zeros((128, 4), np.float32)
    for k in range(4):
        ci[k * 32:(k + 1) * 32, k] = 1.0
    c["chunkind"] = ci
    half = 32
    inv_freq = (np.float32(10000.0) ** (-np.arange(half, dtype=np.float32) / np.float32(half))).astype(np.float32)
    ang = (np.arange(S, dtype=np.float32)[:, None] * inv_freq[None, :]).astype(np.float32)
    cos = np.cos(ang).astype(np.float32).T
    sin = np.sin(ang).astype(np.float32).T
    c["utri"] = (s < t).astype(np.float32)
    wi = np.zeros((128, 12), np.float32)
    for kc in range(8):
        wi[:, kc] = kc * 128 + np.arange(128)
    for fc in range(4):
        wi[:, 8 + fc] = fc * 128 + np.arange(128)
    c["widx"] = wi
    c["bstart"] = (np.arange(128, dtype=np.float32) * 512.0)[None, :]
    c["ropecos"] = np.ascontiguousarray(np.concatenate([cos, cos, cos, cos], 0))
    c["ropesin"] = np.ascontiguousarray(np.concatenate([-sin, sin, -sin, sin], 0))
    return c


def build(NSEQ, S, layers, sub=("mix", "moe"), NE=32):
    NT = S // 128
    NTOK = NSEQ * S
    nc = bass.Bass("TRN2", target_bir_lowering=False)
    dtn = nc.dram_tensor
    x_d = dtn("x", [NTOK, D], F32, kind="ExternalInput").ap()
    c_d = dtn("cT", [128, 8, NSEQ], F32, kind="ExternalInput").ap()
    W = {}
    for nm, shp in wnames():
        W[nm] = dtn(nm, shp, F32, kind="ExternalInput").ap()
    consts = make_consts(S)
    CD = {}
    for nm, arr in consts.items():
        CD[nm] = dtn(nm, list(arr.shape), F32, kind="ExternalInput").ap()
    out_d = dtn("out", [NTOK, D], F32, kind="ExternalOutput").ap()
    xs = [dtn("xs0", [NTOK, D], F32, kind="Internal").ap(), dtn("xs1", [NTOK, D], F32, kind="Internal").ap()]
    mod_d = dtn("mod_d", [WDEPTH, NSEQ, 6144], F32, kind="Internal").ap()
    on_d = dtn("on_d", [NTOK, D], BF16, kind="Internal").ap()
    MOE_NBLK = (2 * NTOK) // 512 + 32
    moe_h2_d = dtn("moe_h2", [NTOK, D], BF16, kind="Internal").ap()
    moe_xbuf_d = dtn("moe_xbuf", [MOE_NBLK * 512, D], BF16, kind="Internal").ap()
    moe_ybuf_d = dtn("moe_ybuf", [MOE_NBLK * 512, D], BF16, kind="Internal").ap()

    P = Prog(nc)
    DBG = {}

    def dbg(name, ap, bufs, psum=False):
        if not DEBUG or name in DBG:
            return
        shp = list(ap.shape)
        d = dtn("dbg_" + name, shp, F32, kind="ExternalOutput").ap()
        DBG[name] = d
        if psum:
            tmp = P.sb("dbgtmp_" + name, shp, F32)
            tb = Buf()
            P.op("dve", lambda e: e.tensor_copy(out=tmp[:], in_=ap), nrm(bufs), [tb])
            P.dma("pool", lambda e: e.dma_start(out=d, in_=tmp[:]), [tb], [])
        else:
            P.dma("pool", lambda e: e.dma_start(out=d, in_=ap), nrm(bufs), [])

    def nrm(l):
        return [b.b if hasattr(b, "b") else b for b in l]

    def MM(out, lhsT, rhs, start, stop, r, w):
        P.op("pe", lambda e: e.matmul(out, lhsT=lhsT, rhs=rhs, start=start, stop=stop), nrm(r), nrm(w), signal=bool(stop))

    def TR(out, in_, ident, r, w):
        P.op("pe", lambda e: e.transpose(out=out, in_=in_, identity=ident), nrm(r), nrm(w))

    def ACT(out, in_, func, r, w, **kw):
        P.op("act", lambda e: e.activation(out=out, in_=in_, func=func, **kw), nrm(r), nrm(w))

    def TT(q, out, in0, in1, op, r, w):
        P.op(q, lambda e: e.tensor_tensor(out=out, in0=in0, in1=in1, op=op), nrm(r), nrm(w))

    def TS(q, out, in0, s1, s2, op0, op1, r, w):
        if op1 is None:
            P.op(q, lambda e: e.tensor_scalar(out=out, in0=in0, scalar1=s1, scalar2=None, op0=op0), nrm(r), nrm(w))
        else:
            P.op(q, lambda e: e.tensor_scalar(out=out, in0=in0, scalar1=s1, scalar2=s2, op0=op0, op1=op1), nrm(r), nrm(w))

    def STT(out, in0, scalar, in1, op0, op1, r, w):
        P.op("dve", lambda e: e.scalar_tensor_tensor(out=out, in0=in0, scalar=scalar, in1=in1, op0=op0, op1=op1), nrm(r), nrm(w))

    def CP(q, out, in_, r, w):
        P.op(q, lambda e: e.tensor_copy(out=out, in_=in_), nrm(r), nrm(w))

    def MS(q, ap, val, w):
        P.op(q, lambda e: e.memset(ap, val), [], nrm(w))

    def DMA(q, out, in_, r, w, **kw):
        P.dma(q, lambda e: e.dma_start(out=out, in_=in_, **kw), nrm(r), nrm(w))

    class Tl:
        def __init__(self, name, shape, dtype, psum=False):
            self.t = P.ps(name, shape, dtype) if psum else P.sb(name, shape, dtype)
            self.b = Buf(name, excl=psum)

        def __getitem__(self, k):
            return self.t[k]

    xb = {"x": [Buf() for _ in range(NSEQ * NT)], 0: [Buf() for _ in range(NSEQ * NT)],
          1: [Buf() for _ in range(NSEQ * NT)], "out": [Buf() for _ in range(NSEQ * NT)]}
    modb = Buf("mod_d")
    onb = [Buf() for _ in range(NSEQ * NT)]

    identf = Tl("identf", [128, 128], F32)
    identb = Tl("identb", [128, 128], BF16)
    blockmask = Tl("blockmask", [128, 128], F32)
    trimask = Tl("trimask", [128, 128], BF16)
    scanmask = Tl("scanmask", [128, 128], F32)
    chunkind = Tl("chunkind", [128, 4], F32)
    DMA("sp", identf[:], CD["identf"], [], [identf])
    DMA("pool", identb[:], CD["identf"], [], [identb])
    DMA("sp", blockmask[:], CD["blockmask"], [], [blockmask])
    DMA("pool", trimask[:], CD["trimask"], [], [trimask])
    DMA("sp", scanmask[:], CD["scanmask"], [], [scanmask])
    DMA("sp", chunkind[:], CD["chunkind"], [], [chunkind])

    def phase_mod():
        P.push()
        condT = Tl("condT", [128, 8, NSEQ], F32)
        DMA("sp", condT[:], c_d, [], [condT])
        ACT(condT[:], condT[:], AF.Silu, [condT], [condT])
        stg = [Tl("adastg%d" % i, [128, 8, 512], F32) for i in range(2)]
        adab = Tl("adab", [NSEQ, 6144], F32)
        modrow = Tl("modrow", [NSEQ, 6144], F32)
        mps = [Tl("modps%d" % i, [128, 512], F32, psum=True) for i in range(2)]
        n = 0
        for l in layers:
            DMA("sp", adab[:], W["ada_b"][l:l + 1, :].partition_broadcast(NSEQ) if NSEQ > 1 else W["ada_b"][l:l + 1, :], [], [adab])
            for j in range(12):
                st = stg[n % 2]
                pp = mps[n % 2]
                n += 1
                DMA("sp", st[:], W["ada_w"][l, :, j * 512:(j + 1) * 512].rearrange("(kc p) n -> p kc n", p=128), [], [st])
                for kc in range(8):
                    MM(pp[0:NSEQ, :], condT[:, kc, :], st[:, kc, :], kc == 0, kc == 7, [condT, st], [pp])
                TT("dve", modrow[:, j * 512:(j + 1) * 512], pp[0:NSEQ, :], adab[:, j * 512:(j + 1) * 512], ALU.add, [pp, adab], [modrow])
            DMA("sp", mod_d[l], modrow[:], [modrow], [modb])
        P.pop()

    def load_mod_bc(tl, l, s, j, plus1=False):
        DMA("sp", tl[:], mod_d[l, s:s + 1, j * 1024:(j + 1) * 1024].partition_broadcast(128), [modb], [tl])
        if plus1:
            TS("pool", tl[:], tl[:], 1.0, None, ALU.add, None, [tl], [tl])

    def make_hT(xt, scp, sh, hT_out_ap, hTbuf, trp, work, hTf_ap=None, hTfbuf=None):
        TT("dve", work[:], xt[:], scp[:], ALU.mult, [xt, scp], [work])
        TT("pool", work[:], work[:], sh[:], ALU.add, [work, sh], [work])
        for half in range(2):
            for i in range(4):
                kc = half * 4 + i
                TR(trp[:, i, :], work[:, kc * 128:(kc + 1) * 128], identf[:], [work, identf], [trp])
            if half == 0:
                ACT(hT_out_ap[:, 0:4, :], trp[:], AF.Copy, [trp], [hTbuf])
            else:
                CP("dve", hT_out_ap[:, 4:8, :], trp[:], [trp], [hTbuf])
            if hTf_ap is not None:
                if half == 0:
                    CP("dve", hTf_ap[:, 0:4, :], trp[:], [trp], [hTfbuf])
                else:
                    ACT(hTf_ap[:, 4:8, :], trp[:], AF.Copy, [trp], [hTfbuf])

    def resid_ln(xt, yps_list, gbc, lng, lnb, r, stat, dst_ap, dst_buf):
        for half in range(2):
            yap, ybuf = yps_list[half]
            sl = slice(half * 512, (half + 1) * 512)
            TT("dve", r[:, sl], yap, gbc[:, sl], ALU.mult, [ybuf, gbc], [r])
        STT(r[:], xt[:], ALPHA, r[:], ALU.mult, ALU.add, [xt, r], [r])
        for c4 in range(2):
            P.op("dve", lambda e, c4=c4: e.bn_stats(out=stat[:, c4 * 6:(c4 + 1) * 6], in_=r[:, c4 * 512:(c4 + 1) * 512]), nrm([r]), nrm([stat]))
        P.op("dve", lambda e: e.bn_aggr(out=stat[:, 12:14], in_=stat[:, 0:12]), nrm([stat]), nrm([stat]))
        TS("dve", stat[:, 14:15], stat[:, 13:14], EPS, None, ALU.add, None, [stat], [stat])
        ACT(stat[:, 14:15], stat[:, 14:15], AF.Sqrt, [stat], [stat])
        P.op("dve", lambda e: e.reciprocal(out=stat[:, 15:16], in_=stat[:, 14:15]), nrm([stat]), nrm([stat]))
        STT(stat[:, 16:17], stat[:, 12:13], -1.0, stat[:, 15:16], ALU.mult, ALU.mult, [stat], [stat])
        ACT(r[:], r[:], AF.Identity, [r, stat], [r], scale=stat[:, 15:16], bias=stat[:, 16:17])
        TT("dve", r[:], r[:], lng[:], ALU.mult, [r, lng], [r])
        TT("pool", r[:], r[:], lnb[:], ALU.add, [r, lnb], [r])
        DMA("sp", dst_ap, r[:], [r], [dst_buf])

    def phase_hgrn(l, src, srcb, dst, dstb):
        j = l // 2
        P.push()
        w_in = Tl("hw_in", [128, 8, 4096], BF16)
        w_out = Tl("hw_out", [128, 8, 1024], BF16)
        for kc in range(8):
            DMA("pool", w_in[:, kc, :], W["hgrn_w_in"][j, kc * 128:(kc + 1) * 128, :], [], [w_in], max_dma_last_dim=4096)
        DMA("pool", w_out[:], W["hgrn_w_out"][j].rearrange("(kc p) n -> p kc n", p=128), [], [w_out], max_dma_last_dim=4096)
        lbraw = Tl("lbraw", [128, 2, 8], F32)
        lbc = Tl("lbc", [128, 8], F32)
        oml = Tl("oml", [128, 8], F32)
        DMA("sp", lbraw[:], W["hgrn_lb"].rearrange("j (h p) -> p j h", p=128), [], [lbraw], allow_slow_non_contiguous=True)
        if j == 0:
            TT("dve", lbc[:], lbraw[:, 0, :], lbraw[:, 0, :], ALU.subtract, [lbraw], [lbc])
        else:
            TT("dve", lbc[:], lbraw[:, 1, :], lbraw[:, 0, :], ALU.subtract, [lbraw], [lbc])
            ACT(lbc[:], lbc[:], AF.Sigmoid, [lbc], [lbc])
        TS("dve", oml[:], lbc[:], -1.0, 1.0, ALU.mult, ALU.add, [lbc], [oml])
        normw = Tl("normw", [128, 1024], F32)
        for h in range(8):
            DMA("sp", normw[:, h * 128:(h + 1) * 128], W["hgrn_norm_w"][j:j + 1, :].partition_broadcast(128), [], [normw])
        lng = Tl("lng", [128, 1024], F32)
        lnb = Tl("lnb", [128, 1024], F32)
        DMA("sp", lng[:], W["ln_g"][l, 0:1, :].partition_broadcast(128), [], [lng])
        DMA("sp", lnb[:], W["ln_b"][l, 0:1, :].partition_broadcast(128), [], [lnb])
        scp = Tl("scp", [128, 1024], F32)
        shb = Tl("shb", [128, 1024], F32)
        gbc = Tl("gbc", [128, 1024], F32)
        xts = [Tl("xt%d" % i, [128, 1024], F32) for i in range(2)]
        work = Tl("work", [128, 1024], F32)
        rr = Tl("rr", [128, 1024], F32)
        stat = Tl("stat", [128, 32], F32)
        rr2 = [rr, Tl("rrb", [128, 1024], F32)]
        stat2 = [stat, Tl("statb", [128, 32], F32)]
        hT = Tl("hT", [128, 8, 128], BF16)
        trp = Tl("trp", [128, 4, 128], F32, psum=True)
        qz = [Tl("qzps%d" % i, [128, 4, 128], F32, psum=True) for i in range(2)]
        qzb = [[Buf(), Buf()], [Buf(), Buf()]]
        vg = [Tl("vgps%d" % i, [128, 512], F32, psum=True) for i in range(2)]
        hd = [Tl("hdps%d" % i, [128, 4, 128], F32, psum=True) for i in range(2)]
        hdb = [[Buf() for _ in range(4)] for _ in range(2)]
        ktv = [qz[i][:, 2, :].bitcast(BF16)[:, 0:128] for i in range(2)]
        ktb = [Buf(), Buf()]

        class OnV:
            b = trp.b
            v = trp[:].bitcast(BF16).rearrange("p a (b c) -> p (a b) c", c=128)

            def __getitem__(self, k):
                return self.v[k]
        ontp = OnV()
        vsb = Tl("vsb", [128, 1024], BF16)
        gw = Tl("gw", [128, 1024], F32)
        sig = [Tl("sig%d" % i, [128, 128], F32) for i in range(2)]
        lf = [Tl("lf%d" % i, [128, 128], F32) for i in range(2)]
        kk = [Tl("kk%d" % i, [128, 128], F32) for i in range(2)]
        bb = [Tl("bb%d" % i, [128, 128], F32) for i in range(2)]
        Ep = [Tl("Ep%d" % i, [128, 128], F32) for i in range(2)]
        Em = [Tl("Em%d" % i, [128, 128], F32) for i in range(2)]
        qT = [Tl("qT%d" % i, [128, 128], BF16) for i in range(2)]
        kT = [Tl("kT%d" % i, [128, 128], BF16) for i in range(2)]
        AT = [Tl("AT%d" % i, [128, 128], BF16) for i in range(2)]
        qpad = [Tl("qpad%d" % i, [128, 640], BF16) for i in range(2)]
        kmask = [Tl("kmask%d" % i, [128, 4, 128], BF16) for i in range(2)]
        kmb = [[Buf() for _ in range(4)] for _ in range(2)]
        s1 = [Tl("s1_%d" % i, [128, 128], F32) for i in range(2)]
        ss = Tl("ssq", [128, 16], F32)
        junk = Tl("junk", [128, 128], F32)
        on_all = Tl("on_all", [128, 8, 128], BF16)
        onT = Tl("onT", [128, 8, 128], BF16)
        state = [[Tl("st_%d_%d" % (s, h), [128, 128], F32) for h in range(8)] for s in range(NSEQ)]
        stbf = [[Tl("stb_%d_%d" % (s, h), [128, 128], BF16) for h in range(8)] for s in range(NSEQ)]
        for i in range(2):
            MS("pool", qpad[i][:], 0.0, [qpad[i]])
        for s in range(NSEQ):
            for h in range(8):
                MS("pool", state[s][h][:], 0.0, [state[s][h]])
                MS("pool", stbf[s][h][:], 0.0, [stbf[s][h]])
        it = 0
        for s in range(NSEQ):
            load_mod_bc(scp, l, s, 1, plus1=True)
            load_mod_bc(shb, l, s, 0)
            load_mod_bc(gbc, l, s, 2)
            for t in range(NT):
                g = s * NT + t
                xt = xts[g % 2]
                DMA("sp", xt[:], src[g * 128:(g + 1) * 128, :], [srcb[g]], [xt])
                make_hT(xt, scp, shb, hT, hT, trp, work)
                for cch in range(4):
                    pp = vg[cch % 2]
                    for kc in range(8):
                        MM(pp[:], hT[:, kc, :], w_in[:, kc, 2048 + cch * 512:2048 + (cch + 1) * 512], kc == 0, kc == 7, [hT, w_in], [pp])
                    if cch < 2:
                        CP("dve", vsb[:, cch * 512:(cch + 1) * 512], pp[:], [pp], [vsb])
                    else:
                        ACT(gw[:, (cch - 2) * 512:(cch - 1) * 512], pp[:], AF.Silu, [pp], [gw])
                TT("pool", gw[:], gw[:], normw[:], ALU.mult, [gw, normw], [gw])
                def head_gen(h, p2, s=s):
                    qps, zps = qz[p2][:, 0, :], qz[p2][:, 1, :]
                    qb_, zb_ = qzb[p2]
                    for kc in range(8):
                        MM(qps, w_in[:, kc, h * 128:(h + 1) * 128], hT[:, kc, :], kc == 0, kc == 7, [hT, w_in], [qb_])
                    for kc in range(8):
                        MM(zps, w_in[:, kc, 1024 + h * 128:1024 + (h + 1) * 128], hT[:, kc, :], kc == 0, kc == 7, [hT, w_in], [zb_])
                    yield
                    ACT(sig[p2][:], zps, AF.Sigmoid, [zb_], [sig[p2]])
                    yield
                    TS("dve", sig[p2][:], sig[p2][:], oml[:, h:h + 1], lbc[:, h:h + 1], ALU.mult, ALU.add, [sig[p2], oml, lbc], [sig[p2]])
                    yield
                    ACT(lf[p2][:], sig[p2][:], AF.Ln, [sig[p2]], [lf[p2]])
                    dbg('f', sig[p2][:], [sig[p2]]); dbg('lf', lf[p2][:], [lf[p2]])
                    TS("pool", kk[p2][:], sig[p2][:], -1.0, 1.0, ALU.mult, ALU.add, [sig[p2]], [kk[p2]])
                    yield
                    P.op("dve", lambda e, p2=p2: e.tensor_tensor_scan(out=bb[p2][:], data0=scanmask[:], data1=lf[p2][:], initial=0.0,
                                                                     op0=ALU.mult, op1=ALU.add), nrm([scanmask, lf[p2]]), nrm([bb[p2]]))
                    yield
                    ACT(Ep[p2][:], bb[p2][:], AF.Exp, [bb[p2]], [Ep[p2]])
                    ACT(Em[p2][:], bb[p2][:], AF.Exp, [bb[p2]], [Em[p2]], scale=-1.0)
                    yield
                    TT("dve", qT[p2][:], qps, Ep[p2][:], ALU.mult, [qb_, Ep[p2]], [qT[p2]])
                    CP("pool", qpad[p2][:].rearrange("p (c x) -> p c x", x=160)[:, :, 0:32],
                       qT[p2][:].rearrange("p (c j) -> p c j", j=32), [qT[p2]], [qpad[p2]])
                    TT("pool", kT[p2][:], kk[p2][:], Em[p2][:], ALU.mult, [kk[p2], Em[p2]], [kT[p2]])
                    yield
                    dbg('bb', bb[p2][:], [bb[p2]]); dbg('qT', qT[p2][:], [qT[p2]]); dbg('kT', kT[p2][:], [kT[p2]]); dbg('qpad', qpad[p2][:], [qpad[p2]])
                    stp, ops_, up = hd[p2][:, 0, :], vg[p2][:, 0:128], [hd[p2][:, 2, :], hd[p2][:, 3, :]]
                    stb_, ob_, ub_ = hdb[p2][0], vg[p2], [hdb[p2][2], hdb[p2][3]]
                    MM(stp, kT[p2][:], qT[p2][:], True, True, [kT[p2], qT[p2]], [stb_])
                    TR(ktv[p2], kT[p2][:], identb[:], [kT[p2], identb], [ktb[p2]])
                    yield
                    TT("dve", AT[p2][:], stp, blockmask[:], ALU.mult, [stb_, blockmask], [AT[p2]])
                    for c in range(4):
                        ACT(kmask[p2][:, c, :], ktv[p2], AF.Copy, [ktb[p2], chunkind], [kmb[p2][c]], scale=chunkind[:, c:c + 1])
                    yield
                    vh = vsb[:, h * 128:(h + 1) * 128]
                    MM(ops_, AT[p2][:], vh, True, False, [AT[p2], vsb], [ob_])
                    yield
                    stt, stb16 = state[s][h], stbf[s][h]
                    for c in range(4):
                        MM(ops_, qpad[p2][:, c * 128:(c + 1) * 128], stb16[:], False, c == 3, [qpad[p2], stb16], [ob_])
                        MM(up[c % 2], kmask[p2][:, c, :], vh, True, True, [kmb[p2][c], vsb], [ub_[c % 2]])
                        yield
                        ebl = Ep[p2][:, 32 * c + 31:32 * c + 32]
                        TS("pool", s1[p2][:], stt[:], ebl, None, ALU.mult, None, [stt, Ep[p2]], [s1[p2]])
                        yield
                        STT(stt[:], up[c % 2], ebl, s1[p2][:], ALU.mult, ALU.add, [ub_[c % 2], Ep[p2], s1[p2]], [stt])
                        yield
                        ACT(stb16[:], stt[:], AF.Copy, [stt], [stb16])
                        yield
                    dbg('AT', AT[p2][:], [AT[p2]]); dbg('ops', ops_, [ob_], psum=True); dbg('kmask', kmask[p2][:], kmb[p2]); dbg('state', stt[:], [stt])
                    ACT(junk[:], ops_, AF.Square, [ob_], [junk, ss], accum_out=ss[:, h:h + 1])
                    yield
                    ACT(ss[:, 8 + h:9 + h], ss[:, h:h + 1], AF.Sqrt, [ss], [ss], scale=1.0 / 128.0, bias=EPS)
                    yield
                    P.op("dve", lambda e, h=h: e.reciprocal(out=ss[:, 8 + h:9 + h], in_=ss[:, 8 + h:9 + h]), nrm([ss]), nrm([ss]))
                    yield
                    STT(on_all[:, h, :], ops_, ss[:, 8 + h:9 + h], gw[:, h * 128:(h + 1) * 128], ALU.mult, ALU.mult, [ob_, ss, gw], [on_all])
                    yield
                    TR(ontp[:, h, :], on_all[:, h, :], identb[:], [on_all, identb], [ontp])
                for hp in range(0, 8, 2):
                    alive = [head_gen(hp, 0), head_gen(hp + 1, 1)]
                    while alive:
                        for gg in list(alive):
                            try:
                                next(gg)
                            except StopIteration:
                                alive.remove(gg)
                dbg('on_all', on_all[:], [on_all]); dbg('gw', gw[:], [gw]); dbg('vsb', vsb[:], [vsb]); dbg('hT', hT[:], [hT]); dbg('ss', ss[:], [ss])
                CP("dve", onT[:, 0:4, :], ontp[:, 0:4, :], [ontp], [onT])
                ACT(onT[:, 4:8, :], ontp[:, 4:8, :], AF.Copy, [ontp], [onT])
                for half in range(2):
                    for h in range(8):
                        MM(vg[half][:], onT[:, h, :], w_out[:, h, half * 512:(half + 1) * 512], h == 0, h == 7, [onT, w_out], [vg[half]])
                dbg('y0', vg[0][:], [vg[0]], psum=True)
                resid_ln(xt, [(vg[0][:], vg[0]), (vg[1][:], vg[1])], gbc, lng, lnb, rr2[g % 2], stat2[g % 2], dst[g * 128:(g + 1) * 128, :], dstb[g])
        P.pop()

    def phase_moe_dense(l, src, srcb, dst, dstb):
        P.push()
        GT = min(16, NT)
        NG = (NSEQ * NT) // GT
        SGT = min(4, GT)
        wr = Tl("wr", [128, 8, 36], F32)
        DMA("sp", wr[:], W["router_w"][l], [], [wr])
        rbias = Tl("rbias", [128, 36], F32)
        DMA("sp", rbias[:], W["router_b"][l:l + 1, :].partition_broadcast(128), [], [rbias])
        lng = Tl("lng", [128, 1024], F32)
        lnb = Tl("lnb", [128, 1024], F32)
        DMA("sp", lng[:], W["ln_g"][l, 1:2, :].partition_broadcast(128), [], [lng])
        DMA("sp", lnb[:], W["ln_b"][l, 1:2, :].partition_broadcast(128), [], [lnb])
        scp = Tl("scp", [128, 1024], F32)
        shb = Tl("shb", [128, 1024], F32)
        gbc = Tl("gbc", [128, 1024], F32)
        xts = [Tl("xt%d" % i, [128, 1024], F32) for i in range(2)]
        work = Tl("work", [128, 1024], F32)
        rr = Tl("rr", [128, 1024], F32)
        stat = Tl("stat", [128, 32], F32)
        h2T = Tl("h2T", [128, 8, GT * 128], BF16)
        h2Tf = Tl("h2Tf", [128, 8, 128], F32)
        yacc = Tl("yacc", [128, GT, 1024], F32)
        yaccb = [Buf() for _ in range(GT)]
        gates = Tl("gates", [128, GT, 32], F32)
        gT = [Tl("gT%d" % i, [128, 4, 512], BF16) for i in range(2)]
        slt = [Tl("slt%d" % i, [128, 512], F32) for i in range(2)]
        wb = [dict(w1=Tl("w1_%d" % i, [128, 8, 512], BF16), w3=Tl("w3_%d" % i, [128, 8, 512], BF16),
                   w2=Tl("w2_%d" % i, [128, 4, 1024], BF16)) for i in range(2)]
        trp = Tl("trp", [128, 4, 128], F32, psum=True)
        lgp = Tl("lgp", [128, 512], F32, psum=True)
        hp1 = [Tl("hp1_%d" % i, [128, 512], F32, psum=True) for i in range(2)]
        hp3 = [Tl("hp3_%d" % i, [128, 512], F32, psum=True) for i in range(2)]
        yp = [Tl("yp%d" % i, [128, 512], F32, psum=True) for i in range(2)]
        lg = Tl("lg", [128, 36], F32)
        sm = Tl("rsm", [128, 16], F32)
        oh = Tl("oh", [128, 4], F32)
        ejunk = Tl("ejunk", [128, 4], F32)
        m1 = Tl("m1", [128, 4], F32)
        m2 = Tl("m2", [128, 4], F32)
        dd = Tl("dd", [128, 4], F32)
        c1 = Tl("c1", [128, 4], F32)
        c2 = Tl("c2", [128, 4], F32)
        mk1 = Tl("mk1", [128, 4, 8], F32)
        mk2 = Tl("mk2", [128, 4, 8], F32)
        el2 = Tl("el2", [128, 4, 8], F32)

        def bc48(ap):
            return ap.unsqueeze(2).to_broadcast([128, 4, 8])

        for gidx in range(NG):
            g0 = gidx * GT
            s = g0 // NT
            load_mod_bc(scp, l, s, 4, plus1=True)
            load_mod_bc(shb, l, s, 3)
            load_mod_bc(gbc, l, s, 5)
            if MOE_CUT != -3:
                MS("pool", yacc[:], 0.0, yaccb)
            for i in range(GT):
                g = g0 + i
                xt = xts[g % 2]
                DMA("sp", xt[:], src[g * 128:(g + 1) * 128, :], [srcb[g]], [xt])
                if MOE_CUT >= -1:
                    make_hT(xt, scp, shb, h2T[:, :, i * 128:(i + 1) * 128], h2T, trp, work, h2Tf if MOE_CUT >= 0 else None, h2Tf)
                if MOE_CUT <= 0:
                    continue
                for kc in range(8):
                    MM(lgp[:, 0:36], h2Tf[:, kc, :], wr[:, kc, :], kc == 0, kc == 7, [h2Tf, wr], [lgp])
                TT("dve", lg[:], lgp[:, 0:36], rbias[:], ALU.add, [lgp, rbias], [lg])
                if MOE_CUT == 1:
                    continue
                gl = lg[:, 0:4]
                el = lg[:, 4:36].rearrange("p (g j) -> p g j", j=8)
                P.op("dve", lambda e, gl=gl: e.tensor_reduce(out=sm[:, 0:1], in_=gl, axis=AX.X, op=ALU.max), nrm([lg]), nrm([sm]))
                TS("dve", oh[:], gl, sm[:, 0:1], None, ALU.is_equal, None, [lg, sm], [oh])
                TS("dve", sm[:, 1:2], sm[:, 0:1], -1.0, None, ALU.mult, None, [sm], [sm])
                ACT(ejunk[:], gl, AF.Exp, [lg, sm], [ejunk, sm], bias=sm[:, 1:2], accum_out=sm[:, 2:3])
                P.op("dve", lambda e: e.reciprocal(out=sm[:, 3:4], in_=sm[:, 2:3]), nrm([sm]), nrm([sm]))
                P.op("dve", lambda e, el=el: e.tensor_reduce(out=m1[:], in_=el, axis=AX.X, op=ALU.max), nrm([lg]), nrm([m1]))
                TT("dve", mk1[:], el, bc48(m1[:]), ALU.is_equal, [lg, m1], [mk1])
                STT(el2[:], mk1[:], -1.0e30, el, ALU.mult, ALU.add, [mk1, lg], [el2])
                P.op("dve", lambda e: e.tensor_reduce(out=m2[:], in_=el2[:], axis=AX.X, op=ALU.max), nrm([el2]), nrm([m2]))
                TT("dve", mk2[:], el2[:], bc48(m2[:]), ALU.is_equal, [el2, m2], [mk2])
                TT("dve", dd[:], m2[:], m1[:], ALU.subtract, [m1, m2], [dd])
                ACT(dd[:], dd[:], AF.Exp, [dd], [dd])
                TS("dve", c1[:], dd[:], 1.0, None, ALU.add, None, [dd], [c1])
                P.op("dve", lambda e: e.reciprocal(out=c1[:], in_=c1[:]), nrm([c1]), nrm([c1]))
                TT("dve", c2[:], dd[:], c1[:], ALU.mult, [dd, c1], [c2])
                TS("dve", oh[:], oh[:], sm[:, 3:4], None, ALU.mult, None, [oh, sm], [oh])
                TT("dve", c1[:], c1[:], oh[:], ALU.mult, [c1, oh], [c1])
                TT("dve", c2[:], c2[:], oh[:], ALU.mult, [c2, oh], [c2])
                TT("dve", mk1[:], mk1[:], bc48(c1[:]), ALU.mult, [mk1, c1], [mk1])
                TT("dve", mk2[:], mk2[:], bc48(c2[:]), ALU.mult, [mk2, c2], [mk2])
                TT("dve", gates[:, i, :].rearrange("p (g j) -> p g j", j=8), mk1[:], mk2[:], ALU.add, [mk1, mk2], [gates])
            if DEBUG:
                dbg("gates", gates[:], [gates])
            nsub = 0
            for e in range(NE if MOE_STAGE >= 2 else 0):
                wbe = wb[e % 2]
                DMA("pool", wbe["w1"][:], W["moe_w1"][l, e].rearrange("(kc p) n -> p kc n", p=128), [], [wbe["w1"]])
                DMA("pool", wbe["w3"][:], W["moe_w3"][l, e].rearrange("(kc p) n -> p kc n", p=128), [], [wbe["w3"]])
                DMA("pool", wbe["w2"][:], W["moe_w2"][l, e].rearrange("(kc p) n -> p kc n", p=128), [], [wbe["w2"]], max_dma_last_dim=4096)
                for sg in range(GT // SGT):
                    ncol = SGT * 128
                    c0 = sg * ncol
                    gt_ = gT[nsub % 2]
                    nsub += 1
                    for fc in range(4):
                        a1, a3 = hp1[fc % 2], hp3[fc % 2]
                        for kc in range(8):
                            MM(a1[:, 0:ncol], wbe["w1"][:, kc, fc * 128:(fc + 1) * 128], h2T[:, kc, c0:c0 + ncol], kc == 0, kc == 7, [wbe["w1"], h2T], [a1])
                        for kc in range(8):
                            MM(a3[:, 0:ncol], wbe["w3"][:, kc, fc * 128:(fc + 1) * 128], h2T[:, kc, c0:c0 + ncol], kc == 0, kc == 7, [wbe["w3"], h2T], [a3])
                        sl = slt[fc % 2]
                        ACT(sl[:, 0:ncol], a1[:, 0:ncol], AF.Silu, [a1], [sl])
                        TT("dve", gt_[:, fc, 0:ncol], sl[:, 0:ncol], a3[:, 0:ncol], ALU.mult, [sl, a3], [gt_])
                    for ti in range(SGT):
                        i = sg * SGT + ti
                        for half in range(2):
                            ypp = yp[half]
                            for fc in range(4):
                                MM(ypp[:], gt_[:, fc, ti * 128:(ti + 1) * 128], wbe["w2"][:, fc, half * 512:(half + 1) * 512], fc == 0, fc == 3, [gt_, wbe["w2"]], [ypp])
                            ya = yacc[:, i, half * 512:(half + 1) * 512]
                            STT(ya, ypp[:], gates[:, i, e:e + 1], ya, ALU.mult, ALU.add, [ypp, gates, yaccb[i]], [yaccb[i]])
            for i in range(GT):
                g = g0 + i
                xt = xts[g % 2]
                DMA("sp", xt[:], src[g * 128:(g + 1) * 128, :], [srcb[g]], [xt])
                resid_ln(xt, [(yacc[:, i, 0:512], yaccb[i]), (yacc[:, i, 512:1024], yaccb[i])], gbc, lng, lnb, rr, stat,
                         dst[g * 128:(g + 1) * 128, :], dstb[g])
        P.pop()

    def phase_moe(l, src, srcb, dst, dstb):
        P.push()
        NTT = NSEQ * NT
        TB = 512
        NBLK = (2 * NTOK) // TB + 32
        wr = Tl("wr", [128, 8, 36], F32)
        DMA("sp", wr[:], W["router_w"][l], [], [wr])
        rbias = Tl("rbias", [128, 36], F32)
        DMA("sp", rbias[:], W["router_b"][l:l + 1, :].partition_broadcast(128), [], [rbias])
        lng = Tl("lng", [128, 1024], F32)
        lnb = Tl("lnb", [128, 1024], F32)
        DMA("sp", lng[:], W["ln_g"][l, 1:2, :].partition_broadcast(128), [], [lng])
        DMA("sp", lnb[:], W["ln_b"][l, 1:2, :].partition_broadcast(128), [], [lnb])
        widx_c = Tl("widx_c", [128, 12], F32)
        DMA("sp", widx_c[:], CD["widx"], [], [widx_c])
        utri = Tl("utri", [128, 128], BF16)
        ones = Tl("ones", [128, 128], BF16)
        DMA("pool", utri[:], CD["utri"], [], [utri])
        MS("pool", ones[:], 1.0, [ones])
        scp = Tl("scp", [128, 1024], F32)
        shb = Tl("shb", [128, 1024], F32)
        gbc = Tl("gbc", [128, 1024], F32)
        xts = [Tl("xt%d" % i, [128, 1024], F32) for i in range(2)]
        work = Tl("work", [128, 1024], F32)
        rr = Tl("rr", [128, 1024], F32)
        stat = Tl("stat", [128, 32], F32)
        h2Tf = Tl("h2Tf", [128, 8, 128], F32)
        h2b = [Tl("h2b%d" % i, [128, 1024], BF16) for i in range(2)]
        m1all = Tl("m1all", [128, NTT, 32], F32)
        m2all = Tl("m2all", [128, NTT, 32], F32)
        rkall = Tl("rkall", [128, NTT, 32], F32)
        wab = Tl("wab", [128, NTT, 2], F32)
        slots = Tl("slots", [128, NTT, 2], I32)
        slotsb = [Buf() for _ in range(NTT)]
        cum = Tl("cum", [128, 32], F32)
        MS("pool", cum[:], 0.0, [cum])
        mb16 = Tl("mb16", [128, 32], BF16)
        bankA = [Tl("mbk%d" % i, [128, 512], F32, psum=True) for i in range(7)]
        trp = Tl("trp", [128, 4, 128], F32, psum=True)
        lgp, rkp, csp = bankA[0], bankA[1], bankA[2]
        lg = Tl("lg", [128, 36], F32)
        sm = Tl("rsm", [128, 16], F32)
        oh = Tl("oh", [128, 4], F32)
        ejunk = Tl("ejunk", [128, 4], F32)
        m1 = Tl("m1", [128, 4], F32)
        m2 = Tl("m2", [128, 4], F32)
        dd = Tl("dd", [128, 4], F32)
        c1 = Tl("c1", [128, 4], F32)
        c2 = Tl("c2", [128, 4], F32)
        mk1 = Tl("mk1", [128, 4, 8], F32)
        mk2 = Tl("mk2", [128, 4, 8], F32)
        el2 = Tl("el2", [128, 4, 8], F32)
        h2_d = moe_h2_d
        h2db = [Buf() for _ in range(NTT)]

        def bc48(ap):
            return ap.unsqueeze(2).to_broadcast([128, 4, 8])

        for g in range(NTT):
            s = g // NT
            if g % NT == 0:
                load_mod_bc(scp, l, s, 4, plus1=True)
                load_mod_bc(shb, l, s, 3)
                load_mod_bc(gbc, l, s, 5)
            xt = xts[g % 2]
            DMA("sp", xt[:], src[g * 128:(g + 1) * 128, :], [srcb[g]], [xt])
            TT("dve", work[:], xt[:], scp[:], ALU.mult, [xt, scp], [work])
            TT("pool", work[:], work[:], shb[:], ALU.add, [work, shb], [work])
            hb = h2b[g % 2]
            ACT(hb[:], work[:], AF.Copy, [work], [hb])
            DMA("sp", h2_d[g * 128:(g + 1) * 128, :], hb[:], [hb], [h2db[g]])
            for half in range(2):
                for i in range(4):
                    kc = half * 4 + i
                    TR(trp[:, i, :], work[:, kc * 128:(kc + 1) * 128], identf[:], [work, identf], [trp])
                if half == 0:
                    CP("dve", h2Tf[:, 0:4, :], trp[:], [trp], [h2Tf])
                else:
                    ACT(h2Tf[:, 4:8, :], trp[:], AF.Copy, [trp], [h2Tf])
            for kc in range(8):
                MM(lgp[:, 0:36], h2Tf[:, kc, :], wr[:, kc, :], kc == 0, kc == 7, [h2Tf, wr], [lgp])
            TT("dve", lg[:], lgp[:, 0:36], rbias[:], ALU.add, [lgp, rbias], [lg])
            gl = lg[:, 0:4]
            el = lg[:, 4:36].rearrange("p (g j) -> p g j", j=8)
            P.op("dve", lambda e, gl=gl: e.tensor_reduce(out=sm[:, 0:1], in_=gl, axis=AX.X, op=ALU.max), nrm([lg]), nrm([sm]))
            TS("dve", oh[:], gl, sm[:, 0:1], None, ALU.is_equal, None, [lg, sm], [oh])
            TS("dve", sm[:, 1:2], sm[:, 0:1], -1.0, None, ALU.mult, None, [sm], [sm])
            ACT(ejunk[:], gl, AF.Exp, [lg, sm], [ejunk, sm], bias=sm[:, 1:2], accum_out=sm[:, 2:3])
            P.op("dve", lambda e: e.reciprocal(out=sm[:, 3:4], in_=sm[:, 2:3]), nrm([sm]), nrm([sm]))
            P.op("dve", lambda e, el=el: e.tensor_reduce(out=m1[:], in_=el, axis=AX.X, op=ALU.max), nrm([lg]), nrm([m1]))
            TT("dve", mk1[:], el, bc48(m1[:]), ALU.is_equal, [lg, m1], [mk1])
            STT(el2[:], mk1[:], -1.0e30, el, ALU.mult, ALU.add, [mk1, lg], [el2])
            P.op("dve", lambda e: e.tensor_reduce(out=m2[:], in_=el2[:], axis=AX.X, op=ALU.max), nrm([el2]), nrm([m2]))
            TT("dve", mk2[:], el2[:], bc48(m2[:]), ALU.is_equal, [el2, m2], [mk2])
            TT("dve", dd[:], m2[:], m1[:], ALU.subtract, [m1, m2], [dd])
            ACT(dd[:], dd[:], AF.Exp, [dd], [dd])
            TS("dve", c1[:], dd[:], 1.0, None, ALU.add, None, [dd], [c1])
            P.op("dve", lambda e: e.reciprocal(out=c1[:], in_=c1[:]), nrm([c1]), nrm([c1]))
            TT("dve", c2[:], dd[:], c1[:], ALU.mult, [dd, c1], [c2])
            m1g = m1all[:, g, :].rearrange("p (g j) -> p g j", j=8)
            m2g = m2all[:, g, :].rearrange("p (g j) -> p g j", j=8)
            TT("dve", m1g, mk1[:], bc48(oh[:]), ALU.mult, [mk1, oh], [m1all])
            TT("dve", m2g, mk2[:], bc48(oh[:]), ALU.mult, [mk2, oh], [m2all])
            TT("dve", c1[:], c1[:], oh[:], ALU.mult, [c1, oh], [c1])
            TT("dve", c2[:], c2[:], oh[:], ALU.mult, [c2, oh], [c2])
            P.op("dve", lambda e: e.tensor_reduce(out=sm[:, 4:5], in_=c1[:], axis=AX.X, op=ALU.add), nrm([c1]), nrm([sm]))
            P.op("dve", lambda e: e.tensor_reduce(out=sm[:, 5:6], in_=c2[:], axis=AX.X, op=ALU.add), nrm([c2]), nrm([sm]))
            TS("dve", wab[:, g, :], sm[:, 4:6], sm[:, 3:4], None, ALU.mult, None, [sm], [wab])
            TT("dve", mb16[:], m1all[:, g, :], m2all[:, g, :], ALU.add, [m1all, m2all], [mb16])
            MM(rkp[:, 0:32], utri[:], mb16[:], True, True, [utri, mb16], [rkp])
            MM(csp[:, 0:32], ones[:], mb16[:], True, True, [ones, mb16], [csp])
            TT("dve", rkall[:, g, :], rkp[:, 0:32], cum[:], ALU.add, [rkp, cum], [rkall])
            TT("dve", cum[:], cum[:], csp[:, 0:32], ALU.add, [cum, csp], [cum])

        if MOE_CUT >= 2:
            pass
        pad = Tl("pad", [128, 32], F32)
        padi = Tl("padi", [128, 32], I32)
        pend = Tl("pend", [128, 32], F32)
        pstart = Tl("pstart", [128, 32], F32)
        onesf = Tl("onesf", [128, 32], F32)
        MS("pool", onesf[:], 1.0, [onesf])
        CP("dve", padi[:], cum[:], [cum], [padi])
        TS("dve", padi[:], padi[:], TB - 1, None, ALU.add, None, [padi], [padi])
        TS("dve", padi[:], padi[:], 9, None, ALU.arith_shift_right, None, [padi], [padi])
        TS("dve", padi[:], padi[:], 9, None, ALU.logical_shift_left, None, [padi], [padi])
        CP("dve", pad[:], padi[:], [padi], [pad])
        P.op("dve", lambda e: e.tensor_tensor_scan(out=pend[:], data0=onesf[:], data1=pad[:], initial=0.0, op0=ALU.mult, op1=ALU.add),
             nrm([onesf, pad]), nrm([pend]))
        TT("dve", pstart[:], pend[:], pad[:], ALU.subtract, [pend, pad], [pstart])
        bstart = Tl("bstart", [128, NBLK], F32)
        DMA("sp", bstart[:], CD["bstart"][0:1, 0:NBLK].partition_broadcast(128), [], [bstart])
        cmp_ = Tl("cmpb", [128, NBLK, 32], F32)
        bexp = Tl("bexp", [128, NBLK], F32)
        TT("dve", cmp_[:], pend[:].unsqueeze(1).to_broadcast([128, NBLK, 32]), bstart[:].unsqueeze(2).to_broadcast([128, NBLK, 32]),
           ALU.is_le, [pend, bstart], [cmp_])
        P.op("dve", lambda e: e.tensor_reduce(out=bexp[:], in_=cmp_[:], axis=AX.X, op=ALU.add), nrm([cmp_]), nrm([bexp]))
        TS("dve", bexp[:], bexp[:], 31.0, None, ALU.min, None, [bexp], [bexp])
        widf = Tl("widf", [128, NBLK, 12], F32)
        widi = Tl("widi", [128, NBLK, 12], I32)
        TS("dve", bexp[:], bexp[:], float(l * 32), None, ALU.add, None, [bexp], [bexp])
        STT(widf[:, :, 0:8], bexp[:].unsqueeze(2).to_broadcast([128, NBLK, 8]), 1024.0,
            widx_c[:, 0:8].unsqueeze(1).to_broadcast([128, NBLK, 8]), ALU.mult, ALU.add, [bexp, widx_c], [widf])
        STT(widf[:, :, 8:12], bexp[:].unsqueeze(2).to_broadcast([128, NBLK, 4]), 512.0,
            widx_c[:, 8:12].unsqueeze(1).to_broadcast([128, NBLK, 4]), ALU.mult, ALU.add, [bexp, widx_c], [widf])
        CP("dve", widi[:], widf[:], [widf], [widi])

        dtmp = Tl("dtmp", [128, 32], F32)
        dtmp2 = Tl("dtmp2", [128, 32], F32)
        slf = Tl("slf", [128, 2], F32)
        xbufb = [Buf() for _ in range(2 * NTT)]
        for g in range(NTT if MOE_CUT >= 3 else 0):
            TT("dve", dtmp[:], rkall[:, g, :], pstart[:], ALU.add, [rkall, pstart], [dtmp])
            TT("dve", dtmp2[:], dtmp[:], m1all[:, g, :], ALU.mult, [dtmp, m1all], [dtmp2])
            P.op("dve", lambda e: e.tensor_reduce(out=slf[:, 0:1], in_=dtmp2[:], axis=AX.X, op=ALU.add), nrm([dtmp2]), nrm([slf]))
            TT("dve", dtmp2[:], dtmp[:], m2all[:, g, :], ALU.mult, [dtmp, m2all], [dtmp2])
            P.op("dve", lambda e: e.tensor_reduce(out=slf[:, 1:2], in_=dtmp2[:], axis=AX.X, op=ALU.add), nrm([dtmp2]), nrm([slf]))
            CP("dve", slots[:, g, :], slf[:], [slf], [slotsb[g]])
            hb = h2b[g % 2]
            DMA("sp", hb[:], h2_d[g * 128:(g + 1) * 128, :], [h2db[g]], [hb])
            for k in range(2):
                P.dma("pool", lambda e, g=g, k=k, hb=hb: e.indirect_dma_start(
                    out=moe_xbuf_d[:, :], out_offset=bass.IndirectOffsetOnAxis(ap=slots[:, g, k:k + 1], axis=0),
                    in_=hb[:], in_offset=None), nrm([hb, slotsb[g]]), [xbufb[2 * g + k]])

        wb = [dict(w1=Tl("w1_%d" % i, [128, 8, 512], BF16), w3=Tl("w3_%d" % i, [128, 8, 512], BF16),
                   w2=Tl("w2_%d" % i, [128, 4, 1024], BF16)) for i in range(2)]
        xg = [Tl("xg%d" % i, [128, 4, 1024], BF16) for i in range(2)]
        xT = [Tl("xTb%d" % i, [128, 8, 512], BF16) for i in range(2)]
        gT = [Tl("gT%d" % i, [128, 4, 512], BF16) for i in range(2)]
        slt = [Tl("slt%d" % i, [128, 512], F32) for i in range(2)]
        yt = [Tl("ytb%d" % i, [128, 1024], BF16) for i in range(2)]
        tpb = bankA[0]
        tpv = tpb[:].bitcast(BF16).rearrange("p (r c) -> p r c", c=512)
        hp1 = [bankA[1], bankA[2]]
        hp3 = [bankA[3], bankA[4]]
        yp = [bankA[5], bankA[6]]
        ybufb = [Buf() for _ in range(NBLK)]
        w1rows = W["moe_w1"].rearrange("l e k f -> (l e k) f")
        w3rows = W["moe_w3"].rearrange("l e k f -> (l e k) f")
        w2rows = W["moe_w2"].rearrange("l e k f -> (l e k) f")
        nyt = 0
        for b in range(NBLK if MOE_CUT >= 4 else 0):
            wbe = wb[b % 2]
            for kc in range(8):
                P.dma("pool", lambda e, b=b, kc=kc, wbe=wbe: e.indirect_dma_start(
                    out=wbe["w1"][:, kc, :], out_offset=None, in_=w1rows,
                    in_offset=bass.IndirectOffsetOnAxis(ap=widi[:, b, kc:kc + 1], axis=0)), nrm([widi]), nrm([wbe["w1"]]))
                P.dma("pool", lambda e, b=b, kc=kc, wbe=wbe: e.indirect_dma_start(
                    out=wbe["w3"][:, kc, :], out_offset=None, in_=w3rows,
                    in_offset=bass.IndirectOffsetOnAxis(ap=widi[:, b, kc:kc + 1], axis=0)), nrm([widi]), nrm([wbe["w3"]]))
            for fc in range(4):
                P.dma("pool", lambda e, b=b, fc=fc, wbe=wbe: e.indirect_dma_start(
                    out=wbe["w2"][:, fc, :], out_offset=None, in_=w2rows,
                    in_offset=bass.IndirectOffsetOnAxis(ap=widi[:, b, 8 + fc:9 + fc], axis=0)), nrm([widi]), nrm([wbe["w2"]]))
            xgb = xg[b % 2]
            DMA("sp", xgb[:], moe_xbuf_d[b * TB:(b + 1) * TB, :].rearrange("(j p) d -> p j d", p=128), xbufb, [xgb])
            xTb = xT[b % 2]
            for kc in range(8):
                for jj in range(4):
                    TR(tpv[:, kc % 2, jj * 128:(jj + 1) * 128], xgb[:, jj, kc * 128:(kc + 1) * 128], identb[:], [xgb, identb], [tpb])
                if kc % 2 == 0:
                    CP("dve", xTb[:, kc, :], tpv[:, kc % 2, :], [tpb], [xTb])
                else:
                    ACT(xTb[:, kc, :], tpv[:, kc % 2, :], AF.Copy, [tpb], [xTb])
            gt_ = gT[b % 2]
            for fc in range(4):
                a1, a3 = hp1[fc % 2], hp3[fc % 2]
                for kc in range(8):
                    MM(a1[:], wbe["w1"][:, kc, fc * 128:(fc + 1) * 128], xTb[:, kc, :], kc == 0, kc == 7, [wbe["w1"], xTb], [a1])
                for kc in range(8):
                    MM(a3[:], wbe["w3"][:, kc, fc * 128:(fc + 1) * 128], xTb[:, kc, :], kc == 0, kc == 7, [wbe["w3"], xTb], [a3])
                sl = slt[fc % 2]
                ACT(sl[:], a1[:], AF.Silu, [a1], [sl])
                TT("dve", gt_[:, fc, :], sl[:], a3[:], ALU.mult, [sl, a3], [gt_])
            for ti in range(4):
                ytt = yt[nyt % 2]
                nyt += 1
                for half in range(2):
                    ypp = yp[half]
                    for fc in range(4):
                        MM(ypp[:], gt_[:, fc, ti * 128:(ti + 1) * 128], wbe["w2"][:, fc, half * 512:(half + 1) * 512], fc == 0, fc == 3, [gt_, wbe["w2"]], [ypp])
                    if half == 0:
                        ACT(ytt[:, 0:512], ypp[:], AF.Copy, [ypp], [ytt])
                    else:
                        CP("dve", ytt[:, 512:1024], ypp[:], [ypp], [ytt])
                r0 = b * TB + ti * 128
                DMA("sp", moe_ybuf_d[r0:r0 + 128, :], ytt[:], [ytt], [ybufb[b]])

        ya = [Tl("yga%d" % i, [128, 1024], BF16) for i in range(2)]
        yb_ = [Tl("ygb%d" % i, [128, 1024], BF16) for i in range(2)]
        ycombs = [Tl("ycomb%d" % i, [128, 1024], F32) for i in range(2)]
        rr2 = [rr, Tl("rrb", [128, 1024], F32)]
        stat2 = [stat, Tl("statb", [128, 32], F32)]
        for g in range(NTT):
            s = g // NT
            if g % NT == 0:
                load_mod_bc(gbc, l, s, 5)
            xt = xts[g % 2]
            DMA("sp", xt[:], src[g * 128:(g + 1) * 128, :], [srcb[g]], [xt])
            ga, gb_ = ya[g % 2], yb_[g % 2]
            ycomb = ycombs[g % 2]
            if MOE_CUT < 5:
                MS("pool", ga[:], 0.0, [ga])
                MS("pool", gb_[:], 0.0, [gb_])
            for k, dstt in (((0, ga), (1, gb_)) if MOE_CUT >= 5 else ()):
                P.dma("pool", lambda e, g=g, k=k, dstt=dstt: e.indirect_dma_start(
                    out=dstt[:], out_offset=None, in_=moe_ybuf_d[:, :],
                    in_offset=bass.IndirectOffsetOnAxis(ap=slots[:, g, k:k + 1], axis=0)), nrm(ybufb + [slotsb[g]]), nrm([dstt]))
            TS("dve", ycomb[:], ga[:], wab[:, g, 0:1], None, ALU.mult, None, [ga, wab], [ycomb])
            STT(ycomb[:], gb_[:], wab[:, g, 1:2], ycomb[:], ALU.mult, ALU.add, [gb_, wab, ycomb], [ycomb])
            resid_ln(xt, [(ycomb[:, 0:512], ycomb), (ycomb[:, 512:1024], ycomb)], gbc, lng, lnb, rr2[g % 2], stat2[g % 2],
                     dst[g * 128:(g + 1) * 128, :], dstb[g])
        P.pop()

    def phase_attn(l, src, srcb, dst, dstb):
        import math
        j = l // 2
        lam_init = 0.8 - 0.6 * math.exp(-0.3 * l)
        QB = min(512, S)
        NQB = QB // 128
        NSB = S // QB
        P.push()
        banks = [Tl("bk%d" % i, [128, 512], F32, psum=True) for i in range(8)]

        class TrV:
            def __init__(self, bank):
                self.b = bank.b
                self.v = bank[:].rearrange("p (i t) -> p i t", t=128)

            def __getitem__(self, k):
                return self.v[k]
        trp_t = banks[0]
        w_out = Tl("aw_out", [128, 8, 1024], BF16)
        DMA("pool", w_out[:], W["attn_w_out"][j].rearrange("(kc p) n -> p kc n", p=128), [], [w_out], max_dma_last_dim=4096)
        lng = Tl("lng", [128, 1024], F32)
        lnb = Tl("lnb", [128, 1024], F32)
        DMA("sp", lng[:], W["ln_g"][l, 0:1, :].partition_broadcast(128), [], [lng])
        DMA("sp", lnb[:], W["ln_b"][l, 0:1, :].partition_broadcast(128), [], [lnb])
        subw = Tl("subw", [128, 128], F32)
        DMA("sp", subw[:], W["attn_subln_w"][j:j + 1, :].partition_broadcast(128), [], [subw])
        TS("dve", subw[:], subw[:], 1.0 - lam_init, None, ALU.mult, None, [subw], [subw])
        lamt = Tl("lamt", [128, 256], F32)
        lams = Tl("lams", [128, 8], F32)
        DMA("sp", lamt[:], W["attn_lambda"][j:j + 1].rearrange("o a d -> o (a d)").partition_broadcast(128), [], [lamt])
        TT("dve", lamt[:, 0:64], lamt[:, 0:64], lamt[:, 64:128], ALU.mult, [lamt], [lamt])
        TT("dve", lamt[:, 128:192], lamt[:, 128:192], lamt[:, 192:256], ALU.mult, [lamt], [lamt])
        P.op("dve", lambda e: e.tensor_reduce(out=lams[:, 0:1], in_=lamt[:, 0:64], axis=AX.X, op=ALU.add), nrm([lamt]), nrm([lams]))
        P.op("dve", lambda e: e.tensor_reduce(out=lams[:, 1:2], in_=lamt[:, 128:192], axis=AX.X, op=ALU.add), nrm([lamt]), nrm([lams]))
        ACT(lams[:, 0:2], lams[:, 0:2], AF.Exp, [lams], [lams])
        TT("dve", lams[:, 2:3], lams[:, 0:1], lams[:, 1:2], ALU.subtract, [lams], [lams])
        TS("dve", lams[:, 2:3], lams[:, 2:3], lam_init, None, ALU.add, None, [lams], [lams])
        lam = lams[:, 2:3]
        cosT = Tl("cosT", [128, S], F32)
        sinT = Tl("sinT", [128, S], F32)
        DMA("sp", cosT[:], CD["ropecos"], [], [cosT])
        DMA("sp", sinT[:], CD["ropesin"], [], [sinT])
        scp = Tl("scp", [128, 1024], F32)
        shb = Tl("shb", [128, 1024], F32)
        gbc = Tl("gbc", [128, 1024], F32)
        xts = [Tl("xt%d" % i, [128, 1024], F32) for i in range(2)]
        work = Tl("work", [128, 1024], F32)
        rr = Tl("rr", [128, 1024], F32)
        stat = Tl("stat", [128, 32], F32)
        hTall = Tl("hTall", [128, 8, S], BF16)
        qT = Tl("aqT", [128, S], BF16)
        kT = Tl("akT", [128, S], BF16)
        vext = Tl("vext", [128, NT, 129], BF16)
        MS("pool", vext[:, :, 128:129], 1.0, [vext])
        wsl = {nm: Tl("aw_" + nm, [128, 8, 128], BF16) for nm in ("q", "qs", "k", "ks", "v")}
        t1 = Tl("rt1", [128, 512], F32)
        t2 = Tl("rt2", [128, 512], F32)
        pT = [[Tl("pT%d_%d" % (i, m), [128, 512], BF16) for m in range(2)] for i in range(2)]
        rs = Tl("ars", [128, 8], F32)
        ot = Tl("aot", [128, 128], F32)
        ot2 = Tl("aot2", [128, 128], F32)
        o1s = Tl("ao1s", [128, 4, 128], F32)
        junk = Tl("ajunk", [128, 128], F32)
        onb16 = [Tl("aon%d" % i, [128, 128], BF16) for i in range(2)]
        ont = Tl("aont", [128, 1024], BF16)
        onT = Tl("aonT", [128, 8, 128], BF16)
        win = W["attn_w_in"][j]
        nst = 0
        for s in range(NSEQ):
            load_mod_bc(scp, l, s, 1, plus1=True)
            load_mod_bc(shb, l, s, 0)
            load_mod_bc(gbc, l, s, 2)
            for t in range(NT):
                g = s * NT + t
                xt = xts[g % 2]
                DMA("sp", xt[:], src[g * 128:(g + 1) * 128, :], [srcb[g]], [xt])
                make_hT(xt, scp, shb, hTall[:, :, t * 128:(t + 1) * 128], hTall, TrV(banks[0]), work)
            for h in range(8):
                def wv3(c0, n):
                    return win[:, c0:c0 + n].rearrange("(kc p) n -> p kc n", p=128)
                DMA("pool", wsl["q"][:], wv3(h * 128, 128), [], [wsl["q"]])
                DMA("pool", wsl["k"][:], wv3(1024 + h * 128, 128), [], [wsl["k"]])
                DMA("pool", wsl["v"][:], wv3(2048 + h * 128, 128), [], [wsl["v"]])
                for nm, base in (("qs", 0), ("ks", 1024)):
                    for m in range(2):
                        b0 = base + h * 128 + m * 64
                        DMA("pool", wsl[nm][:, :, m * 64:m * 64 + 32], wv3(b0 + 32, 32), [], [wsl[nm]])
                        DMA("pool", wsl[nm][:, :, m * 64 + 32:m * 64 + 64], wv3(b0, 32), [], [wsl[nm]])
                for nb in range(NSB):
                    cs = slice(nb * QB, (nb + 1) * QB)
                    for (wn, wsn, dstT, bi) in (("q", "qs", qT, 2), ("k", "ks", kT, 2)):
                        pa, pb = banks[bi], banks[bi + 1]
                        for kc in range(8):
                            MM(pa[:, 0:QB], wsl[wn][:, kc, :], hTall[:, kc, cs], kc == 0, kc == 7, [wsl[wn], hTall], [pa])
                        for kc in range(8):
                            MM(pb[:, 0:QB], wsl[wsn][:, kc, :], hTall[:, kc, cs], kc == 0, kc == 7, [wsl[wsn], hTall], [pb])
                        TT("dve", t1[:, 0:QB], pa[:, 0:QB], cosT[:, cs], ALU.mult, [pa, cosT], [t1])
                        TT("dve", t2[:, 0:QB], pb[:, 0:QB], sinT[:, cs], ALU.mult, [pb, sinT], [t2])
                        TT("pool", dstT[:, cs], t1[:, 0:QB], t2[:, 0:QB], ALU.add, [t1, t2], [dstT])
                for t in range(NT):
                    pv = banks[4 + (t % 2)]
                    for kc in range(8):
                        MM(pv[:, 0:128], hTall[:, kc, t * 128:(t + 1) * 128], wsl["v"][:, kc, :], kc == 0, kc == 7, [hTall, wsl["v"]], [pv])
                    CP("dve", vext[:, t, 0:128], pv[:, 0:128], [pv], [vext])
                steps = []
                for Q in range(NSB):
                    for m in range(2):
                        for kb in range(Q * NQB + NQB):
                            steps.append((Q, m, kb))

                def emit_scores(i):
                    Q, m, kb = steps[i]
                    j0 = max(0, kb - Q * NQB)
                    csl = slice(j0 * 128, QB)
                    stb = banks[i % 2]
                    MM(stb[:, csl], kT[m * 64:(m + 1) * 64, kb * 128:(kb + 1) * 128], qT[m * 64:(m + 1) * 64, Q * QB + j0 * 128:(Q + 1) * QB],
                       True, True, [kT, qT], [stb])

                emit_scores(0)
                for i, (Q, m, kb) in enumerate(steps):
                    if i + 1 < len(steps):
                        emit_scores(i + 1)
                    j0 = max(0, kb - Q * NQB)
                    csl = slice(j0 * 128, QB)
                    stb = banks[i % 2]
                    pt = pT[i % 2][0]
                    ACT(pt[:, csl], stb[:, csl], AF.Exp, [stb], [pt], scale=0.125)
                    if kb >= Q * NQB:
                        dsl = slice(j0 * 128, (j0 + 1) * 128)
                        TT("pool", pt[:, dsl], pt[:, dsl], trimask[:], ALU.mult, [pt, trimask], [pt])
                    for jq in range(j0, NQB):
                        ab = banks[4 + jq]
                        MM(ab[:, 0:129], pt[:, jq * 128:(jq + 1) * 128], vext[:, kb, :], kb == 0, kb == Q * NQB + jq, [pt, vext], [ab])
                    if kb == Q * NQB + NQB - 1:
                        for jq in range(NQB):
                            ab = banks[4 + jq]
                            if m == 0:
                                P.op("dve", lambda e, ab=ab: e.reciprocal(out=rs[:, 0:1], in_=ab[:, 128:129]), nrm([ab]), nrm([rs]))
                                TS("dve", o1s[:, jq, :], ab[:, 0:128], rs[:, 0:1], None, ALU.mult, None, [ab, rs], [o1s])
                            else:
                                t = Q * NQB + jq
                                g = s * NT + t
                                P.op("dve", lambda e, ab=ab: e.reciprocal(out=rs[:, 1:2], in_=ab[:, 128:129]), nrm([ab]), nrm([rs]))
                                TS("dve", rs[:, 1:2], rs[:, 1:2], lam, None, ALU.mult, None, [rs, lams], [rs])
                                TS("dve", ot2[:], ab[:, 0:128], rs[:, 1:2], None, ALU.mult, None, [ab, rs], [ot2])
                                TT("dve", ot[:], o1s[:, jq, :], ot2[:], ALU.subtract, [o1s, ot2], [ot])
                                ACT(junk[:], ot[:], AF.Square, [ot], [junk, rs], accum_out=rs[:, 2:3])
                                ACT(rs[:, 3:4], rs[:, 2:3], AF.Sqrt, [rs], [rs], scale=1.0 / 128.0, bias=EPS)
                                P.op("dve", lambda e: e.reciprocal(out=rs[:, 3:4], in_=rs[:, 3:4]), nrm([rs]), nrm([rs]))
                                ob = onb16[jq % 2]
                                STT(ob[:], ot[:], rs[:, 3:4], subw[:], ALU.mult, ALU.mult, [ot, rs, subw], [ob])
                                DMA("sp", on_d[g * 128:(g + 1) * 128, h * 128:(h + 1) * 128], ob[:], [ob], [onb[g]])
            for t in range(NT):
                g = s * NT + t
                xt = xts[g % 2]
                DMA("sp", xt[:], src[g * 128:(g + 1) * 128, :], [srcb[g]], [xt])
                DMA("sp", ont[:], on_d[g * 128:(g + 1) * 128, :], [onb[g]], [ont])
                tb = banks[1]
                tbv = tb[:].bitcast(BF16).rearrange("p (h t) -> p h t", t=128)
                for h in range(8):
                    TR(tbv[:, h, :], ont[:, h * 128:(h + 1) * 128], identb[:], [ont, identb], [tb])
                CP("dve", onT[:, 0:4, :], tbv[:, 0:4, :], [tb], [onT])
                ACT(onT[:, 4:8, :], tbv[:, 4:8, :], AF.Copy, [tb], [onT])
                for half in range(2):
                    yb = banks[2 + half]
                    for h in range(8):
                        MM(yb[:], onT[:, h, :], w_out[:, h, half * 512:(half + 1) * 512], h == 0, h == 7, [onT, w_out], [yb])
                resid_ln(xt, [(banks[2][:], banks[2]), (banks[3][:], banks[3])], gbc, lng, lnb, rr, stat, dst[g * 128:(g + 1) * 128, :], dstb[g])
        P.pop()

    P.push()
    ztile = Tl("ztile", [128, 8192], BF16)
    MS("pool", ztile[:], 0.0, [ztile])
    zb = Buf("xbufzero")
    nrows = MOE_NBLK * 512
    r0 = 0
    while r0 < nrows:
        nr = min(1024, nrows - r0)
        DMA("sp", moe_xbuf_d[r0:r0 + nr, :].rearrange("(p j) d -> p (j d)", p=128), ztile[:, 0:(nr // 128) * 1024], [ztile], [zb])
        r0 += nr
    P.pop()
    phase_mod()
    P.barrier()
    cur, curb = x_d, xb["x"]
    for li, l in enumerate(layers):
        last = (li == len(layers) - 1)
        if "mix" in sub:
            dst, dstb = (out_d, xb["out"]) if (last and "moe" not in sub) else (xs[0], xb[0])
            if l % 2 == 0:
                phase_hgrn(l, cur, curb, dst, dstb)
            else:
                phase_attn(l, cur, curb, dst, dstb)
            cur, curb = dst, dstb
        if "moe" in sub:
            dst, dstb = (out_d, xb["out"]) if last else (xs[1], xb[1])
            phase_moe(l, cur, curb, dst, dstb)
            cur, curb = dst, dstb
    P.finish()
    P.DBG = DBG
    return nc, consts, P


_CACHE = {}


def run(inputs, NSEQ, S, layers, sub=("mix", "moe"), n_cores=8, NE=32):
    from concourse.bass_utils import run_bass_kernel_spmd
    nc, consts, P = build(NSEQ, S, layers, sub, NE)
    x = np.ascontiguousarray(inputs["x"], dtype=np.float32).reshape(n_cores, NSEQ * S, D)
    c = np.ascontiguousarray(inputs["c"], dtype=np.float32).reshape(n_cores, NSEQ, D)
    inputs = dict(inputs)
    rw = np.concatenate([np.asarray(inputs["router_g_w"], np.float32), np.asarray(inputs["router_e_w"], np.float32)], axis=2)
    inputs["router_w"] = np.ascontiguousarray(rw.reshape(rw.shape[0], 8, 128, 36).transpose(0, 2, 1, 3))
    inputs["router_b"] = np.concatenate([np.asarray(inputs["router_g_b"], np.float32), np.asarray(inputs["router_e_b"], np.float32)], axis=1)
    in_maps = []
    for i in range(n_cores):
        m = {"x": x[i], "cT": np.ascontiguousarray(c[i].reshape(NSEQ, 8, 128).transpose(2, 1, 0))}
        for nm, shp in wnames():
            m[nm] = np.ascontiguousarray(np.asarray(inputs[nm])[:shp[0]], dtype=np.float32)
        for nm, arr in consts.items():
            m[nm] = arr
        in_maps.append(m)
    res = run_bass_kernel_spmd(nc, in_maps, core_ids=list(range(n_cores)))
    out = np.stack([np.asarray(r["out"]) for r in res.results], 0)
    global LAST_DBG
    LAST_DBG = {k: np.asarray(res.results[0]["dbg_" + k]) for k in P.DBG}
    return out.reshape(n_cores * NSEQ, S, D)


def kernel(**inputs):
    out = run(inputs, 2, 4096, [0, 1, 2, 3])
    return out.astype(np.float32)
```
